# Optimizing a Trainium2 kernel written in Bass

```python
import math
import jax
import jax.numpy as jnp
from jax import lax
import numpy as np

D_MODEL = 2048
BATCH = 8
SEQ = 2048
DEPTH = 2

CTX_LEN = 256
GRID_W = 64
EPS = 1e-6
ROPE_BASE = 10000.0
Q_BLOCK = 128
N_BRANCH = 4
BRANCH_W = 512

DA_HEADS = 4
DA_HEAD = 64
DA_V = 2 * DA_HEAD
RT_HEADS = 4
RT_DK = 128
RT_DV = 128
RT_CHUNK = 128
NA_HEADS = 8
NA_HEAD = 64
NA_KH = 8
NA_KW = 16
MLA_HEADS = 4
MLA_Q_LORA = 512
MLA_KV_LORA = 256
MLA_NOPE = 128
MLA_ROPE = 64
MLA_V = 128
PEER_HEADS = 8
PEER_NKEYS = 128
PEER_EXPERTS = PEER_NKEYS * PEER_NKEYS
PEER_DQ = 256
PEER_TOPK = 16
PEER_BLOCK = 128

IN_SPLITS = (
    DA_HEADS * 2 * DA_HEAD, DA_HEADS * 2 * DA_HEAD, DA_HEADS * DA_V,
    RT_HEADS * RT_DK, RT_HEADS * RT_DK, RT_HEADS * RT_DV, RT_HEADS * RT_DV,
    NA_HEADS * NA_HEAD, NA_HEADS * NA_HEAD, NA_HEADS * NA_HEAD,
    MLA_Q_LORA, MLA_KV_LORA, MLA_ROPE)
IN_WIDTH = sum(IN_SPLITS)

kernel_name = 'hybrid_diffusion_gated_block'


def _rms(x, g):
    xf = x.astype(jnp.float32)
    y = xf * lax.rsqrt(jnp.mean(xf * xf, axis=-1, keepdims=True) + EPS)
    return (y * g.astype(jnp.float32)).astype(x.dtype)


def _modulate(x, g, shift, scale):
    return _rms(x, g) * (1 + scale) + shift


def _split_in(p):
    offs = np.cumsum(np.array(IN_SPLITS))[:-1].tolist()
    return jnp.split(p, offs, axis=-1)


def _rope_tables(n_tok, dim):
    m = dim // 2
    inv = ROPE_BASE ** (-jnp.arange(0, m, 2, dtype=jnp.float32) / m)
    t = jnp.arange(n_tok)
    row = (t // GRID_W).astype(jnp.float32)
    col = (t % GRID_W).astype(jnp.float32)
    ar = row[:, None] * inv
    ac = col[:, None] * inv
    ang = jnp.concatenate([ar, ar, ac, ac], axis=-1)
    return jnp.cos(ang), jnp.sin(ang)


def _rope(x, cos, sin):
    shape = (x.shape[1],) + (1,) * (x.ndim - 3) + (x.shape[-1],)
    a, b, c, d = jnp.split(x, 4, axis=-1)
    rot = jnp.concatenate([-b, a, -d, c], axis=-1)
    return (x * cos.reshape(shape) + rot * sin.reshape(shape)).astype(x.dtype)


def _attn(q, k, v, scale):
    s = jnp.einsum('bhqd,bhkd->bhqk', q, k).astype(jnp.float32) * scale
    p = jax.nn.softmax(s, axis=-1)
    return jnp.einsum('bhqk,bhkd->bhqd', p.astype(v.dtype), v)


def _map_query_blocks(fn, q, axis):
    n = q.shape[axis]
    nb = n // Q_BLOCK
    qb = q.reshape(q.shape[:axis] + (nb, Q_BLOCK) + q.shape[axis + 1:])
    out = lax.map(fn, jnp.moveaxis(qb, axis, 0))
    out = jnp.moveaxis(out, 0, -3)
    return out.reshape(out.shape[:-3] + (n, out.shape[-1]))


def _diff_heads(q, k, v, qk_g, rope):
    B, T, _ = q.shape
    q = _rms(q.reshape(B, T, DA_HEADS, 2, DA_HEAD), qk_g[0])
    k = _rms(k.reshape(B, T, DA_HEADS, 2, DA_HEAD), qk_g[1])
    if rope is not None:
        q = _rope(q, *rope)
        k = _rope(k, *rope)
    v = v.reshape(B, T, DA_HEADS, DA_V)
    return q.transpose(0, 2, 3, 1, 4), k.transpose(0, 2, 3, 1, 4), v.transpose(0, 2, 1, 3)


def _diff_core(q, k, v, lam):
    s = jnp.einsum('bhnqd,bhnkd->bhnqk', q, k).astype(jnp.float32) * (DA_HEAD ** -0.5)
    p = jax.nn.softmax(s, axis=-1)
    a = p[:, :, 0] - lam * p[:, :, 1]
    return jnp.einsum('bhqk,bhkd->bhqd', a.astype(v.dtype), v)


def _mix_diff(ql, kl, vl, qc, kc, vc, lam_vec, qk_g, sub_g, lam_init, rope, ctx_out):
    lv = lam_vec.astype(jnp.float32)
    lam = jnp.exp(jnp.sum(lv[0] * lv[1])) - jnp.exp(jnp.sum(lv[2] * lv[3])) + lam_init
    Ql, Kl, Vl = _diff_heads(ql, kl, vl, qk_g, rope)
    Qc, Kc, Vc = _diff_heads(qc, kc, vc, qk_g, None)
    K = jnp.concatenate([Kc, Kl], axis=3)
    V = jnp.concatenate([Vc, Vl], axis=2)
    yl = _map_query_blocks(lambda qb: _diff_core(qb, K, V, lam), Ql, axis=3)

    def post(y):
        B, H, T, dv = y.shape
        y = _rms(y, sub_g) * (1.0 - lam_init)
        return y.transpose(0, 2, 1, 3).reshape(B, T, H * dv)

    yc = post(_diff_core(Qc, Kc, Vc, lam)) if ctx_out else None
    return post(yl), yc


def _retention_scan(q, k, v, log_g, state0):
    C = RT_CHUNK
    B, H, T, _ = q.shape
    nc = T // C
    n = jnp.arange(C, dtype=jnp.float32)
    diff = n[:, None] - n[None, :]
    dmat = jnp.where(diff >= 0, jnp.exp(log_g[:, None, None] * jnp.maximum(diff, 0.0)), 0.0)
    xi = jnp.exp(log_g[:, None] * (n + 1.0))
    zeta = jnp.exp(log_g[:, None] * (C - 1.0 - n))
    g_c = jnp.exp(log_g * C)

    def chunks(a):
        return jnp.moveaxis(a.reshape(B, H, nc, C, a.shape[-1]), 2, 0)

    def step(R, xs):
        qc, kc, vc = xs
        inner = jnp.einsum('bhnm,bhme->bhne', jnp.einsum('bhnd,bhmd->bhnm', qc, kc) * dmat, vc)
        cross = jnp.einsum('bhnd,bhde->bhne', qc, R) * xi[..., None]
        R = g_c[:, None, None] * R + jnp.einsum('bhmd,bhme->bhde', kc * zeta[..., None], vc)
        return R, inner + cross

    R, out = lax.scan(step, state0, (chunks(q), chunks(k), chunks(v)))
    out = jnp.moveaxis(out, 0, 2).reshape(B, H, T, v.shape[-1])
    return out, R


def _retention_state(k, v, log_g):
    L = k.shape[2]
    m = jnp.arange(L, dtype=jnp.float32)
    w = jnp.exp(log_g[:, None] * (L - 1.0 - m))
    return jnp.einsum('bhmd,bhme->bhde', k * w[..., None], v)


def _ret_heads(q, k, v, rope):
    B, T, _ = q.shape
    q = q.reshape(B, T, RT_HEADS, RT_DK)
    k = k.reshape(B, T, RT_HEADS, RT_DK) * (RT_DK ** -0.5)
    if rope is not None:
        q = _rope(q, *rope)
        k = _rope(k, *rope)
    v = v.reshape(B, T, RT_HEADS, RT_DV)
    return [a.transpose(0, 2, 1, 3).astype(jnp.float32) for a in (q, k, v)]


def _mix_ret(ql, kl, vl, gl, qc, kc, vc, gc, decay_logit, norm_g, rope, ctx_out):
    Ql, Kl, Vl = _ret_heads(ql, kl, vl, rope)
    Qc, Kc, Vc = _ret_heads(qc, kc, vc, None)
    log_g = jax.nn.log_sigmoid(decay_logit.astype(jnp.float32))
    B, H = Ql.shape[:2]
    yl = 0.0
    yc = 0.0
    for d in range(2):
        if d == 0:
            t = lambda a: a
        else:
            t = lambda a: jnp.flip(a, axis=2)
        if ctx_out:
            oc, R = _retention_scan(t(Qc), t(Kc), t(Vc), log_g[d],
                                    jnp.zeros((B, H, RT_DK, RT_DV), jnp.float32))
            yc = yc + t(oc)
        else:
            R = _retention_state(t(Kc), t(Vc), log_g[d])
        ol, _ = _retention_scan(t(Ql), t(Kl), t(Vl), log_g[d], R)
        yl = yl + t(ol)

    def post(y, g):
        Bq, Hq, T, dv = y.shape
        y = _rms(y.transpose(0, 2, 1, 3), norm_g).reshape(Bq, T, Hq * dv)
        return (y * jax.nn.silu(g.astype(jnp.float32))).astype(g.dtype)

    return post(yl, gl), (post(yc, gc) if ctx_out else None)


def _na_heads(q, k, v, qk_g):
    B, T, _ = q.shape
    q = _rms(q.reshape(B, T, NA_HEADS, NA_HEAD), qk_g[0])
    k = _rms(k.reshape(B, T, NA_HEADS, NA_HEAD), qk_g[1])
    return q, k, v.reshape(B, T, NA_HEADS, NA_HEAD)


def _na_latent(q, k, v, kc, vc, rpb, rows):
    B, S, H, d = q.shape
    kh = min(NA_KH, rows)
    scale = NA_HEAD ** -0.5
    qg = q.reshape(B, rows, GRID_W, H, d)
    kg = k.reshape(B, rows, GRID_W, H, d)
    vg = v.reshape(B, rows, GRID_W, H, d)
    cols = np.arange(GRID_W)
    col_start = np.clip(cols - NA_KW // 2, 0, GRID_W - NA_KW)
    col_idx = col_start[:, None] + np.arange(NA_KW)[None, :]
    dc = col_idx - cols[:, None] + (NA_KW - 1)
    rpb_c = rpb[:, :, dc]

    def row_fn(args):
        r, q_row = args
        rs = jnp.clip(r - kh // 2, 0, rows - kh)
        k_win = lax.dynamic_slice_in_dim(kg, rs, kh, axis=1)[:, :, col_idx]
        v_win = lax.dynamic_slice_in_dim(vg, rs, kh, axis=1)[:, :, col_idx]
        dr = rs + jnp.arange(kh) - r + (NA_KH - 1)
        bias = jnp.take(rpb_c, dr, axis=1).transpose(0, 2, 1, 3)
        s_loc = jnp.einsum('bchd,bicjhd->bhcij', q_row, k_win).astype(jnp.float32) * scale
        s_loc = s_loc + bias[None].astype(jnp.float32)
        s_ctx = jnp.einsum('bchd,blhd->bhcl', q_row, kc).astype(jnp.float32) * scale
        n_loc = kh * NA_KW
        s = jnp.concatenate([s_loc.reshape(B, H, GRID_W, n_loc), s_ctx], axis=-1)
        p = jax.nn.softmax(s, axis=-1).astype(v.dtype)
        p_loc = p[..., :n_loc].reshape(B, H, GRID_W, kh, NA_KW)
        p_ctx = p[..., n_loc:]
        return (jnp.einsum('bhcij,bicjhd->bchd', p_loc, v_win)
                + jnp.einsum('bhcl,blhd->bchd', p_ctx, vc))

    out = lax.map(row_fn, (jnp.arange(rows), jnp.moveaxis(qg, 1, 0)))
    return jnp.moveaxis(out, 0, 1).reshape(B, S, H * d)


def _mix_na(ql, kl, vl, qc, kc, vc, qk_g, rpb, rows, ctx_out):
    Ql, Kl, Vl = _na_heads(ql, kl, vl, qk_g)
    Qc, Kc, Vc = _na_heads(qc, kc, vc, qk_g)
    yl = _na_latent(Ql, Kl, Vl, Kc, Vc, rpb, rows)
    yc = None
    if ctx_out:
        B, L = Qc.shape[:2]
        o = _attn(Qc.transpose(0, 2, 1, 3), Kc.transpose(0, 2, 1, 3), Vc.transpose(0, 2, 1, 3),
                  NA_HEAD ** -0.5)
        yc = o.transpose(0, 2, 1, 3).reshape(B, L, NA_HEADS * NA_HEAD)
    return yl, yc


def _mla_heads(cq, ckv, kr, q_norm_g, kv_norm_g, w_uq, w_ukv, qk_g, rope):
    B, T, _ = cq.shape
    q = (_rms(cq, q_norm_g) @ w_uq).reshape(B, T, MLA_HEADS, MLA_NOPE + MLA_ROPE)
    kv = (_rms(ckv, kv_norm_g) @ w_ukv).reshape(B, T, MLA_HEADS, MLA_NOPE + MLA_V)
    k_nope, v = kv[..., :MLA_NOPE], kv[..., MLA_NOPE:]
    k = jnp.concatenate([k_nope, jnp.broadcast_to(kr[:, :, None, :], (B, T, MLA_HEADS, MLA_ROPE))], axis=-1)
    q = _rms(q, qk_g[0])
    k = _rms(k, qk_g[1])
    if rope is not None:
        q = jnp.concatenate([q[..., :MLA_NOPE], _rope(q[..., MLA_NOPE:], *rope)], axis=-1)
        k = jnp.concatenate([k[..., :MLA_NOPE], _rope(k[..., MLA_NOPE:], *rope)], axis=-1)
    return q.transpose(0, 2, 1, 3), k.transpose(0, 2, 1, 3), v.transpose(0, 2, 1, 3)


def _mix_mla(cql, ckvl, krl, cqc, ckvc, krc, q_norm_g, kv_norm_g, w_uq, w_ukv, qk_g, rope, ctx_out):
    scale = (MLA_NOPE + MLA_ROPE) ** -0.5
    Ql, Kl, Vl = _mla_heads(cql, ckvl, krl, q_norm_g, kv_norm_g, w_uq, w_ukv, qk_g, rope)
    Qc, Kc, Vc = _mla_heads(cqc, ckvc, krc, q_norm_g, kv_norm_g, w_uq, w_ukv, qk_g, None)
    K = jnp.concatenate([Kc, Kl], axis=2)
    V = jnp.concatenate([Vc, Vl], axis=2)
    yl = _map_query_blocks(lambda qb: _attn(qb, K, V, scale), Ql, axis=2)

    def post(y):
        B, H, T, dv = y.shape
        return y.transpose(0, 2, 1, 3).reshape(B, T, H * dv)

    yc = post(_attn(Qc, Kc, Vc, scale)) if ctx_out else None
    return post(yl), yc


def _merge(h, ys, w_branch, w_gate, b_gate, w_o):
    m = 0.0
    for n in range(N_BRANCH):
        gate = jax.nn.sigmoid(h @ w_gate[n] + b_gate[n])
        m = m + gate * (ys[n] @ w_branch[n])
    return m @ w_o


def _peer(h, w_query, sub_keys, u, v):
    B, T, D = h.shape
    hb_all = h.reshape(-1, PEER_BLOCK, D)
    kk = PEER_TOPK * PEER_TOPK

    def block(hb):
        q = (hb @ w_query).reshape(PEER_BLOCK, PEER_HEADS, 2, PEER_DQ // 2)
        s = jnp.einsum('phsd,hskd->phsk', q, sub_keys).astype(jnp.float32)
        top_s, top_i = lax.top_k(s, PEER_TOPK)
        cand_s = (top_s[:, :, 0, :, None] + top_s[:, :, 1, None, :]).reshape(PEER_BLOCK, PEER_HEADS, kk)
        cand_i = (top_i[:, :, 0, :, None] * PEER_NKEYS + top_i[:, :, 1, None, :]).reshape(PEER_BLOCK, PEER_HEADS, kk)
        sel_s, pos = lax.top_k(cand_s, PEER_TOPK)
        idx = jnp.take_along_axis(cand_i, pos, axis=-1)
        g = jax.nn.softmax(sel_s, axis=-1)
        ue = u[idx]
        ve = v[idx]
        a = jax.nn.gelu(jnp.einsum('pd,phkd->phk', hb, ue).astype(jnp.float32), approximate=False)
        return jnp.einsum('phk,phkd->pd', (g * a).astype(ve.dtype), ve)

    return lax.map(block, hb_all).reshape(B, T, D)


def _layer(x, xc, c, c_ctx, lp, layer_idx, last, rows, rope_a, rope_r):
    ctx_out = not last
    mod = jax.nn.silu(c) @ lp['w_mod'] + lp['b_mod']
    sh1, sc1, g1, sh2, sc2, g2 = [m[:, None, :] for m in jnp.split(mod, 6, axis=-1)]
    modc = jax.nn.silu(c_ctx) @ lp['w_mod'] + lp['b_mod']
    sh1c, sc1c, g1c, sh2c, sc2c, g2c = jnp.split(modc, 6)

    h = _modulate(x, lp['norm1_g'], sh1, sc1)
    hc = _modulate(xc, lp['norm1_g'], sh1c, sc1c)
    pl = _split_in(h @ lp['w_in'])
    pc = _split_in(hc @ lp['w_in'])

    lam_init = 0.8 - 0.6 * math.exp(-0.3 * layer_idx)
    yA = _mix_diff(pl[0], pl[1], pl[2], pc[0], pc[1], pc[2], lp['diff_lambda'], lp['diff_qk_g'],
                   lp['diff_sub_g'], lam_init, rope_a, ctx_out)
    yB = _mix_ret(pl[3], pl[4], pl[5], pl[6], pc[3], pc[4], pc[5], pc[6], lp['ret_decay'],
                  lp['ret_norm_g'], rope_r, ctx_out)
    yC = _mix_na(pl[7], pl[8], pl[9], pc[7], pc[8], pc[9], lp['na_qk_g'], lp['na_rpb'], rows, ctx_out)
    yD = _mix_mla(pl[10], pl[11], pl[12], pc[10], pc[11], pc[12], lp['mla_q_norm_g'],
                  lp['mla_kv_norm_g'], lp['w_uq'], lp['w_ukv'], lp['mla_qk_g'], rope_a, ctx_out)

    x = x + g1 * _merge(h, [yA[0], yB[0], yC[0], yD[0]], lp['w_branch'], lp['w_gate'], lp['b_gate'], lp['w_o'])
    x = x + g2 * _peer(_modulate(x, lp['norm2_g'], sh2, sc2), lp['peer_w_query'], lp['peer_sub_keys'],
                       lp['peer_u'], lp['peer_v'])
    if last:
        return x, None
    xc = xc + g1c * _merge(hc, [yA[1], yB[1], yC[1], yD[1]], lp['w_branch'], lp['w_gate'], lp['b_gate'], lp['w_o'])
    xc = xc + g2c * _peer(_modulate(xc, lp['norm2_g'], sh2c, sc2c), lp['peer_w_query'], lp['peer_sub_keys'],
                          lp['peer_u'], lp['peer_v'])
    return x, xc


def setup_inputs(seed: int = 0) -> dict:
    key = jax.random.key(seed)
    ks = jax.random.split(key, 32)
    f32 = jnp.float32
    D = D_MODEL

    def nrm(k, shape, scale):
        return jax.random.normal(k, shape, f32) * scale

    def gain(k, shape):
        return 1.0 + 0.02 * jax.random.normal(k, shape, f32)

    base = 1.0 - np.exp(np.linspace(np.log(1.0 / 32), np.log(1.0 / 512), RT_HEADS))
    decay_logit = np.log(base / (1.0 - base)).astype(np.float32)

    return {
        'x': nrm(ks[0], (BATCH, SEQ, D), 1.0),
        'c': nrm(ks[1], (BATCH, D), 1.0),
        'ctx': nrm(ks[2], (BATCH, CTX_LEN, D), 1.0),
        'c_ctx': nrm(ks[3], (D,), 1.0),
        'w_mod': nrm(ks[4], (DEPTH, D, 6 * D), 0.5 * D ** -0.5),
        'b_mod': nrm(ks[5], (DEPTH, 6 * D), 0.01),
        'norm1_g': gain(ks[6], (DEPTH, D)),
        'norm2_g': gain(ks[7], (DEPTH, D)),
        'w_in': nrm(ks[8], (DEPTH, D, IN_WIDTH), D ** -0.5),
        'diff_lambda': nrm(ks[9], (DEPTH, 4, DA_HEAD), 0.1),
        'diff_qk_g': gain(ks[10], (DEPTH, 2, DA_HEAD)),
        'diff_sub_g': gain(ks[11], (DEPTH, DA_V)),
        'ret_decay': jnp.asarray(decay_logit)[None, None, :] + nrm(ks[12], (DEPTH, 2, RT_HEADS), 0.1),
        'ret_norm_g': gain(ks[13], (DEPTH, RT_DV)),
        'na_qk_g': gain(ks[14], (DEPTH, 2, NA_HEAD)),
        'na_rpb': nrm(ks[15], (DEPTH, NA_HEADS, 2 * NA_KH - 1, 2 * NA_KW - 1), 0.02),
        'mla_q_norm_g': gain(ks[16], (DEPTH, MLA_Q_LORA)),
        'mla_kv_norm_g': gain(ks[17], (DEPTH, MLA_KV_LORA)),
        'w_uq': nrm(ks[18], (DEPTH, MLA_Q_LORA, MLA_HEADS * (MLA_NOPE + MLA_ROPE)), MLA_Q_LORA ** -0.5),
        'w_ukv': nrm(ks[19], (DEPTH, MLA_KV_LORA, MLA_HEADS * (MLA_NOPE + MLA_V)), MLA_KV_LORA ** -0.5),
        'mla_qk_g': gain(ks[20], (DEPTH, 2, MLA_NOPE + MLA_ROPE)),
        'w_branch': nrm(ks[21], (DEPTH, N_BRANCH, BRANCH_W, D), BRANCH_W ** -0.5),
        'w_gate': nrm(ks[22], (DEPTH, N_BRANCH, D, D), D ** -0.5),
        'b_gate': nrm(ks[23], (DEPTH, N_BRANCH, D), 0.01),
        'w_o': nrm(ks[24], (DEPTH, D, D), D ** -0.5),
        'peer_w_query': nrm(ks[25], (DEPTH, D, PEER_HEADS * PEER_DQ), D ** -0.5),
        'peer_sub_keys': nrm(ks[26], (DEPTH, PEER_HEADS, 2, PEER_NKEYS, PEER_DQ // 2), (PEER_DQ // 2) ** -0.5),
        'peer_u': nrm(ks[27], (DEPTH, PEER_EXPERTS, D), D ** -0.5),
        'peer_v': nrm(ks[28], (DEPTH, PEER_EXPERTS, D), PEER_HEADS ** -0.5),
    }


def reference(x, c, ctx, c_ctx, w_mod, b_mod, norm1_g, norm2_g, w_in, diff_lambda, diff_qk_g,
              diff_sub_g, ret_decay, ret_norm_g, na_qk_g, na_rpb, mla_q_norm_g, mla_kv_norm_g,
              w_uq, w_ukv, mla_qk_g, w_branch, w_gate, b_gate, w_o, peer_w_query, peer_sub_keys,
              peer_u, peer_v):
    n_tok = x.shape[1]
    rows = n_tok // GRID_W
    rope_a = _rope_tables(n_tok, DA_HEAD)
    rope_r = _rope_tables(n_tok, RT_DK)
    xc = ctx
    for l in range(DEPTH):
        lp = dict(w_mod=w_mod[l], b_mod=b_mod[l], norm1_g=norm1_g[l], norm2_g=norm2_g[l],
                  w_in=w_in[l], diff_lambda=diff_lambda[l], diff_qk_g=diff_qk_g[l],
                  diff_sub_g=diff_sub_g[l], ret_decay=ret_decay[l], ret_norm_g=ret_norm_g[l],
                  na_qk_g=na_qk_g[l], na_rpb=na_rpb[l], mla_q_norm_g=mla_q_norm_g[l],
                  mla_kv_norm_g=mla_kv_norm_g[l], w_uq=w_uq[l], w_ukv=w_ukv[l],
                  mla_qk_g=mla_qk_g[l], w_branch=w_branch[l], w_gate=w_gate[l],
                  b_gate=b_gate[l], w_o=w_o[l], peer_w_query=peer_w_query[l],
                  peer_sub_keys=peer_sub_keys[l], peer_u=peer_u[l], peer_v=peer_v[l])
        x, xc = _layer(x, xc, c, c_ctx, lp, l, l == DEPTH - 1, rows, rope_a, rope_r)
    return x
```

```python
import bisect
import math
from contextlib import ExitStack

import numpy as np
import concourse.bass as bass
import concourse.mybir as mybir
from concourse.bass_utils import run_bass_kernel_spmd

F32 = mybir.dt.float32
BF16 = mybir.dt.bfloat16
AF = mybir.ActivationFunctionType
ALU = mybir.AluOpType
AX = mybir.AxisListType

D = 2048
T = 2048
L = 256
NT = T + L
NTB = NT // 128
DEPTH = 2
INW = 5952
EPS = 1e-6
TG = [(0, 256, 1)] + [(256 + 512 * i, 512, 0) for i in range(4)]
NEG = -30000.0
GATE_TBS = None
ATTN_WARM = 0


class Res:
    __slots__ = ("name", "w", "r")

    def __init__(self, name=""):
        self.name = name
        self.w = None
        self.r = {}


class FW:
    NDS = 24

    def __init__(self, nc, es, same_engine_sync=True):
        self.nc = nc
        self.E = {"pe": nc.tensor, "dve": nc.vector, "act": nc.scalar, "pool": nc.gpsimd, "sp": nc.sync}
        self.csem = {e: es.enter_context(nc.semaphore("c_" + e)) for e in ("pe", "dve", "act", "pool")}
        self.seq = {e: 0 for e in self.csem}
        self.last = {e: None for e in self.csem}
        self.incd = {e: ([], []) for e in self.csem}
        self.dsems = [es.enter_context(nc.semaphore("d%d" % i)) for i in range(self.NDS)]
        self.dcnt = [0] * self.NDS
        self.qsl = {"sp": (0, 14), "pool": (14, 24)}
        self.dnext = {"sp": 0, "pool": 14}
        self.seen = {e: {} for e in self.E}
        self.same = same_engine_sync
        self.nwait = 0
        self.nins = 0

    def _resolve(self, y, seq):
        seqs, counts = self.incd[y]
        i = bisect.bisect_left(seqs, seq)
        if i < len(seqs):
            return seqs[i], counts[i]
        h = self.last[y]
        cnt = (counts[-1] if counts else 0) + 1
        h.then_inc(self.csem[y], 1)
        seqs.append(self.seq[y])
        counts.append(cnt)
        return self.seq[y], cnt

    def _wait(self, eng, tok):
        if tok[0] == "c":
            _, y, seq = tok
            if y == eng and (not self.same or eng == "pe"):
                return
            key = ("c", y)
            if self.seen[eng].get(key, 0) >= seq:
                return
            s2, cnt = self._resolve(y, seq)
            self.E[eng].wait_ge(self.csem[y], cnt)
            self.seen[eng][key] = s2
        else:
            _, i, cnt = tok
            key = ("d", i)
            if self.seen[eng].get(key, 0) >= cnt:
                return
            self.E[eng].wait_ge(self.dsems[i], cnt)
            self.seen[eng][key] = cnt
        self.nwait += 1

    def _deps(self, eng, reads, writes):
        for r in reads:
            if r.w is not None:
                self._wait(eng, r.w)
        for w in writes:
            if w.w is not None:
                self._wait(eng, w.w)
            for k, v in w.r.items():
                self._wait(eng, (k[0], k[1], v))

    def op(self, eng, fn, reads=(), writes=(), inc=False):
        self._deps(eng, reads, writes)
        ins = fn(self.E[eng])
        self.nins += 1
        self.seq[eng] += 1
        self.last[eng] = ins
        s = self.seq[eng]
        if inc or eng != "pe":
            seqs, counts = self.incd[eng]
            ins.then_inc(self.csem[eng], 1)
            seqs.append(s)
            counts.append((counts[-1] if counts else 0) + 1)
        for r in reads:
            r.r[("c", eng)] = s
        for w in writes:
            w.w = ("c", eng, s)
            w.r = {}
        return ins

    def dma(self, q, out, in_, reads=(), writes=(), **kw):
        self._deps(q, reads, writes)
        i = self.dnext[q]
        lo, hi = self.qsl[q]
        self.dnext[q] = lo + (i + 1 - lo) % (hi - lo)
        self.dcnt[i] += 16
        self.E[q].dma_start(out=out, in_=in_, **kw).then_inc(self.dsems[i], 16)
        self.nins += 1
        for r in reads:
            r.r[("d", i)] = self.dcnt[i]
        for w in writes:
            w.w = ("d", i, self.dcnt[i])
            w.r = {}

    def barrier(self, engines=("sp", "pe", "dve", "act", "pool")):
        for eng in engines:
            for i in range(self.NDS):
                if self.dcnt[i]:
                    self._wait(eng, ("d", i, self.dcnt[i]))
            for y in self.csem:
                if self.seq[y] and y != eng:
                    self._wait(eng, ("c", y, self.seq[y]))


SM = {}
_o = 0
for _n, _w in [("norm1_g", 16), ("norm2_g", 16), ("b_gate", 64), ("b_mod", 96), ("dqg", 1), ("dkg", 1),
               ("dsub", 1), ("rnorm", 1), ("nqg", 1), ("nkg", 1), ("mqn", 4), ("mkvn", 2),
               ("mq_nope", 1), ("mq_rope", 1), ("mk_nope", 1), ("mk_rope", 1),
               ("dlam", 256), ("rdecay", 8)]:
    SM[_n] = (_o, _w)
    _o += _w
NSM = _o


def _cols(v):
    v = np.asarray(v, np.float32).reshape(-1, 128)
    return np.ascontiguousarray(v.T)


def pack_smalls(inp, l):
    s = np.zeros((128, NSM), np.float32)

    def put(name, arr):
        o, w = SM[name]
        arr = np.asarray(arr, np.float32).reshape(128, w)
        s[:, o:o + w] = arr

    put("norm1_g", _cols(inp["norm1_g"][l]))
    put("norm2_g", _cols(inp["norm2_g"][l]))
    put("b_gate", np.stack([_cols(inp["b_gate"][l][n]) for n in range(4)], axis=1))
    put("b_mod", _cols(inp["b_mod"][l]))
    put("dqg", np.tile(inp["diff_qk_g"][l][0], 2))
    put("dkg", np.tile(inp["diff_qk_g"][l][1], 2))
    put("dsub", inp["diff_sub_g"][l])
    put("rnorm", inp["ret_norm_g"][l])
    put("nqg", np.tile(inp["na_qk_g"][l][0], 2))
    put("nkg", np.tile(inp["na_qk_g"][l][1], 2))
    put("mqn", _cols(inp["mla_q_norm_g"][l]))
    put("mkvn", _cols(inp["mla_kv_norm_g"][l]))
    put("mq_nope", inp["mla_qk_g"][l][0][:128])
    put("mq_rope", np.tile(inp["mla_qk_g"][l][0][128:], 2))
    put("mk_nope", inp["mla_qk_g"][l][1][:128])
    put("mk_rope", np.tile(inp["mla_qk_g"][l][1][128:], 2))
    put("dlam", np.tile(inp["diff_lambda"][l].reshape(1, 256), (128, 1)))
    put("rdecay", np.tile(inp["ret_decay"][l].reshape(1, 8), (128, 1)))
    return s


class Prog:
    def __init__(self, ext_in=(), ext_out=(), same_engine_sync=True):
        self.nc = bass.Bass("TRN2", target_bir_lowering=False)
        self.es = ExitStack()
        self.fw = FW(self.nc, self.es, same_engine_sync)
        self.ext_in = set(ext_in)
        self.ext_out = set(ext_out)
        self.dr = {}
        self.inputs = []
        self.outputs = []
        nc = self.nc
        self.ps = []
        self.psr = []
        for i in range(8):
            self.ps.append(self.es.enter_context(nc.psum_tensor("ps%d" % i, [128, 512], F32)))
            self.psr.append(Res("ps%d" % i))
        self.pnext = 0
        self.pes = None

    def inp(self, name, shape, dt=F32):
        t = self.nc.dram_tensor(name, list(shape), dt, kind="ExternalInput")
        self.inputs.append(name)
        self.dr[name] = (t, Res(name))
        return t

    def scratch(self, name, shape, dt):
        if name in self.ext_in:
            kind = "ExternalInput"
            self.inputs.append(name)
        elif name in self.ext_out:
            kind = "ExternalOutput"
            self.outputs.append(name)
        else:
            kind = "Internal"
        t = self.nc.dram_tensor(name, list(shape), dt, kind=kind)
        self.dr[name] = (t, Res(name))
        return t

    def out(self, name, shape, dt=F32):
        t = self.nc.dram_tensor(name, list(shape), dt, kind="ExternalOutput")
        self.outputs.append(name)
        self.dr[name] = (t, Res(name))
        return t

    def R(self, name):
        return self.dr[name][1]

    def phase_begin(self, name=None):
        self.fw.barrier()
        self.pes = ExitStack()
        self.pidx = getattr(self, "pidx", 0) + 1
        if name is None:
            import inspect
            name = inspect.stack()[1].function
        self.pes.enter_context(self.nc.named_scope("%02d_%s" % (self.pidx, name)))

    def phase_end(self):
        self.fw.barrier()
        self.pes.close()
        self.pes = None

    def sb(self, name, shape, dt=F32, persistent=False):
        es = self.es if persistent else self.pes
        self.uid = getattr(self, "uid", 0) + 1
        t = es.enter_context(self.nc.sbuf_tensor("sb%d_%s" % (self.uid, name), list(shape), dt))
        return t, Res(name)

    def bank(self, i=None):
        if i is None:
            i = self.pnext
            self.pnext = (self.pnext + 1) % 8
        return self.ps[i], self.psr[i]


def rope_tables(n_tok, dim):
    m = dim // 2
    inv = 10000.0 ** (-np.arange(0, m, 2, dtype=np.float32) / m)
    t = np.arange(n_tok)
    row = (t // 64).astype(np.float32)
    col = (t % 64).astype(np.float32)
    ar = row[:, None] * inv
    ac = col[:, None] * inv
    ang = np.concatenate([ar, ar, ac, ac], axis=-1).astype(np.float32)
    return np.cos(ang).astype(np.float32), np.sin(ang).astype(np.float32)


def rot_matrix(dim, reps):
    q = dim // 4
    Rm = np.zeros((dim * reps, dim * reps), np.float32)
    for r in range(reps):
        o = r * dim
        for i in range(q):
            Rm[o + q + i, o + i] = -1.0
            Rm[o + i, o + q + i] = 1.0
            Rm[o + 3 * q + i, o + 2 * q + i] = -1.0
            Rm[o + 2 * q + i, o + 3 * q + i] = 1.0
    return Rm


def build_consts():
    c = {}
    c["ident"] = np.eye(128, dtype=np.float32)
    c["ones"] = np.ones((128, 128), np.float32)
    bd = np.zeros((128, 128), np.float32)
    bd[:64, :64] = 1.0
    bd[64:, 64:] = 1.0
    c["bd64"] = bd
    cos64, sin64 = rope_tables(T, 64)
    c["cos64"] = np.ascontiguousarray(np.tile(cos64.T, (2, 1)))
    c["sin64"] = np.ascontiguousarray(np.tile(sin64.T, (2, 1)))
    cos128, sin128 = rope_tables(T, 128)
    c["cos128"] = np.ascontiguousarray(cos128.T)
    c["sin128"] = np.ascontiguousarray(sin128.T)
    n = np.arange(128, dtype=np.float32)
    dif = n[None, :] - n[:, None]
    c["rett"] = np.ascontiguousarray(np.concatenate([
        np.maximum(dif, 0), (dif >= 0).astype(np.float32), np.maximum(-dif, 0), (dif <= 0).astype(np.float32),
        np.tile(n[None, :] + 1.0, (128, 1)), np.tile(128.0 - n[None, :], (128, 1)),
        (127.0 - n)[:, None], n[:, None]], axis=1).astype(np.float32))
    c["rot64"] = rot_matrix(64, 2)
    c["rot128"] = rot_matrix(128, 1)
    return c


def ph_setup(P, do_mod=True):
    fw, nc = P.fw, P.nc
    P.phase_begin()
    C = {}
    for nm in ("ident", "ones", "bd64", "rot64", "rot128"):
        t, r = P.sb("c_" + nm, [128, 128], F32, persistent=True)
        fw.dma("sp", t[:], P.dr["k_" + nm][0].ap(), writes=[r])
        tb, rb = P.sb("cb_" + nm, [128, 128], BF16, persistent=True)
        fw.op("dve", lambda e, tb=tb, t=t: e.tensor_copy(tb[:], t[:]), reads=[r], writes=[rb])
        C[nm] = (t, r)
        C[nm + "_bf"] = (tb, rb)
    sm, smr = P.sb("smalls", [128, DEPTH, NSM], F32, persistent=True)
    fw.dma("sp", sm[:], P.dr["smalls"][0].ap().rearrange("l p n -> p l n"), writes=[smr])
    C["sm"] = (sm, smr)
    P.C = C

    modv, modr = P.sb("modv", [128, DEPTH, 6, 16, 2], F32, persistent=True)
    a12, a12r = P.sb("a12", [128, DEPTH, 2, 16, 2], F32, persistent=True)
    cv, cvr = P.sb("cvec", [128, 16, 2], F32)
    fw.dma("sp", cv[:], P.dr["cvec"][0].ap(), writes=[cvr])
    sv, svr = P.sb("svec", [128, 16, 2], BF16)
    fw.op("act", lambda e: e.activation(out=sv[:], in_=cv[:], func=AF.Silu), reads=[cvr], writes=[svr])
    NB = 2
    wb = [P.sb("wmod%d" % i, [128, 3072], BF16) for i in range(NB)]
    it = 0
    for l in range(DEPTH if do_mod else 0):
        wm = P.dr["w_mod"][0].ap()
        for g in range(4):
            pst, psr = P.bank()
            for kc in range(16):
                wt, wr = wb[it % NB]
                it += 1
                fw.dma("pool", wt[:], wm[l, kc * 128:(kc + 1) * 128, g * 3072:(g + 1) * 3072], writes=[wr])
                for n in range(24):
                    fw.op("pe", lambda e, n=n, wt=wt, kc=kc, pst=pst: e.matmul(
                        pst[:, n * 2:n * 2 + 2], wt[:, n * 128:(n + 1) * 128], sv[:, kc, :],
                        start=(kc == 0 and n == 0), stop=(kc == 15 and n == 23), skip_group_check=True),
                        reads=[wr, svr], writes=[psr])
            o, w = SM["b_mod"]
            mv = modv[:, l].rearrange("p m f j -> p (m f) j")
            fw.op("dve", lambda e, g=g, pst=pst, mv=mv, l=l, o=o: e.tensor_tensor(
                mv[:, g * 24:(g + 1) * 24, :], pst[:, 0:48].rearrange("p (n j) -> p n j", j=2),
                sm[:, l, o + g * 24:o + (g + 1) * 24].unsqueeze(2).to_broadcast([128, 24, 2]), ALU.add),
                reads=[psr, smr], writes=[modr])
    for l in range(DEPTH if do_mod else 0):
        for w_, (gn, mi) in enumerate((("norm1_g", 1), ("norm2_g", 4))):
            o, _ = SM[gn]
            fw.op("dve", lambda e, l=l, w_=w_, mi=mi, o=o: e.scalar_tensor_tensor(
                a12[:, l, w_], modv[:, l, mi], 1.0, sm[:, l, o:o + 16].unsqueeze(2).to_broadcast([128, 16, 2]),
                ALU.add, ALU.mult), reads=[modr, smr], writes=[a12r])
    P.modv, P.modr, P.a12, P.a12r = modv, modr, a12, a12r
    P.phase_end()


def ph_transpose_in(P):
    fw = P.fw
    P.phase_begin()
    ident, identr = P.C["ident"]
    xT, xTr = P.dr["xT"]
    xTv = xT.ap().rearrange("(fc p) t -> p fc t", p=128)
    srcs = [(P.dr["ctx_b"], 0, 2), (P.dr["x_b"], 0, 4), (P.dr["x_b"], 4, 4), (P.dr["x_b"], 8, 4), (P.dr["x_b"], 12, 4)]
    xin = [P.sb("xin%d" % i, [128, 2048], F32) for i in range(2)]
    stg = [P.sb("xstg%d" % i, [128, 16, 512], F32) for i in range(2)]
    it = 0
    tok0 = 0
    for gi, ((src, srcr), b0, nb) in enumerate(srcs):
        st, sr = stg[gi % 2]
        for j in range(nb):
            xt, xr = xin[it % 2]
            it += 1
            fw.dma("sp", xt[:], src.ap()[(b0 + j) * 128:(b0 + j + 1) * 128, :], reads=[srcr], writes=[xr])
            for f4 in range(4):
                pst, psr = P.bank()
                for q in range(4):
                    fc = f4 * 4 + q
                    fw.op("pe", lambda e, pst=pst, q=q, xt=xt, fc=fc: e.transpose(
                        pst[:, q * 128:(q + 1) * 128], xt[:, fc * 128:(fc + 1) * 128], ident[:]),
                        reads=[xr, identr], writes=[psr])
                eng = "dve" if f4 % 2 == 0 else "act"
                dst = st[:, f4 * 4:(f4 + 1) * 4, j * 128:(j + 1) * 128]
                srcp = pst[:].rearrange("p (q t) -> p q t", q=4)
                if eng == "dve":
                    fw.op("dve", lambda e, dst=dst, srcp=srcp: e.tensor_copy(dst, srcp), reads=[psr], writes=[sr])
                else:
                    fw.op("act", lambda e, dst=dst, srcp=srcp: e.copy(dst, srcp), reads=[psr], writes=[sr])
        w = nb * 128
        fw.dma("sp", xTv[:, :, tok0:tok0 + w], st[:, :, 0:w], reads=[sr], writes=[xTr])
        tok0 += w
    P.phase_end()


def ph_norm(P, l, which, dst):
    fw = P.fw
    P.phase_begin()
    ones, onesr = P.C["ones"]
    xT, xTr = P.dr["xT"]
    hT, hTr = P.dr[dst]
    xTv = xT.ap().rearrange("(fc p) t -> p fc t", p=128)
    hTv = hT.ap().rearrange("(fc p) t -> p fc t", p=128)
    shi = 0 if which == 0 else 3

    def lane(k):
        xt, xr = P.sb("nx%d" % k, [128, 16, 512], F32)
        sqs = [P.sb("nsq%d_%d" % (k, i), [128, 512], F32) for i in range(2)]
        rs, rsr = P.sb("nrs%d" % k, [128, 512], F32)
        tmp = [P.sb("ntmp%d_%d" % (k, i), [128, 512], F32) for i in range(2)]
        ht, hr = P.sb("nh%d" % k, [128, 16, 512], BF16)
        for gi in range(k, len(TG), 2):
            t0, w, j = TG[gi]
            for q4 in range(4):
                fw.dma("sp", xt[:, q4 * 4:(q4 + 1) * 4, 0:w], xTv[:, q4 * 4:(q4 + 1) * 4, t0:t0 + w], reads=[xTr], writes=[xr])
            yield
            pst, psr = P.bank()
            for fc in range(16):
                s_, s_r = sqs[fc % 2]
                fw.op("act", lambda e, s_=s_, fc=fc: e.activation(out=s_[:, 0:w], in_=xt[:, fc, 0:w], func=AF.Square),
                      reads=[xr], writes=[s_r])
                yield
                fw.op("pe", lambda e, s_=s_, fc=fc: e.matmul(pst[:, 0:w], ones[:], s_[:, 0:w], start=(fc == 0), stop=(fc == 15)),
                      reads=[onesr, s_r], writes=[psr])
                yield
            fw.op("act", lambda e: e.activation(out=rs[:, 0:w], in_=pst[:, 0:w], func=AF.Sqrt, bias=EPS, scale=1.0 / D),
                  reads=[psr], writes=[rsr])
            yield
            fw.op("dve", lambda e: e.reciprocal(rs[:, 0:w], rs[:, 0:w]), reads=[rsr], writes=[rsr])
            yield
            for fc in range(16):
                tt, tr = tmp[fc % 2]
                fw.op("dve", lambda e, tt=tt, fc=fc: e.tensor_tensor(tt[:, 0:w], xt[:, fc, 0:w], rs[:, 0:w], ALU.mult),
                      reads=[xr, rsr], writes=[tr])
                yield
                fw.op("act", lambda e, tt=tt, fc=fc: e.activation(
                    out=ht[:, fc, 0:w], in_=tt[:, 0:w], func=AF.Identity,
                    bias=P.modv[:, l, shi, fc, j:j + 1], scale=P.a12[:, l, which, fc, j:j + 1]),
                    reads=[tr, P.modr, P.a12r], writes=[hr])
                yield
            fw.dma("sp", hTv[:, :, t0:t0 + w], ht[:, :, 0:w], reads=[hr], writes=[hTr])
            yield

    interleave([lane(0), lane(1)])
    P.phase_end()


def ph_inproj(P, l):
    fw = P.fw
    P.phase_begin()
    hT, hTr = P.dr["hT"]
    pT, pTr = P.dr["pT"]
    pV, pVr = P.dr["pV"]
    h, hr = P.sb("ih", [128, 16, NT], BF16)
    hv = hT.ap().rearrange("(fc p) t -> p fc t", p=128)
    for q in range(4):
        fw.dma("sp", h[:, q * 4:(q + 1) * 4, :], hv[:, q * 4:(q + 1) * 4, :], reads=[hTr], writes=[hr])
    wbuf = [P.sb("iw%d" % i, [128, 16, 512], BF16) for i in range(2)]
    stg = [P.sb("istg%d" % i, [128, NT], F32) for i in range(2)]
    vst = [P.sb("ivst%d" % i, [128, 512], BF16) for i in range(2)]
    win = P.dr["w_in"][0].ap()
    vmap = {2: 0, 5: 1, 9: 2}
    ev = 0
    si = 0
    for cg in range(12):
        c0 = cg * 512
        cw = min(512, INW - c0)
        wt, wr = wbuf[cg % 2]
        fw.dma("pool", wt[:, :, 0:cw], win[l, :, c0:c0 + cw].rearrange("(kc p) n -> p kc n", p=128), writes=[wr])
        if cg in vmap:
            vi = vmap[cg]
            for tb in range(NTB):
                pst, psr = P.bank()
                for kc in range(16):
                    fw.op("pe", lambda e, pst=pst, kc=kc, tb=tb, wt=wt: e.matmul(
                        pst[:, :], h[:, kc, tb * 128:(tb + 1) * 128], wt[:, kc, :], start=(kc == 0), stop=(kc == 15)),
                        reads=[hr, wr], writes=[psr])
                vt, vr = vst[tb % 2]
                if ev % 2 == 0:
                    fw.op("dve", lambda e, vt=vt, pst=pst: e.tensor_copy(vt[:], pst[:]), reads=[psr], writes=[vr])
                else:
                    fw.op("act", lambda e, vt=vt, pst=pst: e.copy(vt[:], pst[:]), reads=[psr], writes=[vr])
                ev += 1
                fw.dma("sp", pV.ap()[vi, tb * 128:(tb + 1) * 128, :], vt[:], reads=[vr], writes=[pVr])
            continue
        for oc in range((cw + 127) // 128):
            m = min(128, cw - oc * 128)
            st, sr = stg[si % 2]
            si += 1
            for (t0, w, j) in TG:
                pst, psr = P.bank()
                for kc in range(16):
                    fw.op("pe", lambda e, pst=pst, kc=kc, wt=wt, oc=oc, m=m, t0=t0, w=w: e.matmul(
                        pst[0:m, 0:w], wt[:, kc, oc * 128:oc * 128 + m], h[:, kc, t0:t0 + w],
                        start=(kc == 0), stop=(kc == 15)), reads=[hr, wr], writes=[psr])
                if ev % 2 == 0:
                    fw.op("dve", lambda e, st=st, pst=pst, m=m, t0=t0, w=w: e.tensor_copy(st[0:m, t0:t0 + w], pst[0:m, 0:w]),
                          reads=[psr], writes=[sr])
                else:
                    fw.op("act", lambda e, st=st, pst=pst, m=m, t0=t0, w=w: e.copy(st[0:m, t0:t0 + w], pst[0:m, 0:w]),
                          reads=[psr], writes=[sr])
                ev += 1
            r0 = c0 + oc * 128
            fw.dma("sp", pT.ap()[r0:r0 + m, :], st[0:m, :], reads=[sr], writes=[pTr])
    P.phase_end()


WEIGHT_SHAPES = {
    "w_mod": (DEPTH, D, 6 * D), "w_in": (DEPTH, D, INW), "w_uq": (DEPTH, 512, 768), "w_ukv": (DEPTH, 256, 1024),
    "w_branch": (DEPTH, 4, 512, D), "w_gate": (DEPTH, 4, D, D), "w_o": (DEPTH, D, D),
    "peer_w_query": (DEPTH, D, D), "peer_sub_keys": (DEPTH, 8, 2, 128, 128),
    "peer_u": (DEPTH, 16384, D), "peer_v": (DEPTH, 16384, D),
}
CONST_SHAPES = {"k_ident": (128, 128), "k_ones": (128, 128), "k_bd64": (128, 128), "k_rot64": (128, 128),
                "k_rot128": (128, 128), "k_cos64": (128, T), "k_sin64": (128, T), "k_cos128": (128, T),
                "k_sin128": (128, T), "k_rett": (128, 6 * 128 + 2)}


def build_program(phases=None, ext_in=(), ext_out=(), same_engine_sync=True, weights=None, do_mod=True):
    P = Prog(ext_in, ext_out, same_engine_sync)
    P.inp("x_b", (T, D))
    P.inp("ctx_b", (L, D))
    P.inp("cvec", (128, 16, 2))
    P.inp("smalls", (DEPTH, 128, NSM))
    for k, shp in CONST_SHAPES.items():
        P.inp(k, shp)
    for k, shp in WEIGHT_SHAPES.items():
        if weights is None or k in weights:
            P.inp(k, shp)
    P.inp("nab", (DEPTH, 128, 8 * 16 * 64))
    P.scratch("xT", (D, NT), F32)
    P.scratch("hT", (D, NT), BF16)
    P.scratch("pT", (INW, NT), F32)
    P.scratch("pV", (3, NT, 512), BF16)
    P.scratch("yT", (4, 512, NT), BF16)
    P.scratch("qT", (D, NT), BF16)
    P.scratch("GdT", (16384, NT), BF16)
    P.scratch("GAT", (16384, NT), BF16)
    P.out("y", (T, D))
    run = (lambda n: True) if phases is None else (lambda n: n in phases)
    ph_setup(P, do_mod)
    if run("tin"):
        ph_transpose_in(P)
    for l in range(DEPTH):
        if run("norm1_%d" % l):
            ph_norm(P, l, 0, "hT")
        if run("inproj_%d" % l):
            ph_inproj(P, l)
        if run("diff_%d" % l):
            ph_mix_diff(P, l, l < DEPTH - 1)
        if run("mla_%d" % l):
            ph_mix_mla(P, l, l < DEPTH - 1)
        if run("ret_%d" % l):
            ph_mix_ret(P, l, l < DEPTH - 1)
        if run("na_%d" % l):
            ph_mix_na(P, l, l < DEPTH - 1)
        if run("merge_%d" % l):
            ph_merge(P, l, l < DEPTH - 1)
        if run("norm2_%d" % l):
            ph_norm(P, l, 1, "hT")
        if run("peerq_%d" % l):
            ph_peer_q(P, l)
        if run("peerg_%d" % l):
            ph_peer_gate(P, l, l < DEPTH - 1)
        if run("peeru_%d" % l):
            ph_peer_u(P, l, l < DEPTH - 1)
        if run("peerv_%d" % l):
            ph_peer_v(P, l, l < DEPTH - 1)
    if run("tout"):
        ph_transpose_out(P)
    P.fw.barrier()
    P.es.close()
    return P


def host_inputs(inp, b, consts, smalls):
    m = {"x_b": np.ascontiguousarray(inp["x"][b]), "ctx_b": np.ascontiguousarray(inp["ctx"][b])}
    cv = np.stack([_cols(inp["c"][b]), _cols(inp["c_ctx"])], axis=2)
    m["cvec"] = np.ascontiguousarray(cv.astype(np.float32))
    m["smalls"] = smalls
    for k, v in consts.items():
        m["k_" + k] = v
    for k in WEIGHT_SHAPES:
        m[k] = inp[k]
    return m


def load_tables(P, names):
    out = {}
    for nm in names:
        t, r = P.sb("tb_" + nm, [128, T], F32)
        P.fw.dma("sp", t[:], P.dr["k_" + nm][0].ap(), writes=[r])
        out[nm] = (t, r)
    return out


class Scr:
    def __init__(self, P, name, n, dt=F32, w=512):
        self.t = [P.sb("%s%d" % (name, i), [128, w], dt) for i in range(n)]
        self.i = 0

    def get(self):
        x = self.t[self.i % len(self.t)]
        self.i += 1
        return x


def interleave(gens):
    gens = list(gens)
    while gens:
        for g in list(gens):
            try:
                next(g)
            except StopIteration:
                gens.remove(g)


def run(gen):
    for _ in gen:
        pass


def rms_rows_g(P, S, srcs, w, bdmat, gsize, dst_r, out_rs):
    fw = P.fw
    pst, psr = P.bank()
    bd, bdr = bdmat
    for i, (ap, r, k) in enumerate(srcs):
        sq, sqr = S.get()
        fw.op("act", lambda e, sq=sq, ap=ap, k=k: e.activation(out=sq[0:k, 0:w], in_=ap, func=AF.Square), reads=[r], writes=[sqr])
        yield
        fw.op("pe", lambda e, sq=sq, k=k, i=i: e.matmul(pst[:, 0:w], bd[0:k, :], sq[0:k, 0:w], start=(i == 0), stop=(i == len(srcs) - 1)),
              reads=[sqr, bdr], writes=[psr])
        yield
    fw.op("act", lambda e: e.activation(out=out_rs[:, 0:w], in_=pst[:, 0:w], func=AF.Sqrt, bias=EPS, scale=1.0 / gsize),
          reads=[psr], writes=[dst_r])
    yield
    fw.op("dve", lambda e: e.reciprocal(out_rs[:, 0:w], out_rs[:, 0:w]), reads=[dst_r], writes=[dst_r])
    yield


def rms_rows(P, S, srcs, w, bdmat, gsize, dst_r, out_rs):
    run(rms_rows_g(P, S, srcs, w, bdmat, gsize, dst_r, out_rs))


def rope_apply_g(P, S, xn, xnr, k, t0, w, rotm, cos, sin, dst, dstr):
    fw = P.fw
    rm, rmr = rotm
    (ct, cr), (st, sr) = cos, sin
    p0 = t0 - L
    pst, psr = P.bank()
    fw.op("pe", lambda e: e.matmul(pst[0:k, 0:w], rm[0:k, 0:k], xn[0:k, 0:w], start=True, stop=True), reads=[xnr, rmr], writes=[psr])
    yield
    t1, t1r = S.get()
    fw.op("pool", lambda e: e.tensor_tensor(t1[0:k, 0:w], xn[0:k, 0:w], ct[0:k, p0:p0 + w], ALU.mult), reads=[xnr, cr], writes=[t1r])
    yield
    t2, t2r = S.get()
    fw.op("dve", lambda e: e.tensor_tensor(t2[0:k, 0:w], pst[0:k, 0:w], st[0:k, p0:p0 + w], ALU.mult), reads=[psr, sr], writes=[t2r])
    yield
    fw.op("dve", lambda e: e.tensor_tensor(dst[0:k, t0:t0 + w], t1[0:k, 0:w], t2[0:k, 0:w], ALU.add), reads=[t1r, t2r], writes=[dstr])
    yield


def rope_apply(P, S, xn, xnr, k, t0, w, rotm, cos, sin, dst, dstr):
    run(rope_apply_g(P, S, xn, xnr, k, t0, w, rotm, cos, sin, dst, dstr))


def prep_qk_g(P, S, src_ap, srcr, dst, dstr, gain, bdmat, gsize, rope=None, scale=None):
    fw = P.fw
    sm, smr = P.C["sm"]
    for (t0, w, j) in TG:
        x, xr = S.get()
        fw.dma("sp", x[:, 0:w], src_ap[:, t0:t0 + w], reads=[srcr], writes=[xr])
        yield
        do_rope = rope is not None and j == 0
        if bdmat is not None:
            rs, rsr = S.get()
            yield from rms_rows_g(P, S, [(x[:, 0:w], xr, 128)], w, bdmat, gsize, rsr, rs)
            if do_rope:
                xn, xnr = S.get()
                fw.op("dve", lambda e, xn=xn, x=x, rs=rs: e.scalar_tensor_tensor(xn[:, 0:w], x[:, 0:w], gain, rs[:, 0:w], ALU.mult, ALU.mult),
                      reads=[xr, rsr, smr], writes=[xnr])
            else:
                fw.op("dve", lambda e, x=x, rs=rs: e.scalar_tensor_tensor(dst[:, t0:t0 + w], x[:, 0:w], gain, rs[:, 0:w], ALU.mult, ALU.mult),
                      reads=[xr, rsr, smr], writes=[dstr])
            yield
        else:
            if do_rope:
                if scale is not None:
                    xn, xnr = S.get()
                    fw.op("act", lambda e, xn=xn, x=x: e.mul(xn[:, 0:w], x[:, 0:w], scale), reads=[xr], writes=[xnr])
                else:
                    xn, xnr = x, xr
            else:
                fw.op("act", lambda e, x=x: e.mul(dst[:, t0:t0 + w], x[:, 0:w], 1.0 if scale is None else scale), reads=[xr], writes=[dstr])
            yield
        if do_rope:
            yield from rope_apply_g(P, S, xn, xnr, 128, t0, w, rope[0], rope[1], rope[2], dst, dstr)


def prep_qk(P, S, *a, **kw):
    run(prep_qk_g(P, S, *a, **kw))


def attn_core(P, tag, heads, npass, parts_fn, V, Vr, scale, ctx_out, finish_fn, Ebufs):
    fw = P.fw
    onesb, onesbr = P.C["ones_bf"]
    groups = [g for g in TG if (g[2] == 0 or ctx_out)]
    heads = list(heads)

    def lane_gen(lane, lheads):
        osb = [[P.sb("%s_o%d_%d_%d" % (tag, lane, p, i), [128, 512], F32) for i in range(2)] for p in range(npass)]
        rz = [P.sb("%s_rz%d_%d" % (tag, lane, i), [128, 512], F32) for i in range(2)]
        Eb = [P.sb("%s_E%d_%d" % (tag, lane, i), [128, 512], BF16) for i in range(3)]
        ei = 0
        gi = 0
        for h in lheads:
            for (t0, w, j) in groups:
                nkc = 2 if j == 1 else NTB
                outs = []
                for p in range(npass):
                    Ob, Obr = P.bank(4 + 2 * lane)
                    Zb, Zbr = P.bank(5 + 2 * lane)
                    parts = parts_fn(h, p)

                    def _pv(kc, E, Er):
                        fw.op("pe", lambda e, E=E, kc=kc: e.matmul(Ob[:, 0:w], V[:, kc, h * 128:(h + 1) * 128], E[:, 0:w],
                                                                 start=(kc == 0), stop=(kc == nkc - 1)), reads=[Er, Vr], writes=[Obr])
                        yield
                        fw.op("pe", lambda e, E=E, kc=kc: e.matmul(Zb[:, 0:w], onesb[:], E[:, 0:w],
                                                                 start=(kc == 0), stop=(kc == nkc - 1)), reads=[Er, onesbr], writes=[Zbr])
                        yield

                    pend = []
                    for kc in range(nkc):
                        Sb, Sbr = P.bank(2 * lane + ei % 2)
                        for i, (kf, qf, kr_, qr_) in enumerate(parts):
                            fw.op("pe", lambda e, Sb=Sb, kf=kf, qf=qf, kc=kc, i=i: e.matmul(
                                Sb[:, 0:w], kf(kc * 128, (kc + 1) * 128), qf(t0, t0 + w), start=(i == 0), stop=(i == len(parts) - 1)),
                                reads=[kr_, qr_], writes=[Sbr])
                        yield
                        E, Er = Eb[ei % 3]
                        ei += 1
                        fw.op("act", lambda e, E=E, Sb=Sb: e.activation(out=E[:, 0:w], in_=Sb[:, 0:w], func=AF.Exp, scale=scale),
                              reads=[Sbr], writes=[Er], inc=True)
                        yield
                        pend.append((kc, E, Er))
                        if len(pend) > 2:
                            yield from _pv(*pend.pop(0))
                    while pend:
                        yield from _pv(*pend.pop(0))
                    rzt, rzr = rz[p % 2]
                    fw.op("dve", lambda e, rzt=rzt, Zb=Zb: e.reciprocal(rzt[:, 0:w], Zb[:, 0:w]), reads=[Zbr], writes=[rzr])
                    ot, otr = osb[p][gi % 2]
                    fw.op("dve", lambda e, ot=ot, Ob=Ob, rzt=rzt: e.tensor_tensor(ot[:, 0:w], Ob[:, 0:w], rzt[:, 0:w], ALU.mult),
                          reads=[Obr, rzr], writes=[otr])
                    yield
                    outs.append((ot, otr))
                gi += 1
                finish_fn(h, t0, w, outs)
                yield

    nh = len(heads)
    interleave([lane_gen(0, heads[:nh // 2]), lane_gen(1, heads[nh // 2:])])


def ph_mix_diff(P, l, ctx_out):
    fw = P.fw
    P.phase_begin()
    sm, smr = P.C["sm"]
    pT, pTr = P.dr["pT"]
    pV, pVr = P.dr["pV"]
    yT, yTr = P.dr["yT"]
    tabs = load_tables(P, ["cos64", "sin64"])
    S = Scr(P, "dsc", 16)
    q, qr = P.sb("dq", [128, 4, NT], BF16)
    k, kr = P.sb("dk", [128, 4, NT], BF16)
    V, Vr = P.sb("dv", [128, NTB, 512], BF16)
    fw.dma("sp", V[:], pV.ap()[0].rearrange("(tb p) n -> p tb n", p=128), reads=[pVr], writes=[Vr])
    rope = (P.C["rot64"], tabs["cos64"], tabs["sin64"])
    for h in range(4):
        interleave([
            prep_qk_g(P, S, pT.ap()[h * 128:(h + 1) * 128, :], pTr, q[:, h, :], qr, sm[:, l, SM["dqg"][0]:SM["dqg"][0] + 1], P.C["bd64"], 64, rope),
            prep_qk_g(P, S, pT.ap()[512 + h * 128:512 + (h + 1) * 128, :], pTr, k[:, h, :], kr, sm[:, l, SM["dkg"][0]:SM["dkg"][0] + 1], P.C["bd64"], 64, rope)])
    lam_init = 0.8 - 0.6 * math.exp(-0.3 * l)
    o = SM["dlam"][0]
    lt, ltr = P.sb("dlamt", [128, 128], F32)
    lc, lcr = P.sb("dlamc", [128, 4], F32)
    fw.op("dve", lambda e: e.tensor_tensor(lt[:, 0:64], sm[:, l, o:o + 64], sm[:, l, o + 64:o + 128], ALU.mult), reads=[smr], writes=[ltr])
    fw.op("dve", lambda e: e.tensor_tensor(lt[:, 64:128], sm[:, l, o + 128:o + 192], sm[:, l, o + 192:o + 256], ALU.mult), reads=[smr], writes=[ltr])
    fw.op("dve", lambda e: e.tensor_reduce(lc[:, 0:2], lt[:].rearrange("p (a b) -> p a b", a=2), AX.X, ALU.add), reads=[ltr], writes=[lcr])
    fw.op("act", lambda e: e.activation(out=lc[:, 0:2], in_=lc[:, 0:2], func=AF.Exp), reads=[lcr], writes=[lcr])
    fw.op("dve", lambda e: e.scalar_tensor_tensor(lc[:, 2:3], lc[:, 1:2], -lam_init, lc[:, 0:1], ALU.add, ALU.subtract), reads=[lcr], writes=[lcr])
    fw.op("dve", lambda e: e.tensor_scalar(lc[:, 3:4], sm[:, l, SM["dsub"][0]:SM["dsub"][0] + 1], 1.0 - lam_init, None, ALU.mult), reads=[smr], writes=[lcr])
    Ebufs = None
    ystg = [P.sb("dy%d" % i, [128, 512], BF16) for i in range(2)]
    cnt = [0]

    def parts_fn(h, p):
        b = 64 * p
        return [(lambda c0, c1: k[b:b + 64, h, c0:c1], lambda c0, c1: q[b:b + 64, h, c0:c1], kr, qr)]

    def finish(h, t0, w, outs):
        (o1, o1r), (o2, o2r) = outs
        y, yr_ = S.get()
        fw.op("dve", lambda e: e.scalar_tensor_tensor(y[:, 0:w], o2[:, 0:w], lc[:, 2:3], o1[:, 0:w], ALU.mult, ALU.add),
              reads=[o1r, o2r, lcr], writes=[yr_])
        rs, rsr = S.get()
        rms_rows(P, S, [(y[:, 0:w], yr_, 128)], w, P.C["ones"], 128, rsr, rs)
        yo, yor = ystg[cnt[0] % 2]
        cnt[0] += 1
        fw.op("dve", lambda e: e.scalar_tensor_tensor(yo[:, 0:w], y[:, 0:w], lc[:, 3:4], rs[:, 0:w], ALU.mult, ALU.mult),
              reads=[yr_, rsr, lcr], writes=[yor])
        fw.dma("sp", yT.ap()[0, h * 128:(h + 1) * 128, t0:t0 + w], yo[:, 0:w], reads=[yor], writes=[yTr])

    attn_core(P, "da", range(4), 2, parts_fn, V, Vr, 64 ** -0.5, ctx_out, finish, Ebufs)
    P.phase_end()


def ph_mix_mla(P, l, ctx_out):
    fw = P.fw
    P.phase_begin()
    sm, smr = P.C["sm"]
    pT, pTr = P.dr["pT"]
    yT, yTr = P.dr["yT"]
    ones, onesr = P.C["ones"]
    tabs = load_tables(P, ["cos64", "sin64"])
    rope = (P.C["rot64"], tabs["cos64"], tabs["sin64"])
    S = Scr(P, "msc", 10)
    wuq, wuqr = P.sb("m_wuq", [128, 4, 768], BF16)
    fw.dma("pool", wuq[:], P.dr["w_uq"][0].ap()[l].rearrange("(kc p) n -> p kc n", p=128), writes=[wuqr])
    wukv, wukvr = P.sb("m_wukv", [128, 2, 1024], BF16)
    fw.dma("pool", wukv[:], P.dr["w_ukv"][0].ap()[l].rearrange("(kc p) n -> p kc n", p=128), writes=[wukvr])
    cqn, cqnr = P.sb("m_cqn", [128, 4, NT], BF16)
    ckvn, ckvnr = P.sb("m_ckvn", [128, 2, NT], BF16)
    krt, krtr = P.sb("m_kr", [64, NT], F32)
    qn_, qnr = P.sb("m_qn", [128, 4, NT], BF16)
    qr_, qrr = P.sb("m_qr", [64, 4, NT], BF16)
    kn_, knr = P.sb("m_kn", [128, 4, NT], BF16)
    kro, kror = P.sb("m_kro", [64, 4, NT], BF16)
    V, Vr = P.sb("m_v", [128, NTB, 512], BF16)
    fw.dma("sp", krt[:], pT.ap()[5888:5952, :], reads=[pTr], writes=[krtr])
    for (nm, r0, nch, dst, dstr, gname) in (("cq", 5120, 4, cqn, cqnr, "mqn"), ("ckv", 5632, 2, ckvn, ckvnr, "mkvn")):
        go = SM[gname][0]
        for (t0, w, j) in TG:
            xs = []
            for c in range(nch):
                x, xr = S.get()
                fw.dma("sp", x[:, 0:w], pT.ap()[r0 + c * 128:r0 + (c + 1) * 128, t0:t0 + w], reads=[pTr], writes=[xr])
                xs.append((x, xr))
            rs, rsr = S.get()
            rms_rows(P, S, [(x[:, 0:w], xr, 128) for (x, xr) in xs], w, P.C["ones"], nch * 128, rsr, rs)
            for c, (x, xr) in enumerate(xs):
                fw.op("dve", lambda e, x=x, c=c, rs=rs: e.scalar_tensor_tensor(dst[:, c, t0:t0 + w], x[:, 0:w], sm[:, l, go + c:go + c + 1], rs[:, 0:w], ALU.mult, ALU.mult),
                      reads=[xr, rsr, smr], writes=[dstr])
    ev = 0
    for tb in range(NTB):
        pst, psr = P.bank()
        for kc in range(2):
            fw.op("pe", lambda e, pst=pst, kc=kc, tb=tb: e.matmul(
                pst[:].rearrange("p (h d) -> p h d", h=4), ckvn[:, kc, tb * 128:(tb + 1) * 128],
                wukv[:, kc, :].rearrange("p (h x) -> p h x", h=4)[:, :, 128:256], start=(kc == 0), stop=(kc == 1)),
                reads=[ckvnr, wukvr], writes=[psr])
        fw.op("dve" if tb % 2 == 0 else "act",
              (lambda e, pst=pst, tb=tb: e.tensor_copy(V[:, tb, :], pst[:])) if tb % 2 == 0 else (lambda e, pst=pst, tb=tb: e.copy(V[:, tb, :], pst[:])),
              reads=[psr], writes=[Vr])
    gq_n = sm[:, l, SM["mq_nope"][0]:SM["mq_nope"][0] + 1]
    gq_r = sm[:, l, SM["mq_rope"][0]:SM["mq_rope"][0] + 1]
    gk_n = sm[:, l, SM["mk_nope"][0]:SM["mk_nope"][0] + 1]
    gk_r = sm[:, l, SM["mk_rope"][0]:SM["mk_rope"][0] + 1]
    for h in range(4):
        for (t0, w, j) in TG:
            pn, pnr = P.bank()
            pr, prr = P.bank()
            for kc in range(4):
                fw.op("pe", lambda e, kc=kc: e.matmul(pn[:, 0:w], wuq[:, kc, h * 192:h * 192 + 128], cqn[:, kc, t0:t0 + w], start=(kc == 0), stop=(kc == 3)),
                      reads=[wuqr, cqnr], writes=[pnr])
            for kc in range(4):
                fw.op("pe", lambda e, kc=kc: e.matmul(pr[0:64, 0:w], wuq[:, kc, h * 192 + 128:h * 192 + 192], cqn[:, kc, t0:t0 + w], start=(kc == 0), stop=(kc == 3)),
                      reads=[wuqr, cqnr], writes=[prr])
            xn, xnr = S.get()
            xr_, xrr = S.get()
            fw.op("act", lambda e, xn=xn: e.copy(xn[:, 0:w], pn[:, 0:w]), reads=[pnr], writes=[xnr])
            fw.op("dve", lambda e, xr_=xr_: e.tensor_copy(xr_[0:64, 0:w], pr[0:64, 0:w]), reads=[prr], writes=[xrr])
            rs, rsr = S.get()
            rms_rows(P, S, [(xn[:, 0:w], xnr, 128), (xr_[0:64, 0:w], xrr, 64)], w, P.C["ones"], 192, rsr, rs)
            fw.op("dve", lambda e, xn=xn, rs=rs: e.scalar_tensor_tensor(qn_[:, h, t0:t0 + w], xn[:, 0:w], gq_n, rs[:, 0:w], ALU.mult, ALU.mult),
                  reads=[xnr, rsr, smr], writes=[qnr])
            if j == 0:
                xq, xqr = S.get()
                fw.op("dve", lambda e, xq=xq, xr_=xr_, rs=rs: e.scalar_tensor_tensor(xq[0:64, 0:w], xr_[0:64, 0:w], gq_r[0:64], rs[0:64, 0:w], ALU.mult, ALU.mult),
                      reads=[xrr, rsr, smr], writes=[xqr])
                rope_apply(P, S, xq, xqr, 64, t0, w, rope[0], rope[1], rope[2], qr_[:, h, :], qrr)
            else:
                fw.op("dve", lambda e, xr_=xr_, rs=rs: e.scalar_tensor_tensor(qr_[:, h, t0:t0 + w], xr_[0:64, 0:w], gq_r[0:64], rs[0:64, 0:w], ALU.mult, ALU.mult),
                      reads=[xrr, rsr, smr], writes=[qrr])
            pk, pkr = P.bank()
            for kc in range(2):
                fw.op("pe", lambda e, kc=kc: e.matmul(pk[:, 0:w], wukv[:, kc, h * 256:h * 256 + 128], ckvn[:, kc, t0:t0 + w], start=(kc == 0), stop=(kc == 1)),
                      reads=[wukvr, ckvnr], writes=[pkr])
            xk, xkr = S.get()
            fw.op("act", lambda e, xk=xk: e.copy(xk[:, 0:w], pk[:, 0:w]), reads=[pkr], writes=[xkr])
            rs2, rs2r = S.get()
            rms_rows(P, S, [(xk[:, 0:w], xkr, 128), (krt[0:64, t0:t0 + w], krtr, 64)], w, P.C["ones"], 192, rs2r, rs2)
            fw.op("dve", lambda e, xk=xk, rs2=rs2: e.scalar_tensor_tensor(kn_[:, h, t0:t0 + w], xk[:, 0:w], gk_n, rs2[:, 0:w], ALU.mult, ALU.mult),
                  reads=[xkr, rs2r, smr], writes=[knr])
            if j == 0:
                xq2, xq2r = S.get()
                fw.op("dve", lambda e, xq2=xq2, rs2=rs2: e.scalar_tensor_tensor(xq2[0:64, 0:w], krt[0:64, t0:t0 + w], gk_r[0:64], rs2[0:64, 0:w], ALU.mult, ALU.mult),
                      reads=[krtr, rs2r, smr], writes=[xq2r])
                rope_apply(P, S, xq2, xq2r, 64, t0, w, rope[0], rope[1], rope[2], kro[:, h, :], kror)
            else:
                fw.op("dve", lambda e, rs2=rs2: e.scalar_tensor_tensor(kro[:, h, t0:t0 + w], krt[0:64, t0:t0 + w], gk_r[0:64], rs2[0:64, 0:w], ALU.mult, ALU.mult),
                      reads=[krtr, rs2r, smr], writes=[kror])
    Ebufs = None
    ystg = [P.sb("my%d" % i, [128, 512], BF16) for i in range(2)]
    cnt = [0]

    def parts_fn(h, p):
        return [(lambda c0, c1: kn_[:, h, c0:c1], lambda c0, c1: qn_[:, h, c0:c1], knr, qnr),
                (lambda c0, c1: kro[:, h, c0:c1], lambda c0, c1: qr_[:, h, c0:c1], kror, qrr)]

    def finish(h, t0, w, outs):
        (o1, o1r), = outs
        yo, yor = ystg[cnt[0] % 2]
        cnt[0] += 1
        fw.op("act", lambda e: e.copy(yo[:, 0:w], o1[:, 0:w]), reads=[o1r], writes=[yor])
        fw.dma("sp", yT.ap()[3, h * 128:(h + 1) * 128, t0:t0 + w], yo[:, 0:w], reads=[yor], writes=[yTr])

    attn_core(P, "ma", range(4), 1, parts_fn, V, Vr, 192 ** -0.5, ctx_out, finish, Ebufs)
    P.phase_end()


def ph_mix_ret(P, l, ctx_out):
    fw = P.fw
    P.phase_begin()
    sm, smr = P.C["sm"]
    pT, pTr = P.dr["pT"]
    pV, pVr = P.dr["pV"]
    yT, yTr = P.dr["yT"]
    identb, identbr = P.C["ident_bf"]
    tabs = load_tables(P, ["cos128", "sin128"])
    rope = (P.C["rot128"], tabs["cos128"], tabs["sin128"])
    S = Scr(P, "rsc", 16)
    q, qr = P.sb("r_q", [128, 4, NT], BF16)
    k, kr = P.sb("r_k", [128, 4, NT], BF16)
    V, Vr = P.sb("r_v", [128, NTB, 512], BF16)
    fw.dma("sp", V[:], pV.ap()[1].rearrange("(tb p) n -> p tb n", p=128), reads=[pVr], writes=[Vr])
    for h in range(4):
        interleave([
            prep_qk_g(P, S, pT.ap()[1536 + h * 128:1536 + (h + 1) * 128, :], pTr, q[:, h, :], qr, None, None, None, rope, None),
            prep_qk_g(P, S, pT.ap()[2048 + h * 128:2048 + (h + 1) * 128, :], pTr, k[:, h, :], kr, None, None, None, rope, 128 ** -0.5)])
    rt, rtr = P.sb("r_rt", [128, 6 * 128 + 2], F32)
    fw.dma("sp", rt[:], P.dr["k_rett"][0].ap(), writes=[rtr])
    lg, lgr = P.sb("r_lg", [128, 8], F32)
    o = SM["rdecay"][0]
    fw.op("act", lambda e: e.activation(out=lg[:], in_=sm[:, l, o:o + 8], func=AF.Exp, scale=-1.0), reads=[smr], writes=[lgr])
    fw.op("act", lambda e: e.activation(out=lg[:], in_=lg[:], func=AF.Ln, bias=1.0), reads=[lgr], writes=[lgr])
    fw.op("act", lambda e: e.mul(lg[:], lg[:], -1.0), reads=[lgr], writes=[lgr])
    DmT, DmTr = P.sb("r_dm", [128, 8, 128], F32)
    XI, XIr = P.sb("r_xi", [128, 8, 128], F32)
    ZG, ZGr = P.sb("r_zg", [128, 8, 2], F32)
    c128, c128r = P.sb("r_c128", [128, 1], F32)
    fw.op("dve", lambda e: e.memset(c128[:], 128.0), writes=[c128r])
    for d in range(2):
        for h in range(4):
            i = d * 4 + h
            col = lg[:, i:i + 1]
            fw.op("act", lambda e, i=i, d=d, col=col: e.activation(out=DmT[:, i, :], in_=rt[:, d * 256:d * 256 + 128], func=AF.Exp, scale=col),
                  reads=[rtr, lgr], writes=[DmTr])
            fw.op("dve", lambda e, i=i, d=d: e.tensor_tensor(DmT[:, i, :], DmT[:, i, :], rt[:, d * 256 + 128:d * 256 + 256], ALU.mult),
                  reads=[rtr, DmTr], writes=[DmTr])
            fw.op("act", lambda e, i=i, d=d, col=col: e.activation(out=XI[:, i, :], in_=rt[:, 512 + d * 128:512 + (d + 1) * 128], func=AF.Exp, scale=col),
                  reads=[rtr, lgr], writes=[XIr])
            fw.op("act", lambda e, i=i, d=d, col=col: e.activation(out=ZG[:, i, 0:1], in_=rt[:, 768 + d:768 + d + 1], func=AF.Exp, scale=col),
                  reads=[rtr, lgr], writes=[ZGr])
            fw.op("act", lambda e, i=i, col=col: e.activation(out=ZG[:, i, 1:2], in_=c128[:], func=AF.Exp, scale=col),
                  reads=[c128r, lgr], writes=[ZGr])
    yacc, yaccr = P.sb("r_yacc", [128, 4, NT], F32)
    yres = [[Res("yacc%d_%d" % (h, c)) for c in range(NTB)] for h in range(4)]
    for h in range(4):
        fw.op("pool", lambda e, h=h: e.memset(yacc[:, h, :], 0.0), writes=yres[h])
    Rf = [P.sb("r_R%d" % i, [128, 128], F32) for i in range(8)]
    Rb = [P.sb("r_Rb%d" % i, [128, 128], BF16) for i in range(8)]
    stm = [P.sb("r_stm%d" % i, [128, 128], BF16) for i in range(8)]
    qx = [P.sb("r_qx%d" % i, [128, 128], BF16) for i in range(8)]
    kz = [P.sb("r_kz%d" % i, [128, 128], BF16) for i in range(8)]
    order = {0: list(range(NTB)), 1: [1, 0] + list(range(NTB - 1, 1, -1))}
    def _step(si, d, h):
        i = d * 4 + h
        c = order[d][si]
        sl = slice(c * 128, (c + 1) * 128)
        need_out = (c >= 2) or ctx_out
        R_, R_r = Rf[i]
        Rb_, Rb_r = Rb[i]
        if need_out:
            st_, st_r = P.bank()
            fw.op("pe", lambda e: e.matmul(st_[:, 0:128], k[:, h, sl], q[:, h, sl], start=True, stop=True), reads=[kr, qr], writes=[st_r])
            yield
            sm_, sm_r = stm[i]
            fw.op("dve", lambda e: e.tensor_tensor(sm_[:], st_[:, 0:128], DmT[:, i, :], ALU.mult), reads=[st_r, DmTr], writes=[sm_r])
            yield
            ob, obr = P.bank()
            fw.op("pe", lambda e: e.matmul(ob[:, 0:128], V[:, c, h * 128:(h + 1) * 128], sm_[:], start=True, stop=(si == 0)), reads=[Vr, sm_r], writes=[obr])
            yield
            if si > 0:
                qx_, qx_r = qx[i]
                fw.op("pool", lambda e: e.tensor_tensor(qx_[:], q[:, h, sl], XI[:, i, :], ALU.mult), reads=[qr, XIr], writes=[qx_r])
                yield
                fw.op("pe", lambda e: e.matmul(ob[:, 0:128], Rb_[:], qx_[:], start=False, stop=True), reads=[Rb_r, qx_r], writes=[obr])
                yield
            yr_ = yres[h][c]
            fw.op("dve", lambda e: e.tensor_tensor(yacc[:, h, sl], yacc[:, h, sl], ob[:, 0:128], ALU.add), reads=[obr, yr_], writes=[yr_])
            yield
        if si < NTB - 1:
            kt, ktr = P.bank()
            fw.op("pe", lambda e: e.matmul(kt[:, 0:128], k[:, h, sl], identb[:], start=True, stop=True), reads=[kr, identbr], writes=[ktr])
            yield
            kz_, kz_r = kz[i]
            fw.op("act", lambda e: e.activation(out=kz_[:], in_=kt[:, 0:128], func=AF.Copy, scale=ZG[:, i, 0:1]), reads=[ktr, ZGr], writes=[kz_r])
            yield
            rn, rnr = P.bank()
            fw.op("pe", lambda e: e.matmul(rn[:, 0:128], kz_[:], V[:, c, h * 128:(h + 1) * 128], start=True, stop=True), reads=[kz_r, Vr], writes=[rnr])
            yield
            if si == 0:
                fw.op("dve", lambda e: e.tensor_copy(R_[:], rn[:, 0:128]), reads=[rnr], writes=[R_r])
            else:
                fw.op("dve", lambda e: e.scalar_tensor_tensor(R_[:], R_[:], ZG[:, i, 1:2], rn[:, 0:128], ALU.mult, ALU.add), reads=[rnr, R_r, ZGr], writes=[R_r])
            yield
            fw.op("act", lambda e: e.copy(Rb_[:], R_[:]), reads=[R_r], writes=[Rb_r])
            yield

    for si in range(NTB):
        for d in range(2):
            interleave([_step(si, d, h) for h in range(4)])
    allres = [yres[h][c] for h in range(4) for c in range(NTB)]
    ystg = [P.sb("r_y%d" % i, [128, 512], BF16) for i in range(2)]
    cnt = 0
    go = SM["rnorm"][0]
    for h in range(4):
        for (t0, w, j) in TG:
            if j == 1 and not ctx_out:
                continue
            g_, g_r = S.get()
            fw.dma("sp", g_[:, 0:w], pT.ap()[3072 + h * 128:3072 + (h + 1) * 128, t0:t0 + w], reads=[pTr], writes=[g_r])
            fw.op("act", lambda e, g_=g_: e.activation(out=g_[:, 0:w], in_=g_[:, 0:w], func=AF.Silu), reads=[g_r], writes=[g_r])
            rs, rsr = S.get()
            sq, sqr = S.get()
            pst, psr = P.bank()
            ones, onesr = P.C["ones"]
            fw.op("act", lambda e, sq=sq, h=h: e.activation(out=sq[:, 0:w], in_=yacc[:, h, t0:t0 + w], func=AF.Square), reads=allres, writes=[sqr])
            fw.op("pe", lambda e, sq=sq, pst=pst: e.matmul(pst[:, 0:w], ones[:], sq[:, 0:w], start=True, stop=True), reads=[sqr, onesr], writes=[psr])
            fw.op("act", lambda e, rs=rs, pst=pst: e.activation(out=rs[:, 0:w], in_=pst[:, 0:w], func=AF.Sqrt, bias=EPS, scale=1.0 / 128), reads=[psr], writes=[rsr])
            fw.op("dve", lambda e, rs=rs: e.reciprocal(rs[:, 0:w], rs[:, 0:w]), reads=[rsr], writes=[rsr])
            t1, t1r = S.get()
            fw.op("dve", lambda e, t1=t1, h=h, rs=rs: e.scalar_tensor_tensor(t1[:, 0:w], yacc[:, h, t0:t0 + w], sm[:, l, go:go + 1], rs[:, 0:w], ALU.mult, ALU.mult),
                  reads=allres + [rsr, smr], writes=[t1r])
            yo, yor = ystg[cnt % 2]
            cnt += 1
            fw.op("dve", lambda e, yo=yo, t1=t1, g_=g_: e.tensor_tensor(yo[:, 0:w], t1[:, 0:w], g_[:, 0:w], ALU.mult), reads=[t1r, g_r], writes=[yor])
            fw.dma("sp", yT.ap()[1, h * 128:(h + 1) * 128, t0:t0 + w], yo[:, 0:w], reads=[yor], writes=[yTr])
    P.phase_end()


def build_nab(rpb):
    kc = np.arange(64)
    qc = np.arange(64)
    cs = np.clip(qc - 8, 0, 48)
    valid = (kc[:, None] >= cs[None, :]) & (kc[:, None] < cs[None, :] + 16)
    dc = np.clip(kc[:, None] - qc[None, :] + 15, 0, 30)
    out = np.full((2, 64, 8, 16, 64), NEG, np.float32)
    for i2 in range(2):
        for dr in range(16):
            d2 = dr + i2
            if d2 > 14:
                continue
            g = rpb[:, d2, :][:, dc]
            g = np.where(valid[None], g, np.float32(NEG))
            out[i2, :, :, dr, :] = np.transpose(g, (1, 0, 2))
    return np.ascontiguousarray(out.reshape(128, 8 * 16 * 64))


def extra_host_inputs(inp):
    return {"nab": np.stack([build_nab(np.asarray(inp["na_rpb"][l], np.float32)) for l in range(DEPTH)])}


def ph_mix_na(P, l, ctx_out):
    fw = P.fw
    P.phase_begin()
    sm, smr = P.C["sm"]
    pT, pTr = P.dr["pT"]
    pV, pVr = P.dr["pV"]
    yT, yTr = P.dr["yT"]
    identb, identbr = P.C["ident_bf"]
    S = Scr(P, "nsc", 14)
    q, qr = P.sb("n_q", [128, 4, NT], BF16)
    k, kr = P.sb("n_k", [128, 4, NT], BF16)
    for c in range(4):
        interleave([
            prep_qk_g(P, S, pT.ap()[3584 + c * 128:3584 + (c + 1) * 128, :], pTr, q[:, c, :], qr, sm[:, l, SM["nqg"][0]:SM["nqg"][0] + 1], P.C["bd64"], 64),
            prep_qk_g(P, S, pT.ap()[4096 + c * 128:4096 + (c + 1) * 128, :], pTr, k[:, c, :], kr, sm[:, l, SM["nkg"][0]:SM["nkg"][0] + 1], P.C["bd64"], 64)])
    NA_STOP = 99
    Vx, Vxr = P.sb("n_vx", [128, NTB, 8, 128], BF16)
    pv = pV.ap()[2].rearrange("(tb p) (h d) -> p tb h d", p=128, h=8)
    for hh in range(8):
        fw.dma("sp", Vx[:, :, hh, 0:64], pv[:, :, hh, :], reads=[pVr], writes=[Vxr])
    fw.op("pool", lambda e: e.memset(Vx[:, :, :, 64:128], 1.0), writes=[Vxr])
    nab, nabr = P.sb("n_nab", [128, 8, 16, 64], F32)
    fw.dma("sp", nab[:].rearrange("p h r c -> p (h r c)"), P.dr["nab"][0].ap()[l], writes=[nabr])
    es = [P.sb("n_es%d" % i, [128, 8, 128], F32) for i in range(2)]
    Eb = [P.sb("n_E%d" % i, [128, 8, 128], BF16) for i in range(3)]
    ytm = [P.sb("n_ytm%d" % i, [128, 8, 64], BF16) for i in range(2)]
    rzt = [P.sb("n_rz%d" % i, [128, 8], F32) for i in range(2)]
    ystg = [P.sb("n_ys%d" % i, [128, 4, 128], BF16) for i in range(2)]
    blocks = []
    if ctx_out:
        blocks += [("ctx", 0), ("ctx", 1)]
    blocks += [("lat", pr) for pr in range(16)]
    ci = 0
    if NA_STOP <= 2:
        blocks = []
    elif 30 <= NA_STOP < 40 or NA_STOP == 3:
        blocks = blocks[:2]
    elif NA_STOP == 4:
        blocks = blocks[:3]
    for bi, (kind, idx) in enumerate(blocks):
        if kind == "ctx":
            qt0 = idx * 128
            chunks = [(0, None), (1, None)]
        else:
            r = 2 * idx
            qt0 = 256 + r * 64
            rs0 = min(max(r - 4, 0), 24)
            rs1 = min(max(r + 1 - 4, 0), 24)
            nloc = 4 if rs1 == rs0 else 5
            chunks = [(0, None), (1, None)]
            for j in range(nloc):
                info = []
                for a in range(2):
                    rsa = rs0 if a == 0 else rs1
                    dr0 = (rs0 + 2 * j) - (r + a) + 7
                    inval = [i2 for i2 in range(2) if not (rsa <= rs0 + 2 * j + i2 < rsa + 8)]
                    info.append((min(max(dr0, 0), 15), inval))
                chunks.append((2 + rs0 // 2 + j, info))
        Ob = [P.bank(4 + 2 * (bi % 2)), P.bank(5 + 2 * (bi % 2))]

        def _stage1(cj, kc, info):
            nonlocal ci
            Sb = [P.bank(2 * (ci % 2)), P.bank(2 * (ci % 2) + 1)]
            E, Er = Eb[ci % 3]
            e_, e_r = es[ci % 2]
            ci += 1
            for h in range(8):
                hb = 64 * (h % 2)
                sb_, sb_r = Sb[h % 2]
                fw.op("pe", lambda e, sb_=sb_, h=h, hb=hb, kc=kc: e.matmul(
                    sb_[:, (h // 2) * 128:(h // 2 + 1) * 128], k[hb:hb + 64, h // 2, kc * 128:(kc + 1) * 128],
                    q[hb:hb + 64, h // 2, qt0:qt0 + 128], start=True, stop=True), reads=[kr, qr], writes=[sb_r])
            for par in range(2):
                sb_, sb_r = Sb[par]
                hs = slice(par * 4, par * 4 + 4)
                if info is None:
                    fw.op("act", lambda e, sb_=sb_, par=par, E=E: e.activation(
                        out=E[:].rearrange("p h t -> p (h t)")[:, par * 512:(par + 1) * 512], in_=sb_[:], func=AF.Exp, scale=0.125),
                        reads=[sb_r], writes=[Er])
                else:
                    for a_ in range(2):
                        dr0, inval = info[a_]
                        fw.op("dve", lambda e, sb_=sb_, hs=hs, a_=a_, dr0=dr0, e_=e_, par=par: e.scalar_tensor_tensor(
                            e_[:, hs, a_ * 64:(a_ + 1) * 64], sb_[:].rearrange("p (h t) -> p h t", h=4)[:, :, a_ * 64:(a_ + 1) * 64], 0.125,
                            nab[:, par:8:2, dr0, :], ALU.mult, ALU.add), reads=[sb_r, nabr], writes=[e_r])
                    fw.op("act", lambda e, hs=hs, E=E, e_=e_: e.activation(out=E[:, hs, :], in_=e_[:, hs, :], func=AF.Exp),
                          reads=[e_r], writes=[Er])
            if info is not None:
                for a_ in range(2):
                    for i2 in info[a_][1]:
                        fw.op("pool", lambda e, E=E, a_=a_, i2=i2: e.memset(E[i2 * 64:(i2 + 1) * 64, :, a_ * 64:(a_ + 1) * 64], 0.0), writes=[Er])
            return (cj, kc, E, Er)

        def _stage2(cj, kc, E, Er):
            for h in range(8):
                ob, obr = Ob[h // 4]
                first = (cj == 0 and h % 4 == 0)
                last = (cj == len(chunks) - 1 and h % 4 == 3)
                fw.op("pe", lambda e, ob=ob, h=h, E=E, kc=kc, first=first, last=last: e.matmul(
                    ob[:, (h % 4) * 128:(h % 4 + 1) * 128], E[:, (h % 2) * 4 + h // 2, :], Vx[:, kc, h, :], start=first, stop=last, skip_group_check=True),
                    reads=[Er, Vxr], writes=[obr])

        pend = None
        for cj, (kc, info) in enumerate(chunks):
            cur = _stage1(cj, kc, info)
            if pend is not None:
                _stage2(*pend)
            pend = cur
        _stage2(*pend)
        rz_, rz_r = rzt[bi % 2]
        yt_, yt_r = ytm[bi % 2]
        if NA_STOP in (31, 32):
            continue
        for half in range(2):
            ob, obr = Ob[half]
            ov = ob[:].rearrange("p (h d) -> p h d", h=4)
            fw.op("dve", lambda e, ov=ov, half=half, rz_=rz_: e.reciprocal(rz_[:, half * 4:half * 4 + 4], ov[:, :, 64]), reads=[obr], writes=[rz_r])
            fw.op("dve", lambda e, ov=ov, half=half, rz_=rz_, yt_=yt_: e.tensor_tensor(
                yt_[:, half * 4:half * 4 + 4, :], ov[:, :, 0:64], rz_[:, half * 4:half * 4 + 4].unsqueeze(2).to_broadcast([128, 4, 64]), ALU.mult),
                reads=[obr, rz_r], writes=[yt_r])
        if NA_STOP == 33:
            continue
        tp, tpr = P.bank(2 * (ci % 2))
        ytf = yt_[:].rearrange("p h d -> p (h d)")
        for c in range(4):
            fw.op("pe", lambda e, c=c: e.matmul(tp[:, c * 128:(c + 1) * 128], ytf[:, c * 128:(c + 1) * 128], identb[:], start=True, stop=True),
                  reads=[yt_r, identbr], writes=[tpr])
        ys_, ys_r = ystg[bi % 2]
        fw.op("act", lambda e: e.copy(ys_[:].rearrange("p c t -> p (c t)"), tp[:]), reads=[tpr], writes=[ys_r])
        fw.dma("sp", yT.ap()[2].rearrange("(c p) t -> p c t", p=128)[:, :, qt0:qt0 + 128], ys_[:], reads=[ys_r], writes=[yTr])
    P.phase_end()


MERGE_SB = [[(0, 256, 1), (256, 512, 0)], [(768, 512, 0), (1280, 256, 0)], [(1536, 512, 0), (2048, 256, 0)]]


def ph_merge(P, l, ctx_out):
    fw = P.fw
    P.phase_begin()
    sm, smr = P.C["sm"]
    hT, hTr = P.dr["hT"]
    yT, yTr = P.dr["yT"]
    xT, xTr = P.dr["xT"]
    hv = hT.ap().rearrange("(fc p) t -> p fc t", p=128)
    yv = yT.ap().rearrange("n (c p) t -> p (n c) t", p=128)
    wg = P.dr["w_gate"][0].ap()
    wbr = P.dr["w_branch"][0].ap()
    wo = P.dr["w_o"][0].ap()
    h, hr = P.sb("g_h", [128, 16, 768], BF16)
    y, yr = P.sb("g_y", [128, 16, 768], BF16)
    m, mr = P.sb("g_m", [128, 16, 768], BF16)
    Wg = [P.sb("g_wg%d" % i, [128, 4, 16, 128], BF16) for i in range(2)]
    Wb = [P.sb("g_wb%d" % i, [128, 4, 4, 128], BF16) for i in range(2)]
    Wo = [P.sb("g_wo%d" % i, [128, 16, 128], BF16) for i in range(2)]
    gs = [P.sb("g_gs%d" % i, [128, 512], F32) for i in range(3)]
    tm = [P.sb("g_tm%d" % i, [128, 512], F32) for i in range(3)]
    mac = [P.sb("g_mac%d" % i, [128, 512], F32) for i in range(2)]
    xs = [P.sb("g_xs%d" % i, [128, 512], F32) for i in range(3)]
    bo = SM["b_gate"][0]
    wi = 0
    gi = 0
    xi = 0
    for sbi, subs in enumerate(MERGE_SB):
        s0 = subs[0][0]
        subs = [s_ for s_ in subs if (s_[2] == 0 or ctx_out)]
        for q4 in range(4):
            fw.dma("sp", h[:, q4 * 4:(q4 + 1) * 4, :], hv[:, q4 * 4:(q4 + 1) * 4, s0:s0 + 768], reads=[hTr], writes=[hr])
            fw.dma("sp", y[:, q4 * 4:(q4 + 1) * 4, :], yv[:, q4 * 4:(q4 + 1) * 4, s0:s0 + 768], reads=[yTr], writes=[yr])
        def _loadw(fc, slot):
            wgt, wgr = Wg[slot % 2]
            wbt, wbr_ = Wb[slot % 2]
            for n in range(4):
                fw.dma("pool", wgt[:, n], wg[l, n, :, fc * 128:(fc + 1) * 128].rearrange("(kc p) c -> p kc c", p=128), writes=[wgr])
            fw.dma("pool", wbt[:], wbr[l, :, :, fc * 128:(fc + 1) * 128].rearrange("n (kc p) c -> p n kc c", p=128), writes=[wbr_])

        def _loadwo(fo):
            wot, wor = Wo[fo % 2]
            fw.dma("pool", wot[:], wo[l, :, fo * 128:(fo + 1) * 128].rearrange("(kc p) c -> p kc c", p=128), writes=[wor])

        _loadw(0, wi)
        for fc in range(16):
            wgt, wgr = Wg[wi % 2]
            wbt, wbr_ = Wb[wi % 2]
            wi += 1
            if fc + 1 < 16:
                _loadw(fc + 1, wi)
            else:
                _loadwo(0)
            for (t0, w, j) in subs:
                c0 = t0 - s0
                ma, mar = mac[gi % 2]
                for n in range(4):
                    pg, pgr = P.bank()
                    for kc in range(16):
                        fw.op("pe", lambda e, pg=pg, n=n, kc=kc, wgt=wgt: e.matmul(pg[:, 0:w], wgt[:, n, kc, :], h[:, kc, c0:c0 + w], start=(kc == 0), stop=(kc == 15)),
                              reads=[wgr, hr], writes=[pgr])
                    g_, g_r = gs[gi % 3]
                    fw.op("act", lambda e, g_=g_, pg=pg, n=n: e.activation(out=g_[:, 0:w], in_=pg[:, 0:w], func=AF.Sigmoid, bias=sm[:, l, bo + n * 16 + fc:bo + n * 16 + fc + 1]),
                          reads=[pgr, smr], writes=[g_r])
                    pb, pbr = P.bank()
                    for kc in range(4):
                        fw.op("pe", lambda e, pb=pb, n=n, kc=kc, wbt=wbt: e.matmul(pb[:, 0:w], wbt[:, n, kc, :], y[:, n * 4 + kc, c0:c0 + w], start=(kc == 0), stop=(kc == 3)),
                              reads=[wbr_, yr], writes=[pbr])
                    if n == 0:
                        fw.op("dve", lambda e, ma=ma, g_=g_, pb=pb: e.tensor_tensor(ma[:, 0:w], g_[:, 0:w], pb[:, 0:w], ALU.mult), reads=[g_r, pbr], writes=[mar])
                    else:
                        t_, t_r = tm[gi % 3]
                        fw.op("dve", lambda e, t_=t_, g_=g_, pb=pb: e.tensor_tensor(t_[:, 0:w], g_[:, 0:w], pb[:, 0:w], ALU.mult), reads=[g_r, pbr], writes=[t_r])
                        if n < 3:
                            fw.op("dve", lambda e, ma=ma, t_=t_: e.tensor_tensor(ma[:, 0:w], ma[:, 0:w], t_[:, 0:w], ALU.add), reads=[t_r, mar], writes=[mar])
                        else:
                            fw.op("dve", lambda e, ma=ma, t_=t_: e.tensor_tensor(m[:, fc, c0:c0 + w], ma[:, 0:w], t_[:, 0:w], ALU.add), reads=[t_r, mar], writes=[mr])
                    gi += 1
        for fo in range(16):
            wot, wor = Wo[fo % 2]
            if fo + 1 < 16:
                _loadwo(fo + 1)
            for (t0, w, j) in subs:
                c0 = t0 - s0
                po, por = P.bank()
                for kc in range(16):
                    fw.op("pe", lambda e, po=po, kc=kc, wot=wot: e.matmul(po[:, 0:w], wot[:, kc, :], m[:, kc, c0:c0 + w], start=(kc == 0), stop=(kc == 15)),
                          reads=[wor, mr], writes=[por])
                x_, x_r = xs[xi % 3]
                xi += 1
                fw.dma("sp", x_[:, 0:w], xT.ap()[fo * 128:(fo + 1) * 128, t0:t0 + w], reads=[xTr], writes=[x_r])
                fw.op("dve", lambda e, x_=x_, po=po, fo=fo, j=j: e.scalar_tensor_tensor(x_[:, 0:w], po[:, 0:w], P.modv[:, l, 2, fo, j:j + 1], x_[:, 0:w], ALU.mult, ALU.add),
                      reads=[por, x_r, P.modr], writes=[x_r])
                fw.dma("sp", xT.ap()[fo * 128:(fo + 1) * 128, t0:t0 + w], x_[:, 0:w], reads=[x_r], writes=[xTr])
    P.phase_end()


def ph_transpose_out(P):
    fw = P.fw
    P.phase_begin()
    ident, identr = P.C["ident"]
    xT, xTr = P.dr["xT"]
    yo, yor = P.dr["y"]
    xTv = xT.ap().rearrange("(fc p) t -> p fc t", p=128)
    xin = [P.sb("to_x%d" % i, [128, 16, 128], F32) for i in range(2)]
    stg = [P.sb("to_s%d" % i, [128, 2048], F32) for i in range(2)]
    for tb in range(16):
        xt, xr = xin[tb % 2]
        fw.dma("sp", xt[:], xTv[:, :, L + tb * 128:L + (tb + 1) * 128], reads=[xTr], writes=[xr])
        st, sr = stg[tb % 2]
        for f4 in range(4):
            pst, psr = P.bank()
            for q in range(4):
                fc = f4 * 4 + q
                fw.op("pe", lambda e, pst=pst, q=q, xt=xt, fc=fc: e.transpose(pst[:, q * 128:(q + 1) * 128], xt[:, fc, :], ident[:]),
                      reads=[xr, identr], writes=[psr])
            if f4 % 2 == 0:
                fw.op("dve", lambda e, st=st, pst=pst, f4=f4: e.tensor_copy(st[:, f4 * 512:(f4 + 1) * 512], pst[:]), reads=[psr], writes=[sr])
            else:
                fw.op("act", lambda e, st=st, pst=pst, f4=f4: e.copy(st[:, f4 * 512:(f4 + 1) * 512], pst[:]), reads=[psr], writes=[sr])
        fw.dma("sp", yo.ap()[tb * 128:(tb + 1) * 128, :], st[:], reads=[sr], writes=[yor])
    P.phase_end()


def ph_peer_q(P, l):
    fw = P.fw
    P.phase_begin()
    hT, hTr = P.dr["hT"]
    qT, qTr = P.dr["qT"]
    h, hr = P.sb("pq_h", [128, 16, NT], BF16)
    hv = hT.ap().rearrange("(fc p) t -> p fc t", p=128)
    for q4 in range(4):
        fw.dma("sp", h[:, q4 * 4:(q4 + 1) * 4, :], hv[:, q4 * 4:(q4 + 1) * 4, :], reads=[hTr], writes=[hr])
    wbuf = [P.sb("pq_w%d" % i, [128, 16, 512], BF16) for i in range(2)]
    stg = [P.sb("pq_s%d" % i, [128, NT], BF16) for i in range(2)]
    wq = P.dr["peer_w_query"][0].ap()
    ev = 0
    for cg in range(4):
        wt, wr = wbuf[cg % 2]
        fw.dma("pool", wt[:], wq[l, :, cg * 512:(cg + 1) * 512].rearrange("(kc p) n -> p kc n", p=128), writes=[wr])
        for oc in range(4):
            st, sr = stg[oc % 2]
            for (t0, w, j) in TG:
                pst, psr = P.bank()
                for kc in range(16):
                    fw.op("pe", lambda e, pst=pst, kc=kc, wt=wt, oc=oc: e.matmul(pst[:, 0:w], wt[:, kc, oc * 128:(oc + 1) * 128], h[:, kc, t0:t0 + w],
                                                                               start=(kc == 0), stop=(kc == 15)), reads=[hr, wr], writes=[psr])
                if ev % 2 == 0:
                    fw.op("dve", lambda e, st=st, pst=pst: e.tensor_copy(st[:, t0:t0 + w], pst[:, 0:w]), reads=[psr], writes=[sr])
                else:
                    fw.op("act", lambda e, st=st, pst=pst: e.copy(st[:, t0:t0 + w], pst[:, 0:w]), reads=[psr], writes=[sr])
                ev += 1
            r0 = cg * 512 + oc * 128
            fw.dma("sp", qT.ap()[r0:r0 + 128, :], st[:], reads=[sr], writes=[qTr])
    P.phase_end()


def ph_peer_gate(P, l, ctx_out):
    fw = P.fw
    P.phase_begin()
    identb, identbr = P.C["ident_bf"]
    qT, qTr = P.dr["qT"]
    GdT, GdTr = P.dr["GdT"]
    qv = qT.ap().rearrange("(c p) t -> p c t", p=128)
    gv = GdT.ap().rearrange("(i j) t -> j i t", j=128)
    skn, sknr = P.sb("pg_skn", [128, 16, 128], BF16)
    fw.dma("pool", skn[:], P.dr["peer_sub_keys"][0].ap()[l].rearrange("h s k d -> k (h s) d"), writes=[sknr])
    SK, SKr = P.sb("pg_sk", [128, 16, 128], BF16)
    for b4 in range(4):
        pst, psr = P.bank()
        for q_ in range(4):
            c = b4 * 4 + q_
            fw.op("pe", lambda e, pst=pst, q_=q_, c=c: e.matmul(pst[:, q_ * 128:(q_ + 1) * 128], skn[:, c, :], identb[:], start=True, stop=True),
                  reads=[sknr, identbr], writes=[psr])
        fw.op("act", lambda e, pst=pst, b4=b4: e.copy(SK[:, b4 * 4:(b4 + 1) * 4, :].rearrange("p c k -> p (c k)"), pst[:]), reads=[psr], writes=[SKr])
    qts = [P.sb("pg_qt%d" % i, [128, 16, 128], BF16) for i in range(2)]
    ssbs = [P.sb("pg_s%d" % i, [128, 16, 128], F32) for i in range(2)]
    thrs = [P.sb("pg_thr%d" % i, [128, 8, 128], F32) for i in range(2)]
    lncs = [P.sb("pg_lnc%d" % i, [128, 32], F32) for i in range(2)]
    s2, s2r = P.sb("pg_s2", [128, 16, 128], F32)
    top, topr = P.sb("pg_top", [128, 16, 16], F32)
    cand, candr = P.sb("pg_cand", [128, 8, 256], F32)
    cand2, cand2r = P.sb("pg_cand2", [128, 8, 256], F32)
    c8, c8r = P.sb("pg_c8", [128, 8, 16], F32)
    selm, selmr = P.sb("pg_selm", [128, 8, 16], F32)
    zz, zzr = P.sb("pg_z", [128, 16], F32)
    Zt = [P.sb("pg_Z%d" % i, [128, 8, 128], F32) for i in range(6)]
    Mt = [P.sb("pg_M%d" % i, [128, 8, 128], BF16) for i in range(4)]
    Tt = [P.sb("pg_T%d" % i, [128, 8, 128], BF16) for i in range(5)]
    gst = [P.sb("pg_gst%d" % i, [128, 128, 128], BF16) for i in range(1)]
    tbs = list(range(NTB)) if ctx_out else list(range(2, NTB))
    if GATE_TBS is not None:
        tbs = GATE_TBS

    def prologue(ti):
        tb = tbs[ti]
        qt, qtr = qts[ti % 2]
        ssb, ssbr = ssbs[ti % 2]
        thr, thrr = thrs[ti % 2]
        lnc, lncr = lncs[ti % 2]
        fw.dma("sp", qt[:], qv[:, :, tb * 128:(tb + 1) * 128], reads=[qTr], writes=[qtr])
        yield
        for b4 in range(4):
            pst, psr = P.bank()
            for q_ in range(4):
                c = b4 * 4 + q_
                fw.op("pe", lambda e, pst=pst, q_=q_, c=c, qt=qt: e.matmul(pst[:, q_ * 128:(q_ + 1) * 128], qt[:, c, :], SK[:, c, :], start=True, stop=True),
                      reads=[qtr, SKr], writes=[psr])
            dst = ssb[:, b4 * 4:(b4 + 1) * 4, :].rearrange("p c k -> p (c k)")
            fw.op("dve", lambda e, pst=pst, dst=dst: e.tensor_copy(dst, pst[:]), reads=[psr], writes=[ssbr])
            yield
        for c in range(16):
            fw.op("dve", lambda e, c=c: e.max(out=top[:, c, 0:8], in_=ssb[:, c, :]), reads=[ssbr], writes=[topr])
            fw.op("dve", lambda e, c=c: e.match_replace(out=s2[:, c, :], in_to_replace=top[:, c, 0:8], in_values=ssb[:, c, :], imm_value=-1e30),
                  reads=[ssbr, topr], writes=[s2r])
            fw.op("dve", lambda e, c=c: e.max(out=top[:, c, 8:16], in_=s2[:, c, :]), reads=[s2r], writes=[topr])
            yield
        tv = top[:].rearrange("p (h s) a -> p h s a", s=2)
        fw.op("dve", lambda e: e.tensor_tensor(cand[:].rearrange("p h (a b) -> p h a b", a=16),
                                             tv[:, :, 0, :].unsqueeze(3).to_broadcast([128, 8, 16, 16]),
                                             tv[:, :, 1, :].unsqueeze(2).to_broadcast([128, 8, 16, 16]), ALU.add), reads=[topr], writes=[candr])
        yield
        for hh in range(8):
            fw.op("dve", lambda e, hh=hh: e.max(out=c8[:, hh, 0:8], in_=cand[:, hh, :]), reads=[candr], writes=[c8r])
            fw.op("dve", lambda e, hh=hh: e.match_replace(out=cand2[:, hh, :], in_to_replace=c8[:, hh, 0:8], in_values=cand[:, hh, :], imm_value=-1e30),
                  reads=[candr, c8r], writes=[cand2r])
            fw.op("dve", lambda e, hh=hh: e.max(out=c8[:, hh, 8:16], in_=cand2[:, hh, :]), reads=[cand2r], writes=[c8r])
            yield
        fw.op("dve", lambda e: e.tensor_tensor(selm[:], c8[:], c8[:, :, 0:1].to_broadcast([128, 8, 16]), ALU.subtract), reads=[c8r], writes=[selmr])
        fw.op("act", lambda e: e.activation(out=selm[:], in_=selm[:], func=AF.Exp), reads=[selmr], writes=[selmr])
        yield
        fw.op("dve", lambda e: e.tensor_reduce(zz[:, 0:8], selm[:], AX.X, ALU.add), reads=[selmr], writes=[zzr])
        sv = ssb[:].rearrange("p (h s) k -> p h s k", s=2)
        fw.op("dve", lambda e: e.tensor_tensor(thr[:], sv[:, :, 0, :], c8[:, :, 15:16].to_broadcast([128, 8, 128]), ALU.subtract),
              reads=[ssbr, c8r], writes=[thrr])
        yield
        fw.op("act", lambda e: e.activation(out=lnc[:, 0:8], in_=zz[:, 0:8], func=AF.Ln), reads=[zzr], writes=[lncr])
        fw.op("dve", lambda e: e.tensor_tensor(lnc[:, 8:16], c8[:, :, 15], c8[:, :, 0], ALU.subtract), reads=[c8r], writes=[lncr])
        yield
        fw.op("dve", lambda e: e.tensor_tensor(lnc[:, 16:24], lnc[:, 8:16], lnc[:, 0:8], ALU.subtract), reads=[lncr], writes=[lncr])
        fw.op("dve", lambda e: e.tensor_scalar(lnc[:, 24:32], lnc[:, 16:24], -5e-6, None, ALU.add), reads=[lncr], writes=[lncr])
        yield

    steps = [(ig, hh) for ig in range(16) for hh in range(8)]
    nst = len(steps)
    for _ in prologue(0):
        pass
    for ti, tb in enumerate(tbs):
        ssb, ssbr = ssbs[ti % 2]
        thr, thrr = thrs[ti % 2]
        lnc, lncr = lncs[ti % 2]
        sv = ssb[:].rearrange("p (h s) k -> p h s k", s=2)
        nxt = prologue(ti + 1) if ti + 1 < len(tbs) else None
        gs_, gs_r = gst[0]
        zb = {}
        eb = {}
        tbuf = {}

        def _A(si):
            ig, hh = steps[si]
            z_, z_r = Zt[si % len(Zt)]
            fw.op("dve", lambda e: e.tensor_tensor(
                z_[:], sv[:, hh, 1, :].unsqueeze(1).to_broadcast([128, 8, 128]),
                thr[:, hh, ig * 8:(ig + 1) * 8].unsqueeze(2).to_broadcast([128, 8, 128]), ALU.add), reads=[ssbr, thrr], writes=[z_r], inc=True)
            zb[si] = (z_, z_r)

        def _B(si):
            ig, hh = steps[si]
            z_, z_r = zb[si]
            if si % 4 != 3:
                T_, T_r = Tt[si % 5]
                fw.op("act", lambda e: e.activation(out=z_[:], in_=z_[:], func=AF.Prelu, alpha=1e5, bias=5e-6), reads=[z_r], writes=[z_r], inc=True)
                fw.op("act", lambda e: e.activation(out=T_[:], in_=z_[:], func=AF.Exp, bias=lnc[:, 24 + hh:25 + hh]), reads=[z_r, lncr], writes=[T_r], inc=True)
                tbuf[si] = (T_, T_r)
                zb.pop(si)
                return
            e_, e_r = Mt[si % 4]
            fw.op("act", lambda e: e.activation(out=e_[:], in_=z_[:], func=AF.Exp, bias=lnc[:, 16 + hh:17 + hh]), reads=[z_r, lncr], writes=[e_r], inc=True)
            eb[si] = (e_, e_r)

        def _C(si):
            if si % 4 != 3:
                return
            z_, z_r = zb.pop(si)
            e_, e_r = eb.pop(si)
            T_, T_r = Tt[si % 5]
            fw.op("dve", lambda e: e.scalar_tensor_tensor(T_[:], z_[:], -5e-6, e_[:], ALU.is_ge, ALU.mult), reads=[z_r, e_r], writes=[T_r], inc=True)
            tbuf[si] = (T_, T_r)

        banks = None
        _A(0)
        _A(1)
        _A(2)
        _B(0)
        _B(1)
        for si in range(nst):
            ig, hh = steps[si]
            if si + 3 < nst:
                _A(si + 3)
            if si + 2 < nst:
                _B(si + 2)
            _C(si)
            if hh == 0:
                banks = [P.bank(), P.bank()]
            T_, T_r = tbuf.pop(si)
            for ii in range(8):
                bk, bkr = banks[ii // 4]
                fw.op("pe", lambda e, bk=bk, ii=ii, T_=T_, hh=hh: e.matmul(
                    bk[:, (ii % 4) * 128:(ii % 4 + 1) * 128], T_[:, ii, :], identb[:],
                    start=(hh == 0 and ii % 4 == 0), stop=(hh == 7 and ii % 4 == 3), skip_group_check=True), reads=[T_r, identbr], writes=[bkr])
            if hh == 7:
                for b in range(2):
                    bk, bkr = banks[b]
                    fw.op("act", lambda e, bk=bk, b=b, ig=ig: e.copy(gs_[:, ig * 8 + b * 4:ig * 8 + b * 4 + 4, :].rearrange("p i t -> p (i t)"), bk[:]),
                          reads=[bkr], writes=[gs_r])
            if nxt is not None and si >= 8 and hh in (2, 5):
                next(nxt, None)
        if nxt is not None:
            for _ in nxt:
                pass
        for q4 in range(4):
            fw.dma("sp", gv[:, q4 * 32:(q4 + 1) * 32, tb * 128:(tb + 1) * 128], gs_[:, q4 * 32:(q4 + 1) * 32, :], reads=[gs_r], writes=[GdTr])
    P.phase_end()


def ph_peer_u(P, l, ctx_out):
    fw = P.fw
    P.phase_begin()
    identb, identbr = P.C["ident_bf"]
    hT, hTr = P.dr["hT"]
    GdT, GdTr = P.dr["GdT"]
    GAT, GATr = P.dr["GAT"]
    pu = P.dr["peer_u"][0].ap()
    h, hr = P.sb("pu_h", [128, 16, NT], BF16)
    hv = hT.ap().rearrange("(fc p) t -> p fc t", p=128)
    for q4 in range(4):
        fw.dma("sp", h[:, q4 * 4:(q4 + 1) * 4, :], hv[:, q4 * 4:(q4 + 1) * 4, :], reads=[hTr], writes=[hr])
    Ur = [P.sb("pu_ur%d" % i, [128, 2048], BF16) for i in range(2)]
    UT = [P.sb("pu_ut%d" % i, [128, 16, 128], BF16) for i in range(2)]
    gt = [P.sb("pu_g%d" % i, [128, NT], BF16) for i in range(2)]
    at = [P.sb("pu_a%d" % i, [128, 512], BF16) for i in range(3)]
    st = [P.sb("pu_s%d" % i, [128, NT], BF16) for i in range(2)]
    groups = [g for g in TG if (g[2] == 0 or ctx_out)]
    c_lo = 0 if ctx_out else L
    ai = 0
    def _load(ec):
        ur, urr = Ur[ec % 2]
        fw.dma("pool", ur[:], pu[l, ec * 128:(ec + 1) * 128, :], writes=[urr])
        g_, g_r = gt[ec % 2]
        fw.dma("sp", g_[:, c_lo:NT], GdT.ap()[ec * 128:(ec + 1) * 128, c_lo:NT], reads=[GdTr], writes=[g_r])

    _load(0)
    for ec in range(128):
        if ec + 1 < 128:
            _load(ec + 1)
        ur, urr = Ur[ec % 2]
        g_, g_r = gt[ec % 2]
        ut, utr = UT[ec % 2]
        for b4 in range(4):
            pst, psr = P.bank()
            for q_ in range(4):
                kc = b4 * 4 + q_
                fw.op("pe", lambda e, pst=pst, q_=q_, kc=kc, ur=ur: e.matmul(pst[:, q_ * 128:(q_ + 1) * 128], ur[:, kc * 128:(kc + 1) * 128], identb[:], start=True, stop=True),
                      reads=[urr, identbr], writes=[psr])
            dst = ut[:, b4 * 4:(b4 + 1) * 4, :].rearrange("p c k -> p (c k)")
            if b4 % 2 == 0:
                fw.op("dve", lambda e, pst=pst, dst=dst: e.tensor_copy(dst, pst[:]), reads=[psr], writes=[utr])
            else:
                fw.op("pool", lambda e, pst=pst, dst=dst: e.tensor_copy(dst, pst[:]), reads=[psr], writes=[utr]) if False else \
                    fw.op("dve", lambda e, pst=pst, dst=dst: e.tensor_copy(dst, pst[:]), reads=[psr], writes=[utr])
        s_, s_r = st[ec % 2]
        for (t0, w, j) in groups:
            pst, psr = P.bank()
            for kc in range(16):
                fw.op("pe", lambda e, pst=pst, kc=kc, ut=ut: e.matmul(pst[:, 0:w], ut[:, kc, :], h[:, kc, t0:t0 + w], start=(kc == 0), stop=(kc == 15)),
                      reads=[utr, hr], writes=[psr])
            a_, a_r = at[ai % 3]
            ai += 1
            fw.op("act", lambda e, a_=a_, pst=pst: e.activation(out=a_[:, 0:w], in_=pst[:, 0:w], func=AF.Gelu), reads=[psr], writes=[a_r])
            fw.op("pool", lambda e, a_=a_, s_=s_, g_=g_: e.tensor_tensor(s_[:, t0:t0 + w], a_[:, 0:w], g_[:, t0:t0 + w], ALU.mult), reads=[a_r, g_r], writes=[s_r])
        fw.dma("sp", GAT.ap()[ec * 128:(ec + 1) * 128, c_lo:NT], s_[:, c_lo:NT], reads=[s_r], writes=[GATr])
    P.phase_end()


def ph_peer_v(P, l, ctx_out):
    fw = P.fw
    P.phase_begin()
    GAT, GATr = P.dr["GAT"]
    xT, xTr = P.dr["xT"]
    pv = P.dr["peer_v"][0].ap()
    acc, accr = P.sb("pv_acc", [128, 8, NT], F32)
    ares = [[Res("acc%d_%d" % (dc, gi)) for gi in range(len(TG))] for dc in range(8)]
    GA = [P.sb("pv_ga%d" % i, [128, 4, NT], BF16) for i in range(2)]
    Vw = [P.sb("pv_v%d" % i, [128, 4, 1024], BF16) for i in range(2)]
    xs = [P.sb("pv_x%d" % i, [128, 512], F32) for i in range(3)]
    groups = [(gi, g) for gi, g in enumerate(TG) if (g[2] == 0 or ctx_out)]
    c_lo = 0 if ctx_out else L
    gav = GAT.ap().rearrange("(g e p) t -> g p e t", p=128, e=4)
    pvv = pv[l].rearrange("(g e p) d -> g p e d", p=128, e=4)
    xi = 0
    ev = 0
    for dh in range(2):
        for g in range(32):
            ga, gar = GA[g % 2]
            vw, vwr = Vw[g % 2]
            fw.dma("sp", ga[:, :, c_lo:NT], gav[g][:, :, c_lo:NT], reads=[GATr], writes=[gar])
            fw.dma("pool", vw[:], pvv[g][:, :, dh * 1024:(dh + 1) * 1024], writes=[vwr])
            for dc in range(8):
                for gi, (t0, w, j) in groups:
                    pst, psr = P.bank()
                    for e4 in range(4):
                        fw.op("pe", lambda e, pst=pst, e4=e4, vw=vw, ga=ga, dc=dc: e.matmul(pst[:, 0:w], vw[:, e4, dc * 128:(dc + 1) * 128], ga[:, e4, t0:t0 + w],
                                                                                          start=(e4 == 0), stop=(e4 == 3)), reads=[vwr, gar], writes=[psr])
                    ar = ares[dc][gi]
                    eng = "dve" if ev % 2 == 0 else "pool"
                    ev += 1
                    if g == 0:
                        if eng == "dve":
                            fw.op("dve", lambda e, pst=pst, dc=dc: e.tensor_copy(acc[:, dc, t0:t0 + w], pst[:, 0:w]), reads=[psr], writes=[ar])
                        else:
                            fw.op("act", lambda e, pst=pst, dc=dc: e.copy(acc[:, dc, t0:t0 + w], pst[:, 0:w]), reads=[psr], writes=[ar])
                    else:
                        fw.op("dve", lambda e, pst=pst, dc=dc: e.tensor_tensor(acc[:, dc, t0:t0 + w], acc[:, dc, t0:t0 + w], pst[:, 0:w], ALU.add),
                              reads=[psr, ar], writes=[ar])
        for dc in range(8):
            fo = dh * 8 + dc
            for gi, (t0, w, j) in groups:
                x_, x_r = xs[xi % 3]
                xi += 1
                fw.dma("sp", x_[:, 0:w], xT.ap()[fo * 128:(fo + 1) * 128, t0:t0 + w], reads=[xTr], writes=[x_r])
                fw.op("dve", lambda e, x_=x_, dc=dc, fo=fo, j=j: e.scalar_tensor_tensor(x_[:, 0:w], acc[:, dc, t0:t0 + w], P.modv[:, l, 5, fo, j:j + 1], x_[:, 0:w], ALU.mult, ALU.add),
                      reads=[ares[dc][gi], x_r, P.modr], writes=[x_r])
                fw.dma("sp", xT.ap()[fo * 128:(fo + 1) * 128, t0:t0 + w], x_[:, 0:w], reads=[x_r], writes=[xTr])
    P.phase_end()


_PROG = {}


def kernel(**inputs):
    inp = {k: np.asarray(v) for k, v in inputs.items()}
    if "p" not in _PROG:
        _PROG["p"] = build_program()
    P = _PROG["p"]
    consts = build_consts()
    smalls = np.stack([pack_smalls(inp, l) for l in range(DEPTH)])
    extra = extra_host_inputs(inp)
    in_maps = []
    for b in range(8):
        m = host_inputs(inp, b, consts, smalls)
        m.update(extra)
        in_maps.append({k: np.ascontiguousarray(m[k]) for k in P.inputs})
    res = run_bass_kernel_spmd(P.nc, in_maps, core_ids=list(range(8)))
    return np.stack([np.asarray(res.results[b]["y"]) for b in range(8)]).astype(np.float32)
```

```python
import bisect
import math
from contextlib import ExitStack

import numpy as np
import concourse.bass as bass
import concourse.mybir as mybir
from concourse.bass_utils import run_bass_kernel_spmd

F32 = mybir.dt.float32
BF16 = mybir.dt.bfloat16
AF = mybir.ActivationFunctionType
ALU = mybir.AluOpType
AX = mybir.AxisListType

D = 2048
T = 2048
L = 256
NT = T + L
NTB = NT // 128
DEPTH = 2
INW = 5952
EPS = 1e-6
TG = [(0, 256, 1)] + [(256 + 512 * i, 512, 0) for i in range(4)]
NEG = -30000.0
GATE_TBS = None
ATTN_WARM = 0


class Res:
    __slots__ = ("name", "w", "r")

    def __init__(self, name=""):
        self.name = name
        self.w = None
        self.r = {}


class FW:
    NDS = 24

    def __init__(self, nc, es, same_engine_sync=True):
        self.nc = nc
        self.E = {"pe": nc.tensor, "dve": nc.vector, "act": nc.scalar, "pool": nc.gpsimd, "sp": nc.sync}
        self.csem = {e: es.enter_context(nc.semaphore("c_" + e)) for e in ("pe", "dve", "act", "pool")}
        self.seq = {e: 0 for e in self.csem}
        self.last = {e: None for e in self.csem}
        self.incd = {e: ([], []) for e in self.csem}
        self.dsems = [es.enter_context(nc.semaphore("d%d" % i)) for i in range(self.NDS)]
        self.dcnt = [0] * self.NDS
        self.qsl = {"sp": (0, 14), "pool": (14, 24)}
        self.dnext = {"sp": 0, "pool": 14}
        self.seen = {e: {} for e in self.E}
        self.same = same_engine_sync
        self.nwait = 0
        self.nins = 0

    def _resolve(self, y, seq):
        seqs, counts = self.incd[y]
        i = bisect.bisect_left(seqs, seq)
        if i < len(seqs):
            return seqs[i], counts[i]
        h = self.last[y]
        cnt = (counts[-1] if counts else 0) + 1
        h.then_inc(self.csem[y], 1)
        seqs.append(self.seq[y])
        counts.append(cnt)
        return self.seq[y], cnt

    def _wait(self, eng, tok):
        if tok[0] == "c":
            _, y, seq = tok
            if y == eng and (not self.same or eng == "pe"):
                return
            key = ("c", y)
            if self.seen[eng].get(key, 0) >= seq:
                return
            s2, cnt = self._resolve(y, seq)
            self.E[eng].wait_ge(self.csem[y], cnt)
            self.seen[eng][key] = s2
        else:
            _, i, cnt = tok
            key = ("d", i)
            if self.seen[eng].get(key, 0) >= cnt:
                return
            self.E[eng].wait_ge(self.dsems[i], cnt)
            self.seen[eng][key] = cnt
        self.nwait += 1

    def _deps(self, eng, reads, writes):
        for r in reads:
            if r.w is not None:
                self._wait(eng, r.w)
        for w in writes:
            if w.w is not None:
                self._wait(eng, w.w)
            for k, v in w.r.items():
                self._wait(eng, (k[0], k[1], v))

    def op(self, eng, fn, reads=(), writes=(), inc=False):
        self._deps(eng, reads, writes)
        ins = fn(self.E[eng])
        self.nins += 1
        self.seq[eng] += 1
        self.last[eng] = ins
        s = self.seq[eng]
        if inc or eng != "pe":
            seqs, counts = self.incd[eng]
            ins.then_inc(self.csem[eng], 1)
            seqs.append(s)
            counts.append((counts[-1] if counts else 0) + 1)
        for r in reads:
            r.r[("c", eng)] = s
        for w in writes:
            w.w = ("c", eng, s)
            w.r = {}
        return ins

    def dma(self, q, out, in_, reads=(), writes=(), **kw):
        self._deps(q, reads, writes)
        i = self.dnext[q]
        lo, hi = self.qsl[q]
        self.dnext[q] = lo + (i + 1 - lo) % (hi - lo)
        self.dcnt[i] += 16
        self.E[q].dma_start(out=out, in_=in_, **kw).then_inc(self.dsems[i], 16)
        self.nins += 1
        for r in reads:
            r.r[("d", i)] = self.dcnt[i]
        for w in writes:
            w.w = ("d", i, self.dcnt[i])
            w.r = {}

    def barrier(self, engines=("sp", "pe", "dve", "act", "pool")):
        for eng in engines:
            for i in range(self.NDS):
                if self.dcnt[i]:
                    self._wait(eng, ("d", i, self.dcnt[i]))
            for y in self.csem:
                if self.seq[y] and y != eng:
                    self._wait(eng, ("c", y, self.seq[y]))


SM = {}
_o = 0
for _n, _w in [("norm1_g", 16), ("norm2_g", 16), ("b_gate", 64), ("b_mod", 96), ("dqg", 1), ("dkg", 1),
               ("dsub", 1), ("rnorm", 1), ("nqg", 1), ("nkg", 1), ("mqn", 4), ("mkvn", 2),
               ("mq_nope", 1), ("mq_rope", 1), ("mk_nope", 1), ("mk_rope", 1),
               ("dlam", 256), ("rdecay", 8)]:
    SM[_n] = (_o, _w)
    _o += _w
NSM = _o


def _cols(v):
    v = np.asarray(v, np.float32).reshape(-1, 128)
    return np.ascontiguousarray(v.T)


def pack_smalls(inp, l):
    s = np.zeros((128, NSM), np.float32)

    def put(name, arr):
        o, w = SM[name]
        arr = np.asarray(arr, np.float32).reshape(128, w)
        s[:, o:o + w] = arr

    put("norm1_g", _cols(inp["norm1_g"][l]))
    put("norm2_g", _cols(inp["norm2_g"][l]))
    put("b_gate", np.stack([_cols(inp["b_gate"][l][n]) for n in range(4)], axis=1))
    put("b_mod", _cols(inp["b_mod"][l]))
    put("dqg", np.tile(inp["diff_qk_g"][l][0], 2))
    put("dkg", np.tile(inp["diff_qk_g"][l][1], 2))
    put("dsub", inp["diff_sub_g"][l])
    put("rnorm", inp["ret_norm_g"][l])
    put("nqg", np.tile(inp["na_qk_g"][l][0], 2))
    put("nkg", np.tile(inp["na_qk_g"][l][1], 2))
    put("mqn", _cols(inp["mla_q_norm_g"][l]))
    put("mkvn", _cols(inp["mla_kv_norm_g"][l]))
    put("mq_nope", inp["mla_qk_g"][l][0][:128])
    put("mq_rope", np.tile(inp["mla_qk_g"][l][0][128:], 2))
    put("mk_nope", inp["mla_qk_g"][l][1][:128])
    put("mk_rope", np.tile(inp["mla_qk_g"][l][1][128:], 2))
    put("dlam", np.tile(inp["diff_lambda"][l].reshape(1, 256), (128, 1)))
    put("rdecay", np.tile(inp["ret_decay"][l].reshape(1, 8), (128, 1)))
    return s


class Prog:
    def __init__(self, ext_in=(), ext_out=(), same_engine_sync=True):
        self.nc = bass.Bass("TRN2", target_bir_lowering=False)
        self.es = ExitStack()
        self.fw = FW(self.nc, self.es, same_engine_sync)
        self.ext_in = set(ext_in)
        self.ext_out = set(ext_out)
        self.dr = {}
        self.inputs = []
        self.outputs = []
        nc = self.nc
        self.ps = []
        self.psr = []
        for i in range(8):
            self.ps.append(self.es.enter_context(nc.psum_tensor("ps%d" % i, [128, 512], F32)))
            self.psr.append(Res("ps%d" % i))
        self.pnext = 0
        self.pes = None

    def inp(self, name, shape, dt=F32):
        t = self.nc.dram_tensor(name, list(shape), dt, kind="ExternalInput")
        self.inputs.append(name)
        self.dr[name] = (t, Res(name))
        return t

    def scratch(self, name, shape, dt):
        if name in self.ext_in:
            kind = "ExternalInput"
            self.inputs.append(name)
        elif name in self.ext_out:
            kind = "ExternalOutput"
            self.outputs.append(name)
        else:
            kind = "Internal"
        t = self.nc.dram_tensor(name, list(shape), dt, kind=kind)
        self.dr[name] = (t, Res(name))
        return t

    def out(self, name, shape, dt=F32):
        t = self.nc.dram_tensor(name, list(shape), dt, kind="ExternalOutput")
        self.outputs.append(name)
        self.dr[name] = (t, Res(name))
        return t

    def R(self, name):
        return self.dr[name][1]

    def phase_begin(self, name=None):
        self.fw.barrier()
        self.pes = ExitStack()
        self.pidx = getattr(self, "pidx", 0) + 1
        if name is None:
            import inspect
            name = inspect.stack()[1].function
        self.pes.enter_context(self.nc.named_scope("%02d_%s" % (self.pidx, name)))

    def phase_end(self):
        self.fw.barrier()
        self.pes.close()
        self.pes = None

    def sb(self, name, shape, dt=F32, persistent=False):
        es = self.es if persistent else self.pes
        self.uid = getattr(self, "uid", 0) + 1
        t = es.enter_context(self.nc.sbuf_tensor("sb%d_%s" % (self.uid, name), list(shape), dt))
        return t, Res(name)

    def bank(self, i=None):
        if i is None:
            i = self.pnext
            self.pnext = (self.pnext + 1) % 8
        return self.ps[i], self.psr[i]


def rope_tables(n_tok, dim):
    m = dim // 2
    inv = 10000.0 ** (-np.arange(0, m, 2, dtype=np.float32) / m)
    t = np.arange(n_tok)
    row = (t // 64).astype(np.float32)
    col = (t % 64).astype(np.float32)
    ar = row[:, None] * inv
    ac = col[:, None] * inv
    ang = np.concatenate([ar, ar, ac, ac], axis=-1).astype(np.float32)
    return np.cos(ang).astype(np.float32), np.sin(ang).astype(np.float32)


def rot_matrix(dim, reps):
    q = dim // 4
    Rm = np.zeros((dim * reps, dim * reps), np.float32)
    for r in range(reps):
        o = r * dim
        for i in range(q):
            Rm[o + q + i, o + i] = -1.0
            Rm[o + i, o + q + i] = 1.0
            Rm[o + 3 * q + i, o + 2 * q + i] = -1.0
            Rm[o + 2 * q + i, o + 3 * q + i] = 1.0
    return Rm


def build_consts():
    c = {}
    c["ident"] = np.eye(128, dtype=np.float32)
    c["ones"] = np.ones((128, 128), np.float32)
    bd = np.zeros((128, 128), np.float32)
    bd[:64, :64] = 1.0
    bd[64:, 64:] = 1.0
    c["bd64"] = bd
    cos64, sin64 = rope_tables(T, 64)
    c["cos64"] = np.ascontiguousarray(np.tile(cos64.T, (2, 1)))
    c["sin64"] = np.ascontiguousarray(np.tile(sin64.T, (2, 1)))
    cos128, sin128 = rope_tables(T, 128)
    c["cos128"] = np.ascontiguousarray(cos128.T)
    c["sin128"] = np.ascontiguousarray(sin128.T)
    n = np.arange(128, dtype=np.float32)
    dif = n[None, :] - n[:, None]
    c["rett"] = np.ascontiguousarray(np.concatenate([
        np.maximum(dif, 0), (dif >= 0).astype(np.float32), np.maximum(-dif, 0), (dif <= 0).astype(np.float32),
        np.tile(n[None, :] + 1.0, (128, 1)), np.tile(128.0 - n[None, :], (128, 1)),
        (127.0 - n)[:, None], n[:, None]], axis=1).astype(np.float32))
    c["rot64"] = rot_matrix(64, 2)
    c["rot128"] = rot_matrix(128, 1)
    return c


def ph_setup(P, do_mod=True):
    fw, nc = P.fw, P.nc
    P.phase_begin()
    C = {}
    for nm in ("ident", "ones", "bd64", "rot64", "rot128"):
        t, r = P.sb("c_" + nm, [128, 128], F32, persistent=True)
        fw.dma("sp", t[:], P.dr["k_" + nm][0].ap(), writes=[r])
        tb, rb = P.sb("cb_" + nm, [128, 128], BF16, persistent=True)
        fw.op("dve", lambda e, tb=tb, t=t: e.tensor_copy(tb[:], t[:]), reads=[r], writes=[rb])
        C[nm] = (t, r)
        C[nm + "_bf"] = (tb, rb)
    sm, smr = P.sb("smalls", [128, DEPTH, NSM], F32, persistent=True)
    fw.dma("sp", sm[:], P.dr["smalls"][0].ap().rearrange("l p n -> p l n"), writes=[smr])
    C["sm"] = (sm, smr)
    P.C = C

    modv, modr = P.sb("modv", [128, DEPTH, 6, 16, 2], F32, persistent=True)
    a12, a12r = P.sb("a12", [128, DEPTH, 2, 16, 2], F32, persistent=True)
    cv, cvr = P.sb("cvec", [128, 16, 2], F32)
    fw.dma("sp", cv[:], P.dr["cvec"][0].ap(), writes=[cvr])
    sv, svr = P.sb("svec", [128, 16, 2], BF16)
    fw.op("act", lambda e: e.activation(out=sv[:], in_=cv[:], func=AF.Silu), reads=[cvr], writes=[svr])
    NB = 2
    wb = [P.sb("wmod%d" % i, [128, 3072], BF16) for i in range(NB)]
    it = 0
    for l in range(DEPTH if do_mod else 0):
        wm = P.dr["w_mod"][0].ap()
        for g in range(4):
            pst, psr = P.bank()
            for kc in range(16):
                wt, wr = wb[it % NB]
                it += 1
                fw.dma("pool", wt[:], wm[l, kc * 128:(kc + 1) * 128, g * 3072:(g + 1) * 3072], writes=[wr])
                for n in range(24):
                    fw.op("pe", lambda e, n=n, wt=wt, kc=kc, pst=pst: e.matmul(
                        pst[:, n * 2:n * 2 + 2], wt[:, n * 128:(n + 1) * 128], sv[:, kc, :],
                        start=(kc == 0 and n == 0), stop=(kc == 15 and n == 23), skip_group_check=True),
                        reads=[wr, svr], writes=[psr])
            o, w = SM["b_mod"]
            mv = modv[:, l].rearrange("p m f j -> p (m f) j")
            fw.op("dve", lambda e, g=g, pst=pst, mv=mv, l=l, o=o: e.tensor_tensor(
                mv[:, g * 24:(g + 1) * 24, :], pst[:, 0:48].rearrange("p (n j) -> p n j", j=2),
                sm[:, l, o + g * 24:o + (g + 1) * 24].unsqueeze(2).to_broadcast([128, 24, 2]), ALU.add),
                reads=[psr, smr], writes=[modr])
    for l in range(DEPTH if do_mod else 0):
        for w_, (gn, mi) in enumerate((("norm1_g", 1), ("norm2_g", 4))):
            o, _ = SM[gn]
            fw.op("dve", lambda e, l=l, w_=w_, mi=mi, o=o: e.scalar_tensor_tensor(
                a12[:, l, w_], modv[:, l, mi], 1.0, sm[:, l, o:o + 16].unsqueeze(2).to_broadcast([128, 16, 2]),
                ALU.add, ALU.mult), reads=[modr, smr], writes=[a12r])
    P.modv, P.modr, P.a12, P.a12r = modv, modr, a12, a12r
    P.phase_end()


def ph_transpose_in(P):
    fw = P.fw
    P.phase_begin()
    ident, identr = P.C["ident"]
    xT, xTr = P.dr["xT"]
    xTv = xT.ap().rearrange("(fc p) t -> p fc t", p=128)
    srcs = [(P.dr["ctx_b"], 0, 2), (P.dr["x_b"], 0, 4), (P.dr["x_b"], 4, 4), (P.dr["x_b"], 8, 4), (P.dr["x_b"], 12, 4)]
    xin = [P.sb("xin%d" % i, [128, 2048], F32) for i in range(2)]
    stg = [P.sb("xstg%d" % i, [128, 16, 512], F32) for i in range(2)]
    it = 0
    tok0 = 0
    for gi, ((src, srcr), b0, nb) in enumerate(srcs):
        st, sr = stg[gi % 2]
        for j in range(nb):
            xt, xr = xin[it % 2]
            it += 1
            fw.dma("sp", xt[:], src.ap()[(b0 + j) * 128:(b0 + j + 1) * 128, :], reads=[srcr], writes=[xr])
            for f4 in range(4):
                pst, psr = P.bank()
                for q in range(4):
                    fc = f4 * 4 + q
                    fw.op("pe", lambda e, pst=pst, q=q, xt=xt, fc=fc: e.transpose(
                        pst[:, q * 128:(q + 1) * 128], xt[:, fc * 128:(fc + 1) * 128], ident[:]),
                        reads=[xr, identr], writes=[psr])
                eng = "dve" if f4 % 2 == 0 else "act"
                dst = st[:, f4 * 4:(f4 + 1) * 4, j * 128:(j + 1) * 128]
                srcp = pst[:].rearrange("p (q t) -> p q t", q=4)
                if eng == "dve":
                    fw.op("dve", lambda e, dst=dst, srcp=srcp: e.tensor_copy(dst, srcp), reads=[psr], writes=[sr])
                else:
                    fw.op("act", lambda e, dst=dst, srcp=srcp: e.copy(dst, srcp), reads=[psr], writes=[sr])
        w = nb * 128
        fw.dma("sp", xTv[:, :, tok0:tok0 + w], st[:, :, 0:w], reads=[sr], writes=[xTr])
        tok0 += w
    P.phase_end()


def ph_norm(P, l, which, dst):
    fw = P.fw
    P.phase_begin()
    ones, onesr = P.C["ones"]
    xT, xTr = P.dr["xT"]
    hT, hTr = P.dr[dst]
    xTv = xT.ap().rearrange("(fc p) t -> p fc t", p=128)
    hTv = hT.ap().rearrange("(fc p) t -> p fc t", p=128)
    xs = [P.sb("nx%d" % i, [128, 16, 512], F32) for i in range(2)]
    sq, sqr = P.sb("nsq", [128, 512], F32)
    sqs = [P.sb("nsq%d" % i, [128, 512], F32) for i in range(2)]
    rs, rsr = P.sb("nrs", [128, 512], F32)
    tmp = [P.sb("ntmp%d" % i, [128, 512], F32) for i in range(2)]
    hs = [P.sb("nh%d" % i, [128, 16, 512], BF16) for i in range(2)]
    shi = 0 if which == 0 else 3
    for gi, (t0, w, j) in enumerate(TG):
        xt, xr = xs[gi % 2]
        fw.dma("sp", xt[:, :, 0:w], xTv[:, :, t0:t0 + w], reads=[xTr], writes=[xr])
        pst, psr = P.bank()
        for fc in range(16):
            s_, s_r = sqs[fc % 2]
            fw.op("act", lambda e, s_=s_, fc=fc: e.activation(out=s_[:, 0:w], in_=xt[:, fc, 0:w], func=AF.Square),
                  reads=[xr], writes=[s_r])
            fw.op("pe", lambda e, s_=s_, fc=fc: e.matmul(pst[:, 0:w], ones[:], s_[:, 0:w], start=(fc == 0), stop=(fc == 15)),
                  reads=[onesr, s_r], writes=[psr])
        fw.op("act", lambda e: e.activation(out=rs[:, 0:w], in_=pst[:, 0:w], func=AF.Sqrt, bias=EPS, scale=1.0 / D),
              reads=[psr], writes=[rsr])
        fw.op("dve", lambda e: e.reciprocal(rs[:, 0:w], rs[:, 0:w]), reads=[rsr], writes=[rsr])
        ht, hr = hs[gi % 2]
        for fc in range(16):
            tt, tr = tmp[fc % 2]
            fw.op("dve", lambda e, tt=tt, fc=fc: e.tensor_tensor(tt[:, 0:w], xt[:, fc, 0:w], rs[:, 0:w], ALU.mult),
                  reads=[xr, rsr], writes=[tr])
            fw.op("act", lambda e, tt=tt, fc=fc: e.activation(
                out=ht[:, fc, 0:w], in_=tt[:, 0:w], func=AF.Identity,
                bias=P.modv[:, l, shi, fc, j:j + 1], scale=P.a12[:, l, which, fc, j:j + 1]),
                reads=[tr, P.modr, P.a12r], writes=[hr])
        fw.dma("sp", hTv[:, :, t0:t0 + w], ht[:, :, 0:w], reads=[hr], writes=[hTr])
    P.phase_end()


def ph_inproj(P, l):
    fw = P.fw
    P.phase_begin()
    hT, hTr = P.dr["hT"]
    pT, pTr = P.dr["pT"]
    pV, pVr = P.dr["pV"]
    h, hr = P.sb("ih", [128, 16, NT], BF16)
    hv = hT.ap().rearrange("(fc p) t -> p fc t", p=128)
    for q in range(4):
        fw.dma("sp", h[:, q * 4:(q + 1) * 4, :], hv[:, q * 4:(q + 1) * 4, :], reads=[hTr], writes=[hr])
    wbuf = [P.sb("iw%d" % i, [128, 16, 512], BF16) for i in range(2)]
    stg = [P.sb("istg%d" % i, [128, NT], F32) for i in range(2)]
    vst = [P.sb("ivst%d" % i, [128, 512], BF16) for i in range(2)]
    win = P.dr["w_in"][0].ap()
    vmap = {2: 0, 5: 1, 9: 2}
    ev = 0
    si = 0
    for cg in range(12):
        c0 = cg * 512
        cw = min(512, INW - c0)
        wt, wr = wbuf[cg % 2]
        fw.dma("pool", wt[:, :, 0:cw], win[l, :, c0:c0 + cw].rearrange("(kc p) n -> p kc n", p=128), writes=[wr])
        if cg in vmap:
            vi = vmap[cg]
            for tb in range(NTB):
                pst, psr = P.bank()
                for kc in range(16):
                    fw.op("pe", lambda e, pst=pst, kc=kc, tb=tb, wt=wt: e.matmul(
                        pst[:, :], h[:, kc, tb * 128:(tb + 1) * 128], wt[:, kc, :], start=(kc == 0), stop=(kc == 15)),
                        reads=[hr, wr], writes=[psr])
                vt, vr = vst[tb % 2]
                if ev % 2 == 0:
                    fw.op("dve", lambda e, vt=vt, pst=pst: e.tensor_copy(vt[:], pst[:]), reads=[psr], writes=[vr])
                else:
                    fw.op("act", lambda e, vt=vt, pst=pst: e.copy(vt[:], pst[:]), reads=[psr], writes=[vr])
                ev += 1
                fw.dma("sp", pV.ap()[vi, tb * 128:(tb + 1) * 128, :], vt[:], reads=[vr], writes=[pVr])
            continue
        for oc in range((cw + 127) // 128):
            m = min(128, cw - oc * 128)
            st, sr = stg[si % 2]
            si += 1
            for (t0, w, j) in TG:
                pst, psr = P.bank()
                for kc in range(16):
                    fw.op("pe", lambda e, pst=pst, kc=kc, wt=wt, oc=oc, m=m, t0=t0, w=w: e.matmul(
                        pst[0:m, 0:w], wt[:, kc, oc * 128:oc * 128 + m], h[:, kc, t0:t0 + w],
                        start=(kc == 0), stop=(kc == 15)), reads=[hr, wr], writes=[psr])
                if ev % 2 == 0:
                    fw.op("dve", lambda e, st=st, pst=pst, m=m, t0=t0, w=w: e.tensor_copy(st[0:m, t0:t0 + w], pst[0:m, 0:w]),
                          reads=[psr], writes=[sr])
                else:
                    fw.op("act", lambda e, st=st, pst=pst, m=m, t0=t0, w=w: e.copy(st[0:m, t0:t0 + w], pst[0:m, 0:w]),
                          reads=[psr], writes=[sr])
                ev += 1
            r0 = c0 + oc * 128
            fw.dma("sp", pT.ap()[r0:r0 + m, :], st[0:m, :], reads=[sr], writes=[pTr])
    P.phase_end()


WEIGHT_SHAPES = {
    "w_mod": (DEPTH, D, 6 * D), "w_in": (DEPTH, D, INW), "w_uq": (DEPTH, 512, 768), "w_ukv": (DEPTH, 256, 1024),
    "w_branch": (DEPTH, 4, 512, D), "w_gate": (DEPTH, 4, D, D), "w_o": (DEPTH, D, D),
    "peer_w_query": (DEPTH, D, D), "peer_sub_keys": (DEPTH, 8, 2, 128, 128),
    "peer_u": (DEPTH, 16384, D), "peer_v": (DEPTH, 16384, D),
}
CONST_SHAPES = {"k_ident": (128, 128), "k_ones": (128, 128), "k_bd64": (128, 128), "k_rot64": (128, 128),
                "k_rot128": (128, 128), "k_cos64": (128, T), "k_sin64": (128, T), "k_cos128": (128, T),
                "k_sin128": (128, T), "k_rett": (128, 6 * 128 + 2)}


def build_program(phases=None, ext_in=(), ext_out=(), same_engine_sync=True, weights=None, do_mod=True):
    P = Prog(ext_in, ext_out, same_engine_sync)
    P.inp("x_b", (T, D))
    P.inp("ctx_b", (L, D))
    P.inp("cvec", (128, 16, 2))
    P.inp("smalls", (DEPTH, 128, NSM))
    for k, shp in CONST_SHAPES.items():
        P.inp(k, shp)
    for k, shp in WEIGHT_SHAPES.items():
        if weights is None or k in weights:
            P.inp(k, shp)
    P.inp("nab", (DEPTH, 128, 8 * 16 * 64))
    P.scratch("xT", (D, NT), F32)
    P.scratch("hT", (D, NT), BF16)
    P.scratch("pT", (INW, NT), F32)
    P.scratch("pV", (3, NT, 512), BF16)
    P.scratch("yT", (4, 512, NT), BF16)
    P.scratch("qT", (D, NT), BF16)
    P.scratch("GdT", (16384, NT), BF16)
    P.scratch("GAT", (16384, NT), BF16)
    P.out("y", (T, D))
    run = (lambda n: True) if phases is None else (lambda n: n in phases)
    ph_setup(P, do_mod)
    if run("tin"):
        ph_transpose_in(P)
    for l in range(DEPTH):
        if run("norm1_%d" % l):
            ph_norm(P, l, 0, "hT")
        if run("inproj_%d" % l):
            ph_inproj(P, l)
        if run("diff_%d" % l):
            ph_mix_diff(P, l, l < DEPTH - 1)
        if run("mla_%d" % l):
            ph_mix_mla(P, l, l < DEPTH - 1)
        if run("ret_%d" % l):
            ph_mix_ret(P, l, l < DEPTH - 1)
        if run("na_%d" % l):
            ph_mix_na(P, l, l < DEPTH - 1)
        if run("merge_%d" % l):
            ph_merge(P, l, l < DEPTH - 1)
        if run("norm2_%d" % l):
            ph_norm(P, l, 1, "hT")
        if run("peerq_%d" % l):
            ph_peer_q(P, l)
        if run("peerg_%d" % l):
            ph_peer_gate(P, l, l < DEPTH - 1)
        if run("peeru_%d" % l):
            ph_peer_u(P, l, l < DEPTH - 1)
        if run("peerv_%d" % l):
            ph_peer_v(P, l, l < DEPTH - 1)
    if run("tout"):
        ph_transpose_out(P)
    P.fw.barrier()
    P.es.close()
    return P


def host_inputs(inp, b, consts, smalls):
    m = {"x_b": np.ascontiguousarray(inp["x"][b]), "ctx_b": np.ascontiguousarray(inp["ctx"][b])}
    cv = np.stack([_cols(inp["c"][b]), _cols(inp["c_ctx"])], axis=2)
    m["cvec"] = np.ascontiguousarray(cv.astype(np.float32))
    m["smalls"] = smalls
    for k, v in consts.items():
        m["k_" + k] = v
    for k in WEIGHT_SHAPES:
        m[k] = inp[k]
    return m


def load_tables(P, names):
    out = {}
    for nm in names:
        t, r = P.sb("tb_" + nm, [128, T], F32)
        P.fw.dma("sp", t[:], P.dr["k_" + nm][0].ap(), writes=[r])
        out[nm] = (t, r)
    return out


class Scr:
    def __init__(self, P, name, n, dt=F32, w=512):
        self.t = [P.sb("%s%d" % (name, i), [128, w], dt) for i in range(n)]
        self.i = 0

    def get(self):
        x = self.t[self.i % len(self.t)]
        self.i += 1
        return x


def interleave(gens):
    gens = list(gens)
    while gens:
        for g in list(gens):
            try:
                next(g)
            except StopIteration:
                gens.remove(g)


def run(gen):
    for _ in gen:
        pass


def rms_rows_g(P, S, srcs, w, bdmat, gsize, dst_r, out_rs):
    fw = P.fw
    pst, psr = P.bank()
    bd, bdr = bdmat
    for i, (ap, r, k) in enumerate(srcs):
        sq, sqr = S.get()
        fw.op("act", lambda e, sq=sq, ap=ap, k=k: e.activation(out=sq[0:k, 0:w], in_=ap, func=AF.Square), reads=[r], writes=[sqr])
        yield
        fw.op("pe", lambda e, sq=sq, k=k, i=i: e.matmul(pst[:, 0:w], bd[0:k, :], sq[0:k, 0:w], start=(i == 0), stop=(i == len(srcs) - 1)),
              reads=[sqr, bdr], writes=[psr])
        yield
    fw.op("act", lambda e: e.activation(out=out_rs[:, 0:w], in_=pst[:, 0:w], func=AF.Sqrt, bias=EPS, scale=1.0 / gsize),
          reads=[psr], writes=[dst_r])
    yield
    fw.op("dve", lambda e: e.reciprocal(out_rs[:, 0:w], out_rs[:, 0:w]), reads=[dst_r], writes=[dst_r])
    yield


def rms_rows(P, S, srcs, w, bdmat, gsize, dst_r, out_rs):
    run(rms_rows_g(P, S, srcs, w, bdmat, gsize, dst_r, out_rs))


def rope_apply_g(P, S, xn, xnr, k, t0, w, rotm, cos, sin, dst, dstr):
    fw = P.fw
    rm, rmr = rotm
    (ct, cr), (st, sr) = cos, sin
    p0 = t0 - L
    pst, psr = P.bank()
    fw.op("pe", lambda e: e.matmul(pst[0:k, 0:w], rm[0:k, 0:k], xn[0:k, 0:w], start=True, stop=True), reads=[xnr, rmr], writes=[psr])
    yield
    t1, t1r = S.get()
    fw.op("pool", lambda e: e.tensor_tensor(t1[0:k, 0:w], xn[0:k, 0:w], ct[0:k, p0:p0 + w], ALU.mult), reads=[xnr, cr], writes=[t1r])
    yield
    t2, t2r = S.get()
    fw.op("dve", lambda e: e.tensor_tensor(t2[0:k, 0:w], pst[0:k, 0:w], st[0:k, p0:p0 + w], ALU.mult), reads=[psr, sr], writes=[t2r])
    yield
    fw.op("dve", lambda e: e.tensor_tensor(dst[0:k, t0:t0 + w], t1[0:k, 0:w], t2[0:k, 0:w], ALU.add), reads=[t1r, t2r], writes=[dstr])
    yield


def rope_apply(P, S, xn, xnr, k, t0, w, rotm, cos, sin, dst, dstr):
    run(rope_apply_g(P, S, xn, xnr, k, t0, w, rotm, cos, sin, dst, dstr))


def prep_qk_g(P, S, src_ap, srcr, dst, dstr, gain, bdmat, gsize, rope=None, scale=None):
    fw = P.fw
    sm, smr = P.C["sm"]
    for (t0, w, j) in TG:
        x, xr = S.get()
        fw.dma("sp", x[:, 0:w], src_ap[:, t0:t0 + w], reads=[srcr], writes=[xr])
        yield
        do_rope = rope is not None and j == 0
        if bdmat is not None:
            rs, rsr = S.get()
            yield from rms_rows_g(P, S, [(x[:, 0:w], xr, 128)], w, bdmat, gsize, rsr, rs)
            if do_rope:
                xn, xnr = S.get()
                fw.op("dve", lambda e, xn=xn, x=x, rs=rs: e.scalar_tensor_tensor(xn[:, 0:w], x[:, 0:w], gain, rs[:, 0:w], ALU.mult, ALU.mult),
                      reads=[xr, rsr, smr], writes=[xnr])
            else:
                fw.op("dve", lambda e, x=x, rs=rs: e.scalar_tensor_tensor(dst[:, t0:t0 + w], x[:, 0:w], gain, rs[:, 0:w], ALU.mult, ALU.mult),
                      reads=[xr, rsr, smr], writes=[dstr])
            yield
        else:
            if do_rope:
                if scale is not None:
                    xn, xnr = S.get()
                    fw.op("act", lambda e, xn=xn, x=x: e.mul(xn[:, 0:w], x[:, 0:w], scale), reads=[xr], writes=[xnr])
                else:
                    xn, xnr = x, xr
            else:
                fw.op("act", lambda e, x=x: e.mul(dst[:, t0:t0 + w], x[:, 0:w], 1.0 if scale is None else scale), reads=[xr], writes=[dstr])
            yield
        if do_rope:
            yield from rope_apply_g(P, S, xn, xnr, 128, t0, w, rope[0], rope[1], rope[2], dst, dstr)


def prep_qk(P, S, *a, **kw):
    run(prep_qk_g(P, S, *a, **kw))


def attn_core(P, tag, heads, npass, parts_fn, V, Vr, scale, ctx_out, finish_fn, Ebufs):
    fw = P.fw
    onesb, onesbr = P.C["ones_bf"]
    groups = [g for g in TG if (g[2] == 0 or ctx_out)]
    heads = list(heads)

    def lane_gen(lane, lheads):
        osb = [[P.sb("%s_o%d_%d_%d" % (tag, lane, p, i), [128, 512], F32) for i in range(2)] for p in range(npass)]
        rz = [P.sb("%s_rz%d_%d" % (tag, lane, i), [128, 512], F32) for i in range(2)]
        Eb = [P.sb("%s_E%d_%d" % (tag, lane, i), [128, 512], BF16) for i in range(3)]
        ei = 0
        gi = 0
        for h in lheads:
            for (t0, w, j) in groups:
                nkc = 2 if j == 1 else NTB
                outs = []
                for p in range(npass):
                    Ob, Obr = P.bank(4 + 2 * lane)
                    Zb, Zbr = P.bank(5 + 2 * lane)
                    parts = parts_fn(h, p)

                    def _pv(kc, E, Er):
                        fw.op("pe", lambda e, E=E, kc=kc: e.matmul(Ob[:, 0:w], V[:, kc, h * 128:(h + 1) * 128], E[:, 0:w],
                                                                 start=(kc == 0), stop=(kc == nkc - 1)), reads=[Er, Vr], writes=[Obr])
                        yield
                        fw.op("pe", lambda e, E=E, kc=kc: e.matmul(Zb[:, 0:w], onesb[:], E[:, 0:w],
                                                                 start=(kc == 0), stop=(kc == nkc - 1)), reads=[Er, onesbr], writes=[Zbr])
                        yield

                    pend = []
                    for kc in range(nkc):
                        Sb, Sbr = P.bank(2 * lane + ei % 2)
                        for i, (kf, qf, kr_, qr_) in enumerate(parts):
                            fw.op("pe", lambda e, Sb=Sb, kf=kf, qf=qf, kc=kc, i=i: e.matmul(
                                Sb[:, 0:w], kf(kc * 128, (kc + 1) * 128), qf(t0, t0 + w), start=(i == 0), stop=(i == len(parts) - 1)),
                                reads=[kr_, qr_], writes=[Sbr])
                        yield
                        E, Er = Eb[ei % 3]
                        ei += 1
                        fw.op("act", lambda e, E=E, Sb=Sb: e.activation(out=E[:, 0:w], in_=Sb[:, 0:w], func=AF.Exp, scale=scale),
                              reads=[Sbr], writes=[Er], inc=True)
                        yield
                        pend.append((kc, E, Er))
                        if len(pend) > 2:
                            yield from _pv(*pend.pop(0))
                    while pend:
                        yield from _pv(*pend.pop(0))
                    rzt, rzr = rz[p % 2]
                    fw.op("dve", lambda e, rzt=rzt, Zb=Zb: e.reciprocal(rzt[:, 0:w], Zb[:, 0:w]), reads=[Zbr], writes=[rzr])
                    ot, otr = osb[p][gi % 2]
                    fw.op("dve", lambda e, ot=ot, Ob=Ob, rzt=rzt: e.tensor_tensor(ot[:, 0:w], Ob[:, 0:w], rzt[:, 0:w], ALU.mult),
                          reads=[Obr, rzr], writes=[otr])
                    yield
                    outs.append((ot, otr))
                gi += 1
                finish_fn(h, t0, w, outs)
                yield

    nh = len(heads)
    interleave([lane_gen(0, heads[:nh // 2]), lane_gen(1, heads[nh // 2:])])


def ph_mix_diff(P, l, ctx_out):
    fw = P.fw
    P.phase_begin()
    sm, smr = P.C["sm"]
    pT, pTr = P.dr["pT"]
    pV, pVr = P.dr["pV"]
    yT, yTr = P.dr["yT"]
    tabs = load_tables(P, ["cos64", "sin64"])
    S = Scr(P, "dsc", 16)
    q, qr = P.sb("dq", [128, 4, NT], BF16)
    k, kr = P.sb("dk", [128, 4, NT], BF16)
    V, Vr = P.sb("dv", [128, NTB, 512], BF16)
    fw.dma("sp", V[:], pV.ap()[0].rearrange("(tb p) n -> p tb n", p=128), reads=[pVr], writes=[Vr])
    rope = (P.C["rot64"], tabs["cos64"], tabs["sin64"])
    for h in range(4):
        interleave([
            prep_qk_g(P, S, pT.ap()[h * 128:(h + 1) * 128, :], pTr, q[:, h, :], qr, sm[:, l, SM["dqg"][0]:SM["dqg"][0] + 1], P.C["bd64"], 64, rope),
            prep_qk_g(P, S, pT.ap()[512 + h * 128:512 + (h + 1) * 128, :], pTr, k[:, h, :], kr, sm[:, l, SM["dkg"][0]:SM["dkg"][0] + 1], P.C["bd64"], 64, rope)])
    lam_init = 0.8 - 0.6 * math.exp(-0.3 * l)
    o = SM["dlam"][0]
    lt, ltr = P.sb("dlamt", [128, 128], F32)
    lc, lcr = P.sb("dlamc", [128, 4], F32)
    fw.op("dve", lambda e: e.tensor_tensor(lt[:, 0:64], sm[:, l, o:o + 64], sm[:, l, o + 64:o + 128], ALU.mult), reads=[smr], writes=[ltr])
    fw.op("dve", lambda e: e.tensor_tensor(lt[:, 64:128], sm[:, l, o + 128:o + 192], sm[:, l, o + 192:o + 256], ALU.mult), reads=[smr], writes=[ltr])
    fw.op("dve", lambda e: e.tensor_reduce(lc[:, 0:2], lt[:].rearrange("p (a b) -> p a b", a=2), AX.X, ALU.add), reads=[ltr], writes=[lcr])
    fw.op("act", lambda e: e.activation(out=lc[:, 0:2], in_=lc[:, 0:2], func=AF.Exp), reads=[lcr], writes=[lcr])
    fw.op("dve", lambda e: e.scalar_tensor_tensor(lc[:, 2:3], lc[:, 1:2], -lam_init, lc[:, 0:1], ALU.add, ALU.subtract), reads=[lcr], writes=[lcr])
    fw.op("dve", lambda e: e.tensor_scalar(lc[:, 3:4], sm[:, l, SM["dsub"][0]:SM["dsub"][0] + 1], 1.0 - lam_init, None, ALU.mult), reads=[smr], writes=[lcr])
    Ebufs = None
    ystg = [P.sb("dy%d" % i, [128, 512], BF16) for i in range(2)]
    cnt = [0]

    def parts_fn(h, p):
        b = 64 * p
        return [(lambda c0, c1: k[b:b + 64, h, c0:c1], lambda c0, c1: q[b:b + 64, h, c0:c1], kr, qr)]

    def finish(h, t0, w, outs):
        (o1, o1r), (o2, o2r) = outs
        y, yr_ = S.get()
        fw.op("dve", lambda e: e.scalar_tensor_tensor(y[:, 0:w], o2[:, 0:w], lc[:, 2:3], o1[:, 0:w], ALU.mult, ALU.add),
              reads=[o1r, o2r, lcr], writes=[yr_])
        rs, rsr = S.get()
        rms_rows(P, S, [(y[:, 0:w], yr_, 128)], w, P.C["ones"], 128, rsr, rs)
        yo, yor = ystg[cnt[0] % 2]
        cnt[0] += 1
        fw.op("dve", lambda e: e.scalar_tensor_tensor(yo[:, 0:w], y[:, 0:w], lc[:, 3:4], rs[:, 0:w], ALU.mult, ALU.mult),
              reads=[yr_, rsr, lcr], writes=[yor])
        fw.dma("sp", yT.ap()[0, h * 128:(h + 1) * 128, t0:t0 + w], yo[:, 0:w], reads=[yor], writes=[yTr])

    attn_core(P, "da", range(4), 2, parts_fn, V, Vr, 64 ** -0.5, ctx_out, finish, Ebufs)
    P.phase_end()


def ph_mix_mla(P, l, ctx_out):
    fw = P.fw
    P.phase_begin()
    sm, smr = P.C["sm"]
    pT, pTr = P.dr["pT"]
    yT, yTr = P.dr["yT"]
    ones, onesr = P.C["ones"]
    tabs = load_tables(P, ["cos64", "sin64"])
    rope = (P.C["rot64"], tabs["cos64"], tabs["sin64"])
    S = Scr(P, "msc", 10)
    wuq, wuqr = P.sb("m_wuq", [128, 4, 768], BF16)
    fw.dma("pool", wuq[:], P.dr["w_uq"][0].ap()[l].rearrange("(kc p) n -> p kc n", p=128), writes=[wuqr])
    wukv, wukvr = P.sb("m_wukv", [128, 2, 1024], BF16)
    fw.dma("pool", wukv[:], P.dr["w_ukv"][0].ap()[l].rearrange("(kc p) n -> p kc n", p=128), writes=[wukvr])
    cqn, cqnr = P.sb("m_cqn", [128, 4, NT], BF16)
    ckvn, ckvnr = P.sb("m_ckvn", [128, 2, NT], BF16)
    krt, krtr = P.sb("m_kr", [64, NT], F32)
    qn_, qnr = P.sb("m_qn", [128, 4, NT], BF16)
    qr_, qrr = P.sb("m_qr", [64, 4, NT], BF16)
    kn_, knr = P.sb("m_kn", [128, 4, NT], BF16)
    kro, kror = P.sb("m_kro", [64, 4, NT], BF16)
    V, Vr = P.sb("m_v", [128, NTB, 512], BF16)
    fw.dma("sp", krt[:], pT.ap()[5888:5952, :], reads=[pTr], writes=[krtr])
    for (nm, r0, nch, dst, dstr, gname) in (("cq", 5120, 4, cqn, cqnr, "mqn"), ("ckv", 5632, 2, ckvn, ckvnr, "mkvn")):
        go = SM[gname][0]
        for (t0, w, j) in TG:
            xs = []
            for c in range(nch):
                x, xr = S.get()
                fw.dma("sp", x[:, 0:w], pT.ap()[r0 + c * 128:r0 + (c + 1) * 128, t0:t0 + w], reads=[pTr], writes=[xr])
                xs.append((x, xr))
            rs, rsr = S.get()
            rms_rows(P, S, [(x[:, 0:w], xr, 128) for (x, xr) in xs], w, P.C["ones"], nch * 128, rsr, rs)
            for c, (x, xr) in enumerate(xs):
                fw.op("dve", lambda e, x=x, c=c, rs=rs: e.scalar_tensor_tensor(dst[:, c, t0:t0 + w], x[:, 0:w], sm[:, l, go + c:go + c + 1], rs[:, 0:w], ALU.mult, ALU.mult),
                      reads=[xr, rsr, smr], writes=[dstr])
    ev = 0
    for tb in range(NTB):
        pst, psr = P.bank()
        for kc in range(2):
            fw.op("pe", lambda e, pst=pst, kc=kc, tb=tb: e.matmul(
                pst[:].rearrange("p (h d) -> p h d", h=4), ckvn[:, kc, tb * 128:(tb + 1) * 128],
                wukv[:, kc, :].rearrange("p (h x) -> p h x", h=4)[:, :, 128:256], start=(kc == 0), stop=(kc == 1)),
                reads=[ckvnr, wukvr], writes=[psr])
        fw.op("dve" if tb % 2 == 0 else "act",
              (lambda e, pst=pst, tb=tb: e.tensor_copy(V[:, tb, :], pst[:])) if tb % 2 == 0 else (lambda e, pst=pst, tb=tb: e.copy(V[:, tb, :], pst[:])),
              reads=[psr], writes=[Vr])
    gq_n = sm[:, l, SM["mq_nope"][0]:SM["mq_nope"][0] + 1]
    gq_r = sm[:, l, SM["mq_rope"][0]:SM["mq_rope"][0] + 1]
    gk_n = sm[:, l, SM["mk_nope"][0]:SM["mk_nope"][0] + 1]
    gk_r = sm[:, l, SM["mk_rope"][0]:SM["mk_rope"][0] + 1]
    for h in range(4):
        for (t0, w, j) in TG:
            pn, pnr = P.bank()
            pr, prr = P.bank()
            for kc in range(4):
                fw.op("pe", lambda e, kc=kc: e.matmul(pn[:, 0:w], wuq[:, kc, h * 192:h * 192 + 128], cqn[:, kc, t0:t0 + w], start=(kc == 0), stop=(kc == 3)),
                      reads=[wuqr, cqnr], writes=[pnr])
            for kc in range(4):
                fw.op("pe", lambda e, kc=kc: e.matmul(pr[0:64, 0:w], wuq[:, kc, h * 192 + 128:h * 192 + 192], cqn[:, kc, t0:t0 + w], start=(kc == 0), stop=(kc == 3)),
                      reads=[wuqr, cqnr], writes=[prr])
            xn, xnr = S.get()
            xr_, xrr = S.get()
            fw.op("act", lambda e, xn=xn: e.copy(xn[:, 0:w], pn[:, 0:w]), reads=[pnr], writes=[xnr])
            fw.op("dve", lambda e, xr_=xr_: e.tensor_copy(xr_[0:64, 0:w], pr[0:64, 0:w]), reads=[prr], writes=[xrr])
            rs, rsr = S.get()
            rms_rows(P, S, [(xn[:, 0:w], xnr, 128), (xr_[0:64, 0:w], xrr, 64)], w, P.C["ones"], 192, rsr, rs)
            fw.op("dve", lambda e, xn=xn, rs=rs: e.scalar_tensor_tensor(qn_[:, h, t0:t0 + w], xn[:, 0:w], gq_n, rs[:, 0:w], ALU.mult, ALU.mult),
                  reads=[xnr, rsr, smr], writes=[qnr])
            if j == 0:
                xq, xqr = S.get()
                fw.op("dve", lambda e, xq=xq, xr_=xr_, rs=rs: e.scalar_tensor_tensor(xq[0:64, 0:w], xr_[0:64, 0:w], gq_r[0:64], rs[0:64, 0:w], ALU.mult, ALU.mult),
                      reads=[xrr, rsr, smr], writes=[xqr])
                rope_apply(P, S, xq, xqr, 64, t0, w, rope[0], rope[1], rope[2], qr_[:, h, :], qrr)
            else:
                fw.op("dve", lambda e, xr_=xr_, rs=rs: e.scalar_tensor_tensor(qr_[:, h, t0:t0 + w], xr_[0:64, 0:w], gq_r[0:64], rs[0:64, 0:w], ALU.mult, ALU.mult),
                      reads=[xrr, rsr, smr], writes=[qrr])
            pk, pkr = P.bank()
            for kc in range(2):
                fw.op("pe", lambda e, kc=kc: e.matmul(pk[:, 0:w], wukv[:, kc, h * 256:h * 256 + 128], ckvn[:, kc, t0:t0 + w], start=(kc == 0), stop=(kc == 1)),
                      reads=[wukvr, ckvnr], writes=[pkr])
            xk, xkr = S.get()
            fw.op("act", lambda e, xk=xk: e.copy(xk[:, 0:w], pk[:, 0:w]), reads=[pkr], writes=[xkr])
            rs2, rs2r = S.get()
            rms_rows(P, S, [(xk[:, 0:w], xkr, 128), (krt[0:64, t0:t0 + w], krtr, 64)], w, P.C["ones"], 192, rs2r, rs2)
            fw.op("dve", lambda e, xk=xk, rs2=rs2: e.scalar_tensor_tensor(kn_[:, h, t0:t0 + w], xk[:, 0:w], gk_n, rs2[:, 0:w], ALU.mult, ALU.mult),
                  reads=[xkr, rs2r, smr], writes=[knr])
            if j == 0:
                xq2, xq2r = S.get()
                fw.op("dve", lambda e, xq2=xq2, rs2=rs2: e.scalar_tensor_tensor(xq2[0:64, 0:w], krt[0:64, t0:t0 + w], gk_r[0:64], rs2[0:64, 0:w], ALU.mult, ALU.mult),
                      reads=[krtr, rs2r, smr], writes=[xq2r])
                rope_apply(P, S, xq2, xq2r, 64, t0, w, rope[0], rope[1], rope[2], kro[:, h, :], kror)
            else:
                fw.op("dve", lambda e, rs2=rs2: e.scalar_tensor_tensor(kro[:, h, t0:t0 + w], krt[0:64, t0:t0 + w], gk_r[0:64], rs2[0:64, 0:w], ALU.mult, ALU.mult),
                      reads=[krtr, rs2r, smr], writes=[kror])
    Ebufs = None
    ystg = [P.sb("my%d" % i, [128, 512], BF16) for i in range(2)]
    cnt = [0]

    def parts_fn(h, p):
        return [(lambda c0, c1: kn_[:, h, c0:c1], lambda c0, c1: qn_[:, h, c0:c1], knr, qnr),
                (lambda c0, c1: kro[:, h, c0:c1], lambda c0, c1: qr_[:, h, c0:c1], kror, qrr)]

    def finish(h, t0, w, outs):
        (o1, o1r), = outs
        yo, yor = ystg[cnt[0] % 2]
        cnt[0] += 1
        fw.op("act", lambda e: e.copy(yo[:, 0:w], o1[:, 0:w]), reads=[o1r], writes=[yor])
        fw.dma("sp", yT.ap()[3, h * 128:(h + 1) * 128, t0:t0 + w], yo[:, 0:w], reads=[yor], writes=[yTr])

    attn_core(P, "ma", range(4), 1, parts_fn, V, Vr, 192 ** -0.5, ctx_out, finish, Ebufs)
    P.phase_end()


def ph_mix_ret(P, l, ctx_out):
    fw = P.fw
    P.phase_begin()
    sm, smr = P.C["sm"]
    pT, pTr = P.dr["pT"]
    pV, pVr = P.dr["pV"]
    yT, yTr = P.dr["yT"]
    identb, identbr = P.C["ident_bf"]
    tabs = load_tables(P, ["cos128", "sin128"])
    rope = (P.C["rot128"], tabs["cos128"], tabs["sin128"])
    S = Scr(P, "rsc", 16)
    q, qr = P.sb("r_q", [128, 4, NT], BF16)
    k, kr = P.sb("r_k", [128, 4, NT], BF16)
    V, Vr = P.sb("r_v", [128, NTB, 512], BF16)
    fw.dma("sp", V[:], pV.ap()[1].rearrange("(tb p) n -> p tb n", p=128), reads=[pVr], writes=[Vr])
    for h in range(4):
        interleave([
            prep_qk_g(P, S, pT.ap()[1536 + h * 128:1536 + (h + 1) * 128, :], pTr, q[:, h, :], qr, None, None, None, rope, None),
            prep_qk_g(P, S, pT.ap()[2048 + h * 128:2048 + (h + 1) * 128, :], pTr, k[:, h, :], kr, None, None, None, rope, 128 ** -0.5)])
    rt, rtr = P.sb("r_rt", [128, 6 * 128 + 2], F32)
    fw.dma("sp", rt[:], P.dr["k_rett"][0].ap(), writes=[rtr])
    lg, lgr = P.sb("r_lg", [128, 8], F32)
    o = SM["rdecay"][0]
    fw.op("act", lambda e: e.activation(out=lg[:], in_=sm[:, l, o:o + 8], func=AF.Exp, scale=-1.0), reads=[smr], writes=[lgr])
    fw.op("act", lambda e: e.activation(out=lg[:], in_=lg[:], func=AF.Ln, bias=1.0), reads=[lgr], writes=[lgr])
    fw.op("act", lambda e: e.mul(lg[:], lg[:], -1.0), reads=[lgr], writes=[lgr])
    DmT, DmTr = P.sb("r_dm", [128, 8, 128], F32)
    XI, XIr = P.sb("r_xi", [128, 8, 128], F32)
    ZG, ZGr = P.sb("r_zg", [128, 8, 2], F32)
    c128, c128r = P.sb("r_c128", [128, 1], F32)
    fw.op("dve", lambda e: e.memset(c128[:], 128.0), writes=[c128r])
    for d in range(2):
        for h in range(4):
            i = d * 4 + h
            col = lg[:, i:i + 1]
            fw.op("act", lambda e, i=i, d=d, col=col: e.activation(out=DmT[:, i, :], in_=rt[:, d * 256:d * 256 + 128], func=AF.Exp, scale=col),
                  reads=[rtr, lgr], writes=[DmTr])
            fw.op("dve", lambda e, i=i, d=d: e.tensor_tensor(DmT[:, i, :], DmT[:, i, :], rt[:, d * 256 + 128:d * 256 + 256], ALU.mult),
                  reads=[rtr, DmTr], writes=[DmTr])
            fw.op("act", lambda e, i=i, d=d, col=col: e.activation(out=XI[:, i, :], in_=rt[:, 512 + d * 128:512 + (d + 1) * 128], func=AF.Exp, scale=col),
                  reads=[rtr, lgr], writes=[XIr])
            fw.op("act", lambda e, i=i, d=d, col=col: e.activation(out=ZG[:, i, 0:1], in_=rt[:, 768 + d:768 + d + 1], func=AF.Exp, scale=col),
                  reads=[rtr, lgr], writes=[ZGr])
            fw.op("act", lambda e, i=i, col=col: e.activation(out=ZG[:, i, 1:2], in_=c128[:], func=AF.Exp, scale=col),
                  reads=[c128r, lgr], writes=[ZGr])
    yacc, yaccr = P.sb("r_yacc", [128, 4, NT], F32)
    yres = [[Res("yacc%d_%d" % (h, c)) for c in range(NTB)] for h in range(4)]
    for h in range(4):
        fw.op("pool", lambda e, h=h: e.memset(yacc[:, h, :], 0.0), writes=yres[h])
    Rf = [P.sb("r_R%d" % i, [128, 128], F32) for i in range(8)]
    Rb = [P.sb("r_Rb%d" % i, [128, 128], BF16) for i in range(8)]
    stm = [P.sb("r_stm%d" % i, [128, 128], BF16) for i in range(8)]
    qx = [P.sb("r_qx%d" % i, [128, 128], BF16) for i in range(8)]
    kz = [P.sb("r_kz%d" % i, [128, 128], BF16) for i in range(8)]
    order = {0: list(range(NTB)), 1: [1, 0] + list(range(NTB - 1, 1, -1))}
    def _step(si, d, h):
        i = d * 4 + h
        c = order[d][si]
        sl = slice(c * 128, (c + 1) * 128)
        need_out = (c >= 2) or ctx_out
        R_, R_r = Rf[i]
        Rb_, Rb_r = Rb[i]
        if need_out:
            st_, st_r = P.bank()
            fw.op("pe", lambda e: e.matmul(st_[:, 0:128], k[:, h, sl], q[:, h, sl], start=True, stop=True), reads=[kr, qr], writes=[st_r])
            yield
            sm_, sm_r = stm[i]
            fw.op("dve", lambda e: e.tensor_tensor(sm_[:], st_[:, 0:128], DmT[:, i, :], ALU.mult), reads=[st_r, DmTr], writes=[sm_r])
            yield
            ob, obr = P.bank()
            fw.op("pe", lambda e: e.matmul(ob[:, 0:128], V[:, c, h * 128:(h + 1) * 128], sm_[:], start=True, stop=(si == 0)), reads=[Vr, sm_r], writes=[obr])
            yield
            if si > 0:
                qx_, qx_r = qx[i]
                fw.op("pool", lambda e: e.tensor_tensor(qx_[:], q[:, h, sl], XI[:, i, :], ALU.mult), reads=[qr, XIr], writes=[qx_r])
                yield
                fw.op("pe", lambda e: e.matmul(ob[:, 0:128], Rb_[:], qx_[:], start=False, stop=True), reads=[Rb_r, qx_r], writes=[obr])
                yield
            yr_ = yres[h][c]
            fw.op("dve", lambda e: e.tensor_tensor(yacc[:, h, sl], yacc[:, h, sl], ob[:, 0:128], ALU.add), reads=[obr, yr_], writes=[yr_])
            yield
        if si < NTB - 1:
            kt, ktr = P.bank()
            fw.op("pe", lambda e: e.matmul(kt[:, 0:128], k[:, h, sl], identb[:], start=True, stop=True), reads=[kr, identbr], writes=[ktr])
            yield
            kz_, kz_r = kz[i]
            fw.op("act", lambda e: e.activation(out=kz_[:], in_=kt[:, 0:128], func=AF.Copy, scale=ZG[:, i, 0:1]), reads=[ktr, ZGr], writes=[kz_r])
            yield
            rn, rnr = P.bank()
            fw.op("pe", lambda e: e.matmul(rn[:, 0:128], kz_[:], V[:, c, h * 128:(h + 1) * 128], start=True, stop=True), reads=[kz_r, Vr], writes=[rnr])
            yield
            if si == 0:
                fw.op("dve", lambda e: e.tensor_copy(R_[:], rn[:, 0:128]), reads=[rnr], writes=[R_r])
            else:
                fw.op("dve", lambda e: e.scalar_tensor_tensor(R_[:], R_[:], ZG[:, i, 1:2], rn[:, 0:128], ALU.mult, ALU.add), reads=[rnr, R_r, ZGr], writes=[R_r])
            yield
            fw.op("act", lambda e: e.copy(Rb_[:], R_[:]), reads=[R_r], writes=[Rb_r])
            yield

    for si in range(NTB):
        for d in range(2):
            interleave([_step(si, d, h) for h in range(4)])
    allres = [yres[h][c] for h in range(4) for c in range(NTB)]
    ystg = [P.sb("r_y%d" % i, [128, 512], BF16) for i in range(2)]
    cnt = 0
    go = SM["rnorm"][0]
    for h in range(4):
        for (t0, w, j) in TG:
            if j == 1 and not ctx_out:
                continue
            g_, g_r = S.get()
            fw.dma("sp", g_[:, 0:w], pT.ap()[3072 + h * 128:3072 + (h + 1) * 128, t0:t0 + w], reads=[pTr], writes=[g_r])
            fw.op("act", lambda e, g_=g_: e.activation(out=g_[:, 0:w], in_=g_[:, 0:w], func=AF.Silu), reads=[g_r], writes=[g_r])
            rs, rsr = S.get()
            sq, sqr = S.get()
            pst, psr = P.bank()
            ones, onesr = P.C["ones"]
            fw.op("act", lambda e, sq=sq, h=h: e.activation(out=sq[:, 0:w], in_=yacc[:, h, t0:t0 + w], func=AF.Square), reads=allres, writes=[sqr])
            fw.op("pe", lambda e, sq=sq, pst=pst: e.matmul(pst[:, 0:w], ones[:], sq[:, 0:w], start=True, stop=True), reads=[sqr, onesr], writes=[psr])
            fw.op("act", lambda e, rs=rs, pst=pst: e.activation(out=rs[:, 0:w], in_=pst[:, 0:w], func=AF.Sqrt, bias=EPS, scale=1.0 / 128), reads=[psr], writes=[rsr])
            fw.op("dve", lambda e, rs=rs: e.reciprocal(rs[:, 0:w], rs[:, 0:w]), reads=[rsr], writes=[rsr])
            t1, t1r = S.get()
            fw.op("dve", lambda e, t1=t1, h=h, rs=rs: e.scalar_tensor_tensor(t1[:, 0:w], yacc[:, h, t0:t0 + w], sm[:, l, go:go + 1], rs[:, 0:w], ALU.mult, ALU.mult),
                  reads=allres + [rsr, smr], writes=[t1r])
            yo, yor = ystg[cnt % 2]
            cnt += 1
            fw.op("dve", lambda e, yo=yo, t1=t1, g_=g_: e.tensor_tensor(yo[:, 0:w], t1[:, 0:w], g_[:, 0:w], ALU.mult), reads=[t1r, g_r], writes=[yor])
            fw.dma("sp", yT.ap()[1, h * 128:(h + 1) * 128, t0:t0 + w], yo[:, 0:w], reads=[yor], writes=[yTr])
    P.phase_end()


def build_nab(rpb):
    kc = np.arange(64)
    qc = np.arange(64)
    cs = np.clip(qc - 8, 0, 48)
    valid = (kc[:, None] >= cs[None, :]) & (kc[:, None] < cs[None, :] + 16)
    dc = np.clip(kc[:, None] - qc[None, :] + 15, 0, 30)
    out = np.full((2, 64, 8, 16, 64), NEG, np.float32)
    for i2 in range(2):
        for dr in range(16):
            d2 = dr + i2
            if d2 > 14:
                continue
            g = rpb[:, d2, :][:, dc]
            g = np.where(valid[None], g, np.float32(NEG))
            out[i2, :, :, dr, :] = np.transpose(g, (1, 0, 2))
    return np.ascontiguousarray(out.reshape(128, 8 * 16 * 64))


def extra_host_inputs(inp):
    return {"nab": np.stack([build_nab(np.asarray(inp["na_rpb"][l], np.float32)) for l in range(DEPTH)])}


def ph_mix_na(P, l, ctx_out):
    fw = P.fw
    P.phase_begin()
    sm, smr = P.C["sm"]
    pT, pTr = P.dr["pT"]
    pV, pVr = P.dr["pV"]
    yT, yTr = P.dr["yT"]
    identb, identbr = P.C["ident_bf"]
    S = Scr(P, "nsc", 14)
    q, qr = P.sb("n_q", [128, 4, NT], BF16)
    k, kr = P.sb("n_k", [128, 4, NT], BF16)
    for c in range(4):
        interleave([
            prep_qk_g(P, S, pT.ap()[3584 + c * 128:3584 + (c + 1) * 128, :], pTr, q[:, c, :], qr, sm[:, l, SM["nqg"][0]:SM["nqg"][0] + 1], P.C["bd64"], 64),
            prep_qk_g(P, S, pT.ap()[4096 + c * 128:4096 + (c + 1) * 128, :], pTr, k[:, c, :], kr, sm[:, l, SM["nkg"][0]:SM["nkg"][0] + 1], P.C["bd64"], 64)])
    NA_STOP = 99
    Vx, Vxr = P.sb("n_vx", [128, NTB, 8, 128], BF16)
    pv = pV.ap()[2].rearrange("(tb p) (h d) -> p tb h d", p=128, h=8)
    for hh in range(8):
        fw.dma("sp", Vx[:, :, hh, 0:64], pv[:, :, hh, :], reads=[pVr], writes=[Vxr])
    fw.op("pool", lambda e: e.memset(Vx[:, :, :, 64:128], 1.0), writes=[Vxr])
    nab, nabr = P.sb("n_nab", [128, 8, 16, 64], F32)
    fw.dma("sp", nab[:].rearrange("p h r c -> p (h r c)"), P.dr["nab"][0].ap()[l], writes=[nabr])
    es = [P.sb("n_es%d" % i, [128, 8, 128], F32) for i in range(2)]
    Eb = [P.sb("n_E%d" % i, [128, 8, 128], BF16) for i in range(3)]
    ytm = [P.sb("n_ytm%d" % i, [128, 8, 64], BF16) for i in range(2)]
    rzt = [P.sb("n_rz%d" % i, [128, 8], F32) for i in range(2)]
    ystg = [P.sb("n_ys%d" % i, [128, 4, 128], BF16) for i in range(2)]
    blocks = []
    if ctx_out:
        blocks += [("ctx", 0), ("ctx", 1)]
    blocks += [("lat", pr) for pr in range(16)]
    ci = 0
    if NA_STOP <= 2:
        blocks = []
    elif 30 <= NA_STOP < 40 or NA_STOP == 3:
        blocks = blocks[:2]
    elif NA_STOP == 4:
        blocks = blocks[:3]
    for bi, (kind, idx) in enumerate(blocks):
        if kind == "ctx":
            qt0 = idx * 128
            chunks = [(0, None), (1, None)]
        else:
            r = 2 * idx
            qt0 = 256 + r * 64
            rs0 = min(max(r - 4, 0), 24)
            rs1 = min(max(r + 1 - 4, 0), 24)
            nloc = 4 if rs1 == rs0 else 5
            chunks = [(0, None), (1, None)]
            for j in range(nloc):
                info = []
                for a in range(2):
                    rsa = rs0 if a == 0 else rs1
                    dr0 = (rs0 + 2 * j) - (r + a) + 7
                    inval = [i2 for i2 in range(2) if not (rsa <= rs0 + 2 * j + i2 < rsa + 8)]
                    info.append((min(max(dr0, 0), 15), inval))
                chunks.append((2 + rs0 // 2 + j, info))
        Ob = [P.bank(4 + 2 * (bi % 2)), P.bank(5 + 2 * (bi % 2))]

        def _stage1(cj, kc, info):
            nonlocal ci
            Sb = [P.bank(2 * (ci % 2)), P.bank(2 * (ci % 2) + 1)]
            E, Er = Eb[ci % 3]
            e_, e_r = es[ci % 2]
            ci += 1
            for h in range(8):
                hb = 64 * (h % 2)
                sb_, sb_r = Sb[h % 2]
                fw.op("pe", lambda e, sb_=sb_, h=h, hb=hb, kc=kc: e.matmul(
                    sb_[:, (h // 2) * 128:(h // 2 + 1) * 128], k[hb:hb + 64, h // 2, kc * 128:(kc + 1) * 128],
                    q[hb:hb + 64, h // 2, qt0:qt0 + 128], start=True, stop=True), reads=[kr, qr], writes=[sb_r])
            for par in range(2):
                sb_, sb_r = Sb[par]
                hs = slice(par * 4, par * 4 + 4)
                if info is None:
                    fw.op("act", lambda e, sb_=sb_, par=par, E=E: e.activation(
                        out=E[:].rearrange("p h t -> p (h t)")[:, par * 512:(par + 1) * 512], in_=sb_[:], func=AF.Exp, scale=0.125),
                        reads=[sb_r], writes=[Er])
                else:
                    for a_ in range(2):
                        dr0, inval = info[a_]
                        fw.op("dve", lambda e, sb_=sb_, hs=hs, a_=a_, dr0=dr0, e_=e_, par=par: e.scalar_tensor_tensor(
                            e_[:, hs, a_ * 64:(a_ + 1) * 64], sb_[:].rearrange("p (h t) -> p h t", h=4)[:, :, a_ * 64:(a_ + 1) * 64], 0.125,
                            nab[:, par:8:2, dr0, :], ALU.mult, ALU.add), reads=[sb_r, nabr], writes=[e_r])
                    fw.op("act", lambda e, hs=hs, E=E, e_=e_: e.activation(out=E[:, hs, :], in_=e_[:, hs, :], func=AF.Exp),
                          reads=[e_r], writes=[Er])
            if info is not None:
                for a_ in range(2):
                    for i2 in info[a_][1]:
                        fw.op("pool", lambda e, E=E, a_=a_, i2=i2: e.memset(E[i2 * 64:(i2 + 1) * 64, :, a_ * 64:(a_ + 1) * 64], 0.0), writes=[Er])
            return (cj, kc, E, Er)

        def _stage2(cj, kc, E, Er):
            for h in range(8):
                ob, obr = Ob[h // 4]
                first = (cj == 0 and h % 4 == 0)
                last = (cj == len(chunks) - 1 and h % 4 == 3)
                fw.op("pe", lambda e, ob=ob, h=h, E=E, kc=kc, first=first, last=last: e.matmul(
                    ob[:, (h % 4) * 128:(h % 4 + 1) * 128], E[:, (h % 2) * 4 + h // 2, :], Vx[:, kc, h, :], start=first, stop=last, skip_group_check=True),
                    reads=[Er, Vxr], writes=[obr])

        pend = None
        for cj, (kc, info) in enumerate(chunks):
            cur = _stage1(cj, kc, info)
            if pend is not None:
                _stage2(*pend)
            pend = cur
        _stage2(*pend)
        rz_, rz_r = rzt[bi % 2]
        yt_, yt_r = ytm[bi % 2]
        if NA_STOP in (31, 32):
            continue
        for half in range(2):
            ob, obr = Ob[half]
            ov = ob[:].rearrange("p (h d) -> p h d", h=4)
            fw.op("dve", lambda e, ov=ov, half=half, rz_=rz_: e.reciprocal(rz_[:, half * 4:half * 4 + 4], ov[:, :, 64]), reads=[obr], writes=[rz_r])
            fw.op("dve", lambda e, ov=ov, half=half, rz_=rz_, yt_=yt_: e.tensor_tensor(
                yt_[:, half * 4:half * 4 + 4, :], ov[:, :, 0:64], rz_[:, half * 4:half * 4 + 4].unsqueeze(2).to_broadcast([128, 4, 64]), ALU.mult),
                reads=[obr, rz_r], writes=[yt_r])
        if NA_STOP == 33:
            continue
        tp, tpr = P.bank(2 * (ci % 2))
        ytf = yt_[:].rearrange("p h d -> p (h d)")
        for c in range(4):
            fw.op("pe", lambda e, c=c: e.matmul(tp[:, c * 128:(c + 1) * 128], ytf[:, c * 128:(c + 1) * 128], identb[:], start=True, stop=True),
                  reads=[yt_r, identbr], writes=[tpr])
        ys_, ys_r = ystg[bi % 2]
        fw.op("act", lambda e: e.copy(ys_[:].rearrange("p c t -> p (c t)"), tp[:]), reads=[tpr], writes=[ys_r])
        fw.dma("sp", yT.ap()[2].rearrange("(c p) t -> p c t", p=128)[:, :, qt0:qt0 + 128], ys_[:], reads=[ys_r], writes=[yTr])
    P.phase_end()


MERGE_SB = [[(0, 256, 1), (256, 512, 0)], [(768, 512, 0), (1280, 256, 0)], [(1536, 512, 0), (2048, 256, 0)]]


def ph_merge(P, l, ctx_out):
    fw = P.fw
    P.phase_begin()
    sm, smr = P.C["sm"]
    hT, hTr = P.dr["hT"]
    yT, yTr = P.dr["yT"]
    xT, xTr = P.dr["xT"]
    hv = hT.ap().rearrange("(fc p) t -> p fc t", p=128)
    yv = yT.ap().rearrange("n (c p) t -> p (n c) t", p=128)
    wg = P.dr["w_gate"][0].ap()
    wbr = P.dr["w_branch"][0].ap()
    wo = P.dr["w_o"][0].ap()
    h, hr = P.sb("g_h", [128, 16, 768], BF16)
    y, yr = P.sb("g_y", [128, 16, 768], BF16)
    m, mr = P.sb("g_m", [128, 16, 768], BF16)
    Wg = [P.sb("g_wg%d" % i, [128, 4, 16, 128], BF16) for i in range(2)]
    Wb = [P.sb("g_wb%d" % i, [128, 4, 4, 128], BF16) for i in range(2)]
    Wo = [P.sb("g_wo%d" % i, [128, 16, 128], BF16) for i in range(2)]
    gs = [P.sb("g_gs%d" % i, [128, 512], F32) for i in range(3)]
    tm = [P.sb("g_tm%d" % i, [128, 512], F32) for i in range(3)]
    mac = [P.sb("g_mac%d" % i, [128, 512], F32) for i in range(2)]
    xs = [P.sb("g_xs%d" % i, [128, 512], F32) for i in range(6)]
    bo = SM["b_gate"][0]
    wi = 0
    gi = 0
    xi = 0
    for sbi, subs in enumerate(MERGE_SB):
        s0 = subs[0][0]
        subs = [s_ for s_ in subs if (s_[2] == 0 or ctx_out)]
        for q4 in range(4):
            fw.dma("sp", h[:, q4 * 4:(q4 + 1) * 4, :], hv[:, q4 * 4:(q4 + 1) * 4, s0:s0 + 768], reads=[hTr], writes=[hr])
            fw.dma("sp", y[:, q4 * 4:(q4 + 1) * 4, :], yv[:, q4 * 4:(q4 + 1) * 4, s0:s0 + 768], reads=[yTr], writes=[yr])
        def _loadw(fc, slot):
            wgt, wgr = Wg[slot % 2]
            wbt, wbr_ = Wb[slot % 2]
            for n in range(4):
                fw.dma("pool", wgt[:, n], wg[l, n, :, fc * 128:(fc + 1) * 128].rearrange("(kc p) c -> p kc c", p=128), writes=[wgr])
            fw.dma("pool", wbt[:], wbr[l, :, :, fc * 128:(fc + 1) * 128].rearrange("n (kc p) c -> p n kc c", p=128), writes=[wbr_])

        def _loadwo(fo):
            wot, wor = Wo[fo % 2]
            fw.dma("pool", wot[:], wo[l, :, fo * 128:(fo + 1) * 128].rearrange("(kc p) c -> p kc c", p=128), writes=[wor])

        _loadw(0, wi)
        for fc in range(16):
            wgt, wgr = Wg[wi % 2]
            wbt, wbr_ = Wb[wi % 2]
            wi += 1
            if fc + 1 < 16:
                _loadw(fc + 1, wi)
            else:
                _loadwo(0)
            for (t0, w, j) in subs:
                c0 = t0 - s0
                ma, mar = mac[gi % 2]
                for n in range(4):
                    pg, pgr = P.bank()
                    for kc in range(16):
                        fw.op("pe", lambda e, pg=pg, n=n, kc=kc, wgt=wgt: e.matmul(pg[:, 0:w], wgt[:, n, kc, :], h[:, kc, c0:c0 + w], start=(kc == 0), stop=(kc == 15)),
                              reads=[wgr, hr], writes=[pgr])
                    g_, g_r = gs[gi % 3]
                    fw.op("act", lambda e, g_=g_, pg=pg, n=n: e.activation(out=g_[:, 0:w], in_=pg[:, 0:w], func=AF.Sigmoid, bias=sm[:, l, bo + n * 16 + fc:bo + n * 16 + fc + 1]),
                          reads=[pgr, smr], writes=[g_r])
                    pb, pbr = P.bank()
                    for kc in range(4):
                        fw.op("pe", lambda e, pb=pb, n=n, kc=kc, wbt=wbt: e.matmul(pb[:, 0:w], wbt[:, n, kc, :], y[:, n * 4 + kc, c0:c0 + w], start=(kc == 0), stop=(kc == 3)),
                              reads=[wbr_, yr], writes=[pbr])
                    if n == 0:
                        fw.op("dve", lambda e, ma=ma, g_=g_, pb=pb: e.tensor_tensor(ma[:, 0:w], g_[:, 0:w], pb[:, 0:w], ALU.mult), reads=[g_r, pbr], writes=[mar])
                    else:
                        t_, t_r = tm[gi % 3]
                        fw.op("dve", lambda e, t_=t_, g_=g_, pb=pb: e.tensor_tensor(t_[:, 0:w], g_[:, 0:w], pb[:, 0:w], ALU.mult), reads=[g_r, pbr], writes=[t_r])
                        if n < 3:
                            fw.op("dve", lambda e, ma=ma, t_=t_: e.tensor_tensor(ma[:, 0:w], ma[:, 0:w], t_[:, 0:w], ALU.add), reads=[t_r, mar], writes=[mar])
                        else:
                            fw.op("dve", lambda e, ma=ma, t_=t_: e.tensor_tensor(m[:, fc, c0:c0 + w], ma[:, 0:w], t_[:, 0:w], ALU.add), reads=[t_r, mar], writes=[mr])
                    gi += 1
        for fo in range(16):
            wot, wor = Wo[fo % 2]
            if fo + 1 < 16:
                _loadwo(fo + 1)
            for (t0, w, j) in subs:
                c0 = t0 - s0
                po, por = P.bank()
                for kc in range(16):
                    fw.op("pe", lambda e, po=po, kc=kc, wot=wot: e.matmul(po[:, 0:w], wot[:, kc, :], m[:, kc, c0:c0 + w], start=(kc == 0), stop=(kc == 15)),
                          reads=[wor, mr], writes=[por])
                x_, x_r = xs[xi % 6]
                xi += 1
                xres = Res("xtile")
                fw.dma("sp", x_[:, 0:w], xT.ap()[fo * 128:(fo + 1) * 128, t0:t0 + w], reads=[xres], writes=[x_r])
                fw.op("dve", lambda e, x_=x_, po=po, fo=fo, j=j: e.scalar_tensor_tensor(x_[:, 0:w], po[:, 0:w], P.modv[:, l, 2, fo, j:j + 1], x_[:, 0:w], ALU.mult, ALU.add),
                      reads=[por, x_r, P.modr], writes=[x_r])
                fw.dma("pool", xT.ap()[fo * 128:(fo + 1) * 128, t0:t0 + w], x_[:, 0:w], reads=[x_r], writes=[xres])
    P.phase_end()


def ph_transpose_out(P):
    fw = P.fw
    P.phase_begin()
    ident, identr = P.C["ident"]
    xT, xTr = P.dr["xT"]
    yo, yor = P.dr["y"]
    xTv = xT.ap().rearrange("(fc p) t -> p fc t", p=128)
    xin = [P.sb("to_x%d" % i, [128, 16, 128], F32) for i in range(2)]
    stg = [P.sb("to_s%d" % i, [128, 2048], F32) for i in range(2)]
    for tb in range(16):
        xt, xr = xin[tb % 2]
        fw.dma("sp", xt[:], xTv[:, :, L + tb * 128:L + (tb + 1) * 128], reads=[xTr], writes=[xr])
        st, sr = stg[tb % 2]
        for f4 in range(4):
            pst, psr = P.bank()
            for q in range(4):
                fc = f4 * 4 + q
                fw.op("pe", lambda e, pst=pst, q=q, xt=xt, fc=fc: e.transpose(pst[:, q * 128:(q + 1) * 128], xt[:, fc, :], ident[:]),
                      reads=[xr, identr], writes=[psr])
            if f4 % 2 == 0:
                fw.op("dve", lambda e, st=st, pst=pst, f4=f4: e.tensor_copy(st[:, f4 * 512:(f4 + 1) * 512], pst[:]), reads=[psr], writes=[sr])
            else:
                fw.op("act", lambda e, st=st, pst=pst, f4=f4: e.copy(st[:, f4 * 512:(f4 + 1) * 512], pst[:]), reads=[psr], writes=[sr])
        fw.dma("sp", yo.ap()[tb * 128:(tb + 1) * 128, :], st[:], reads=[sr], writes=[yor])
    P.phase_end()


def ph_peer_q(P, l):
    fw = P.fw
    P.phase_begin()
    hT, hTr = P.dr["hT"]
    qT, qTr = P.dr["qT"]
    h, hr = P.sb("pq_h", [128, 16, NT], BF16)
    hv = hT.ap().rearrange("(fc p) t -> p fc t", p=128)
    for q4 in range(4):
        fw.dma("sp", h[:, q4 * 4:(q4 + 1) * 4, :], hv[:, q4 * 4:(q4 + 1) * 4, :], reads=[hTr], writes=[hr])
    wbuf = [P.sb("pq_w%d" % i, [128, 16, 512], BF16) for i in range(2)]
    stg = [P.sb("pq_s%d" % i, [128, NT], BF16) for i in range(2)]
    wq = P.dr["peer_w_query"][0].ap()
    ev = 0
    for cg in range(4):
        wt, wr = wbuf[cg % 2]
        fw.dma("pool", wt[:], wq[l, :, cg * 512:(cg + 1) * 512].rearrange("(kc p) n -> p kc n", p=128), writes=[wr])
        for oc in range(4):
            st, sr = stg[oc % 2]
            for (t0, w, j) in TG:
                pst, psr = P.bank()
                for kc in range(16):
                    fw.op("pe", lambda e, pst=pst, kc=kc, wt=wt, oc=oc: e.matmul(pst[:, 0:w], wt[:, kc, oc * 128:(oc + 1) * 128], h[:, kc, t0:t0 + w],
                                                                               start=(kc == 0), stop=(kc == 15)), reads=[hr, wr], writes=[psr])
                if ev % 2 == 0:
                    fw.op("dve", lambda e, st=st, pst=pst: e.tensor_copy(st[:, t0:t0 + w], pst[:, 0:w]), reads=[psr], writes=[sr])
                else:
                    fw.op("act", lambda e, st=st, pst=pst: e.copy(st[:, t0:t0 + w], pst[:, 0:w]), reads=[psr], writes=[sr])
                ev += 1
            r0 = cg * 512 + oc * 128
            fw.dma("sp", qT.ap()[r0:r0 + 128, :], st[:], reads=[sr], writes=[qTr])
    P.phase_end()


def ph_peer_gate(P, l, ctx_out):
    fw = P.fw
    P.phase_begin()
    identb, identbr = P.C["ident_bf"]
    qT, qTr = P.dr["qT"]
    GdT, GdTr = P.dr["GdT"]
    qv = qT.ap().rearrange("(c p) t -> p c t", p=128)
    gv = GdT.ap().rearrange("(i j) t -> j i t", j=128)
    skn, sknr = P.sb("pg_skn", [128, 16, 128], BF16)
    fw.dma("pool", skn[:], P.dr["peer_sub_keys"][0].ap()[l].rearrange("h s k d -> k (h s) d"), writes=[sknr])
    SK, SKr = P.sb("pg_sk", [128, 16, 128], BF16)
    for b4 in range(4):
        pst, psr = P.bank()
        for q_ in range(4):
            c = b4 * 4 + q_
            fw.op("pe", lambda e, pst=pst, q_=q_, c=c: e.matmul(pst[:, q_ * 128:(q_ + 1) * 128], skn[:, c, :], identb[:], start=True, stop=True),
                  reads=[sknr, identbr], writes=[psr])
        fw.op("act", lambda e, pst=pst, b4=b4: e.copy(SK[:, b4 * 4:(b4 + 1) * 4, :].rearrange("p c k -> p (c k)"), pst[:]), reads=[psr], writes=[SKr])
    qts = [P.sb("pg_qt%d" % i, [128, 16, 128], BF16) for i in range(2)]
    ssbs = [P.sb("pg_s%d" % i, [128, 16, 128], F32) for i in range(2)]
    thrs = [P.sb("pg_thr%d" % i, [128, 8, 128], F32) for i in range(2)]
    lncs = [P.sb("pg_lnc%d" % i, [128, 32], F32) for i in range(2)]
    s2, s2r = P.sb("pg_s2", [128, 16, 128], F32)
    top, topr = P.sb("pg_top", [128, 16, 16], F32)
    cand, candr = P.sb("pg_cand", [128, 8, 256], F32)
    cand2, cand2r = P.sb("pg_cand2", [128, 8, 256], F32)
    c8, c8r = P.sb("pg_c8", [128, 8, 16], F32)
    selm, selmr = P.sb("pg_selm", [128, 8, 16], F32)
    zz, zzr = P.sb("pg_z", [128, 16], F32)
    Zt = [P.sb("pg_Z%d" % i, [128, 8, 128], F32) for i in range(6)]
    Mt = [P.sb("pg_M%d" % i, [128, 8, 128], BF16) for i in range(4)]
    Tt = [P.sb("pg_T%d" % i, [128, 8, 128], BF16) for i in range(5)]
    gst = [P.sb("pg_gst%d" % i, [128, 128, 128], BF16) for i in range(1)]
    tbs = list(range(NTB)) if ctx_out else list(range(2, NTB))
    if GATE_TBS is not None:
        tbs = GATE_TBS

    def prologue(ti):
        tb = tbs[ti]
        qt, qtr = qts[ti % 2]
        ssb, ssbr = ssbs[ti % 2]
        thr, thrr = thrs[ti % 2]
        lnc, lncr = lncs[ti % 2]
        fw.dma("sp", qt[:], qv[:, :, tb * 128:(tb + 1) * 128], reads=[qTr], writes=[qtr])
        yield
        for b4 in range(4):
            pst, psr = P.bank()
            for q_ in range(4):
                c = b4 * 4 + q_
                fw.op("pe", lambda e, pst=pst, q_=q_, c=c, qt=qt: e.matmul(pst[:, q_ * 128:(q_ + 1) * 128], qt[:, c, :], SK[:, c, :], start=True, stop=True),
                      reads=[qtr, SKr], writes=[psr])
            dst = ssb[:, b4 * 4:(b4 + 1) * 4, :].rearrange("p c k -> p (c k)")
            fw.op("dve", lambda e, pst=pst, dst=dst: e.tensor_copy(dst, pst[:]), reads=[psr], writes=[ssbr])
            yield
        for c in range(16):
            fw.op("dve", lambda e, c=c: e.max(out=top[:, c, 0:8], in_=ssb[:, c, :]), reads=[ssbr], writes=[topr])
            fw.op("dve", lambda e, c=c: e.match_replace(out=s2[:, c, :], in_to_replace=top[:, c, 0:8], in_values=ssb[:, c, :], imm_value=-1e30),
                  reads=[ssbr, topr], writes=[s2r])
            fw.op("dve", lambda e, c=c: e.max(out=top[:, c, 8:16], in_=s2[:, c, :]), reads=[s2r], writes=[topr])
            yield
        tv = top[:].rearrange("p (h s) a -> p h s a", s=2)
        fw.op("dve", lambda e: e.tensor_tensor(cand[:].rearrange("p h (a b) -> p h a b", a=16),
                                             tv[:, :, 0, :].unsqueeze(3).to_broadcast([128, 8, 16, 16]),
                                             tv[:, :, 1, :].unsqueeze(2).to_broadcast([128, 8, 16, 16]), ALU.add), reads=[topr], writes=[candr])
        yield
        for hh in range(8):
            fw.op("dve", lambda e, hh=hh: e.max(out=c8[:, hh, 0:8], in_=cand[:, hh, :]), reads=[candr], writes=[c8r])
            fw.op("dve", lambda e, hh=hh: e.match_replace(out=cand2[:, hh, :], in_to_replace=c8[:, hh, 0:8], in_values=cand[:, hh, :], imm_value=-1e30),
                  reads=[candr, c8r], writes=[cand2r])
            fw.op("dve", lambda e, hh=hh: e.max(out=c8[:, hh, 8:16], in_=cand2[:, hh, :]), reads=[cand2r], writes=[c8r])
            yield
        fw.op("dve", lambda e: e.tensor_tensor(selm[:], c8[:], c8[:, :, 0:1].to_broadcast([128, 8, 16]), ALU.subtract), reads=[c8r], writes=[selmr])
        fw.op("act", lambda e: e.activation(out=selm[:], in_=selm[:], func=AF.Exp), reads=[selmr], writes=[selmr])
        yield
        fw.op("dve", lambda e: e.tensor_reduce(zz[:, 0:8], selm[:], AX.X, ALU.add), reads=[selmr], writes=[zzr])
        sv = ssb[:].rearrange("p (h s) k -> p h s k", s=2)
        fw.op("dve", lambda e: e.tensor_tensor(thr[:], sv[:, :, 0, :], c8[:, :, 15:16].to_broadcast([128, 8, 128]), ALU.subtract),
              reads=[ssbr, c8r], writes=[thrr])
        yield
        fw.op("act", lambda e: e.activation(out=lnc[:, 0:8], in_=zz[:, 0:8], func=AF.Ln), reads=[zzr], writes=[lncr])
        fw.op("dve", lambda e: e.tensor_tensor(lnc[:, 8:16], c8[:, :, 15], c8[:, :, 0], ALU.subtract), reads=[c8r], writes=[lncr])
        yield
        fw.op("dve", lambda e: e.tensor_tensor(lnc[:, 16:24], lnc[:, 8:16], lnc[:, 0:8], ALU.subtract), reads=[lncr], writes=[lncr])
        fw.op("dve", lambda e: e.tensor_scalar(lnc[:, 24:32], lnc[:, 16:24], -5e-6, None, ALU.add), reads=[lncr], writes=[lncr])
        yield

    steps = [(ig, hh) for ig in range(16) for hh in range(8)]
    nst = len(steps)
    for _ in prologue(0):
        pass
    for ti, tb in enumerate(tbs):
        ssb, ssbr = ssbs[ti % 2]
        thr, thrr = thrs[ti % 2]
        lnc, lncr = lncs[ti % 2]
        sv = ssb[:].rearrange("p (h s) k -> p h s k", s=2)
        nxt = prologue(ti + 1) if ti + 1 < len(tbs) else None
        gs_, gs_r = gst[0]
        zb = {}
        eb = {}
        tbuf = {}

        def _A(si):
            ig, hh = steps[si]
            z_, z_r = Zt[si % len(Zt)]
            fw.op("dve", lambda e: e.tensor_tensor(
                z_[:], sv[:, hh, 1, :].unsqueeze(1).to_broadcast([128, 8, 128]),
                thr[:, hh, ig * 8:(ig + 1) * 8].unsqueeze(2).to_broadcast([128, 8, 128]), ALU.add), reads=[ssbr, thrr], writes=[z_r], inc=True)
            zb[si] = (z_, z_r)

        def _B(si):
            ig, hh = steps[si]
            z_, z_r = zb[si]
            if si % 4 != 3:
                T_, T_r = Tt[si % 5]
                fw.op("act", lambda e: e.activation(out=z_[:], in_=z_[:], func=AF.Prelu, alpha=1e5, bias=5e-6), reads=[z_r], writes=[z_r], inc=True)
                fw.op("act", lambda e: e.activation(out=T_[:], in_=z_[:], func=AF.Exp, bias=lnc[:, 24 + hh:25 + hh]), reads=[z_r, lncr], writes=[T_r], inc=True)
                tbuf[si] = (T_, T_r)
                zb.pop(si)
                return
            e_, e_r = Mt[si % 4]
            fw.op("act", lambda e: e.activation(out=e_[:], in_=z_[:], func=AF.Exp, bias=lnc[:, 16 + hh:17 + hh]), reads=[z_r, lncr], writes=[e_r], inc=True)
            eb[si] = (e_, e_r)

        def _C(si):
            if si % 4 != 3:
                return
            z_, z_r = zb.pop(si)
            e_, e_r = eb.pop(si)
            T_, T_r = Tt[si % 5]
            fw.op("dve", lambda e: e.scalar_tensor_tensor(T_[:], z_[:], -5e-6, e_[:], ALU.is_ge, ALU.mult), reads=[z_r, e_r], writes=[T_r], inc=True)
            tbuf[si] = (T_, T_r)

        banks = None
        _A(0)
        _A(1)
        _A(2)
        _B(0)
        _B(1)
        for si in range(nst):
            ig, hh = steps[si]
            if si + 3 < nst:
                _A(si + 3)
            if si + 2 < nst:
                _B(si + 2)
            _C(si)
            if hh == 0:
                banks = [P.bank(), P.bank()]
            T_, T_r = tbuf.pop(si)
            for ii in range(8):
                bk, bkr = banks[ii // 4]
                fw.op("pe", lambda e, bk=bk, ii=ii, T_=T_, hh=hh: e.matmul(
                    bk[:, (ii % 4) * 128:(ii % 4 + 1) * 128], T_[:, ii, :], identb[:],
                    start=(hh == 0 and ii % 4 == 0), stop=(hh == 7 and ii % 4 == 3), skip_group_check=True), reads=[T_r, identbr], writes=[bkr])
            if hh == 7:
                for b in range(2):
                    bk, bkr = banks[b]
                    fw.op("act", lambda e, bk=bk, b=b, ig=ig: e.copy(gs_[:, ig * 8 + b * 4:ig * 8 + b * 4 + 4, :].rearrange("p i t -> p (i t)"), bk[:]),
                          reads=[bkr], writes=[gs_r])
            if nxt is not None and si >= 8 and hh in (2, 5):
                next(nxt, None)
        if nxt is not None:
            for _ in nxt:
                pass
        for q4 in range(4):
            fw.dma("sp", gv[:, q4 * 32:(q4 + 1) * 32, tb * 128:(tb + 1) * 128], gs_[:, q4 * 32:(q4 + 1) * 32, :], reads=[gs_r], writes=[GdTr])
    P.phase_end()


def ph_peer_u(P, l, ctx_out):
    fw = P.fw
    P.phase_begin()
    identb, identbr = P.C["ident_bf"]
    hT, hTr = P.dr["hT"]
    GdT, GdTr = P.dr["GdT"]
    GAT, GATr = P.dr["GAT"]
    pu = P.dr["peer_u"][0].ap()
    h, hr = P.sb("pu_h", [128, 16, NT], BF16)
    hv = hT.ap().rearrange("(fc p) t -> p fc t", p=128)
    for q4 in range(4):
        fw.dma("sp", h[:, q4 * 4:(q4 + 1) * 4, :], hv[:, q4 * 4:(q4 + 1) * 4, :], reads=[hTr], writes=[hr])
    Ur = [P.sb("pu_ur%d" % i, [128, 2048], BF16) for i in range(2)]
    UT = [P.sb("pu_ut%d" % i, [128, 16, 128], BF16) for i in range(2)]
    gt = [P.sb("pu_g%d" % i, [128, NT], BF16) for i in range(2)]
    at = [P.sb("pu_a%d" % i, [128, 512], BF16) for i in range(3)]
    st = [P.sb("pu_s%d" % i, [128, NT], BF16) for i in range(2)]
    groups = [g for g in TG if (g[2] == 0 or ctx_out)]
    c_lo = 0 if ctx_out else L
    ai = 0
    def _load(ec):
        ur, urr = Ur[ec % 2]
        fw.dma("pool", ur[:], pu[l, ec * 128:(ec + 1) * 128, :], writes=[urr])
        g_, g_r = gt[ec % 2]
        fw.dma("sp", g_[:, c_lo:NT], GdT.ap()[ec * 128:(ec + 1) * 128, c_lo:NT], reads=[GdTr], writes=[g_r])

    _load(0)
    for ec in range(128):
        if ec + 1 < 128:
            _load(ec + 1)
        ur, urr = Ur[ec % 2]
        g_, g_r = gt[ec % 2]
        ut, utr = UT[ec % 2]
        for b4 in range(4):
            pst, psr = P.bank()
            for q_ in range(4):
                kc = b4 * 4 + q_
                fw.op("pe", lambda e, pst=pst, q_=q_, kc=kc, ur=ur: e.matmul(pst[:, q_ * 128:(q_ + 1) * 128], ur[:, kc * 128:(kc + 1) * 128], identb[:], start=True, stop=True),
                      reads=[urr, identbr], writes=[psr])
            dst = ut[:, b4 * 4:(b4 + 1) * 4, :].rearrange("p c k -> p (c k)")
            if b4 % 2 == 0:
                fw.op("dve", lambda e, pst=pst, dst=dst: e.tensor_copy(dst, pst[:]), reads=[psr], writes=[utr])
            else:
                fw.op("pool", lambda e, pst=pst, dst=dst: e.tensor_copy(dst, pst[:]), reads=[psr], writes=[utr]) if False else \
                    fw.op("dve", lambda e, pst=pst, dst=dst: e.tensor_copy(dst, pst[:]), reads=[psr], writes=[utr])
        s_, s_r = st[ec % 2]
        for (t0, w, j) in groups:
            pst, psr = P.bank()
            for kc in range(16):
                fw.op("pe", lambda e, pst=pst, kc=kc, ut=ut: e.matmul(pst[:, 0:w], ut[:, kc, :], h[:, kc, t0:t0 + w], start=(kc == 0), stop=(kc == 15)),
                      reads=[utr, hr], writes=[psr])
            a_, a_r = at[ai % 3]
            ai += 1
            fw.op("act", lambda e, a_=a_, pst=pst: e.activation(out=a_[:, 0:w], in_=pst[:, 0:w], func=AF.Gelu), reads=[psr], writes=[a_r])
            fw.op("pool", lambda e, a_=a_, s_=s_, g_=g_: e.tensor_tensor(s_[:, t0:t0 + w], a_[:, 0:w], g_[:, t0:t0 + w], ALU.mult), reads=[a_r, g_r], writes=[s_r])
        fw.dma("sp", GAT.ap()[ec * 128:(ec + 1) * 128, c_lo:NT], s_[:, c_lo:NT], reads=[s_r], writes=[GATr])
    P.phase_end()


def ph_peer_v(P, l, ctx_out):
    fw = P.fw
    P.phase_begin()
    GAT, GATr = P.dr["GAT"]
    xT, xTr = P.dr["xT"]
    pv = P.dr["peer_v"][0].ap()
    acc, accr = P.sb("pv_acc", [128, 8, NT], F32)
    ares = [[Res("acc%d_%d" % (dc, gi)) for gi in range(len(TG))] for dc in range(8)]
    GA = [P.sb("pv_ga%d" % i, [128, 4, NT], BF16) for i in range(2)]
    Vw = [P.sb("pv_v%d" % i, [128, 4, 1024], BF16) for i in range(2)]
    xs = [P.sb("pv_x%d" % i, [128, 512], F32) for i in range(6)]
    groups = [(gi, g) for gi, g in enumerate(TG) if (g[2] == 0 or ctx_out)]
    c_lo = 0 if ctx_out else L
    gav = GAT.ap().rearrange("(g e p) t -> g p e t", p=128, e=4)
    pvv = pv[l].rearrange("(g e p) d -> g p e d", p=128, e=4)
    xi = 0
    ev = 0
    for dh in range(2):
        for g in range(32):
            ga, gar = GA[g % 2]
            vw, vwr = Vw[g % 2]
            fw.dma("sp", ga[:, :, c_lo:NT], gav[g][:, :, c_lo:NT], reads=[GATr], writes=[gar])
            fw.dma("pool", vw[:], pvv[g][:, :, dh * 1024:(dh + 1) * 1024], writes=[vwr])
            for dc in range(8):
                for gi, (t0, w, j) in groups:
                    pst, psr = P.bank()
                    for e4 in range(4):
                        fw.op("pe", lambda e, pst=pst, e4=e4, vw=vw, ga=ga, dc=dc: e.matmul(pst[:, 0:w], vw[:, e4, dc * 128:(dc + 1) * 128], ga[:, e4, t0:t0 + w],
                                                                                          start=(e4 == 0), stop=(e4 == 3)), reads=[vwr, gar], writes=[psr])
                    ar = ares[dc][gi]
                    eng = "dve" if ev % 2 == 0 else "pool"
                    ev += 1
                    if g == 0:
                        if eng == "dve":
                            fw.op("dve", lambda e, pst=pst, dc=dc: e.tensor_copy(acc[:, dc, t0:t0 + w], pst[:, 0:w]), reads=[psr], writes=[ar])
                        else:
                            fw.op("act", lambda e, pst=pst, dc=dc: e.copy(acc[:, dc, t0:t0 + w], pst[:, 0:w]), reads=[psr], writes=[ar])
                    else:
                        fw.op("dve", lambda e, pst=pst, dc=dc: e.tensor_tensor(acc[:, dc, t0:t0 + w], acc[:, dc, t0:t0 + w], pst[:, 0:w], ALU.add),
                              reads=[psr, ar], writes=[ar])
        for dc in range(8):
            fo = dh * 8 + dc
            for gi, (t0, w, j) in groups:
                x_, x_r = xs[xi % 6]
                xi += 1
                xres = Res("xtile")
                fw.dma("sp", x_[:, 0:w], xT.ap()[fo * 128:(fo + 1) * 128, t0:t0 + w], reads=[xres], writes=[x_r])
                fw.op("dve", lambda e, x_=x_, dc=dc, fo=fo, j=j: e.scalar_tensor_tensor(x_[:, 0:w], acc[:, dc, t0:t0 + w], P.modv[:, l, 5, fo, j:j + 1], x_[:, 0:w], ALU.mult, ALU.add),
                      reads=[ares[dc][gi], x_r, P.modr], writes=[x_r])
                fw.dma("pool", xT.ap()[fo * 128:(fo + 1) * 128, t0:t0 + w], x_[:, 0:w], reads=[x_r], writes=[xres])
    P.phase_end()


_PROG = {}


def kernel(**inputs):
    inp = {k: np.asarray(v) for k, v in inputs.items()}
    if "p" not in _PROG:
        _PROG["p"] = build_program()
    P = _PROG["p"]
    consts = build_consts()
    smalls = np.stack([pack_smalls(inp, l) for l in range(DEPTH)])
    extra = extra_host_inputs(inp)
    in_maps = []
    for b in range(8):
        m = host_inputs(inp, b, consts, smalls)
        m.update(extra)
        in_maps.append({k: np.ascontiguousarray(m[k]) for k in P.inputs})
    res = run_bass_kernel_spmd(P.nc, in_maps, core_ids=list(range(8)))
    return np.stack([np.asarray(res.results[b]["y"]) for b in range(8)]).astype(np.float32)
```

```python
import bisect
import math
from contextlib import ExitStack

import numpy as np
import concourse.bass as bass
import concourse.mybir as mybir
from concourse.bass_utils import run_bass_kernel_spmd

F32 = mybir.dt.float32
BF16 = mybir.dt.bfloat16
AF = mybir.ActivationFunctionType
ALU = mybir.AluOpType
AX = mybir.AxisListType

D = 2048
T = 2048
L = 256
NT = T + L
NTB = NT // 128
DEPTH = 2
INW = 5952
EPS = 1e-6
TG = [(0, 256, 1)] + [(256 + 512 * i, 512, 0) for i in range(4)]
NEG = -30000.0
GATE_TBS = None
ATTN_WARM = 0


class Res:
    __slots__ = ("name", "w", "r")

    def __init__(self, name=""):
        self.name = name
        self.w = None
        self.r = {}


class FW:
    NDS = 24

    def __init__(self, nc, es, same_engine_sync=True):
        self.nc = nc
        self.E = {"pe": nc.tensor, "dve": nc.vector, "act": nc.scalar, "pool": nc.gpsimd, "sp": nc.sync}
        self.csem = {e: es.enter_context(nc.semaphore("c_" + e)) for e in ("pe", "dve", "act", "pool")}
        self.seq = {e: 0 for e in self.csem}
        self.last = {e: None for e in self.csem}
        self.incd = {e: ([], []) for e in self.csem}
        self.dsems = [es.enter_context(nc.semaphore("d%d" % i)) for i in range(self.NDS)]
        self.dcnt = [0] * self.NDS
        self.qsl = {"sp": (0, 14), "pool": (14, 24)}
        self.dnext = {"sp": 0, "pool": 14}
        self.seen = {e: {} for e in self.E}
        self.same = same_engine_sync
        self.nwait = 0
        self.nins = 0

    def _resolve(self, y, seq):
        seqs, counts = self.incd[y]
        i = bisect.bisect_left(seqs, seq)
        if i < len(seqs):
            return seqs[i], counts[i]
        h = self.last[y]
        cnt = (counts[-1] if counts else 0) + 1
        h.then_inc(self.csem[y], 1)
        seqs.append(self.seq[y])
        counts.append(cnt)
        return self.seq[y], cnt

    def _wait(self, eng, tok):
        if tok[0] == "c":
            _, y, seq = tok
            if y == eng and (not self.same or eng == "pe"):
                return
            key = ("c", y)
            if self.seen[eng].get(key, 0) >= seq:
                return
            s2, cnt = self._resolve(y, seq)
            self.E[eng].wait_ge(self.csem[y], cnt)
            self.seen[eng][key] = s2
        else:
            _, i, cnt = tok
            key = ("d", i)
            if self.seen[eng].get(key, 0) >= cnt:
                return
            self.E[eng].wait_ge(self.dsems[i], cnt)
            self.seen[eng][key] = cnt
        self.nwait += 1

    def _deps(self, eng, reads, writes):
        for r in reads:
            if r.w is not None:
                self._wait(eng, r.w)
        for w in writes:
            if w.w is not None:
                self._wait(eng, w.w)
            for k, v in w.r.items():
                self._wait(eng, (k[0], k[1], v))

    def op(self, eng, fn, reads=(), writes=(), inc=False):
        self._deps(eng, reads, writes)
        ins = fn(self.E[eng])
        self.nins += 1
        self.seq[eng] += 1
        self.last[eng] = ins
        s = self.seq[eng]
        if inc or eng != "pe":
            seqs, counts = self.incd[eng]
            ins.then_inc(self.csem[eng], 1)
            seqs.append(s)
            counts.append((counts[-1] if counts else 0) + 1)
        for r in reads:
            r.r[("c", eng)] = s
        for w in writes:
            w.w = ("c", eng, s)
            w.r = {}
        return ins

    def dma(self, q, out, in_, reads=(), writes=(), **kw):
        self._deps(q, reads, writes)
        i = self.dnext[q]
        lo, hi = self.qsl[q]
        self.dnext[q] = lo + (i + 1 - lo) % (hi - lo)
        self.dcnt[i] += 16
        self.E[q].dma_start(out=out, in_=in_, **kw).then_inc(self.dsems[i], 16)
        self.nins += 1
        for r in reads:
            r.r[("d", i)] = self.dcnt[i]
        for w in writes:
            w.w = ("d", i, self.dcnt[i])
            w.r = {}

    def barrier(self, engines=("sp", "pe", "dve", "act", "pool")):
        for eng in engines:
            for i in range(self.NDS):
                if self.dcnt[i]:
                    self._wait(eng, ("d", i, self.dcnt[i]))
            for y in self.csem:
                if self.seq[y] and y != eng:
                    self._wait(eng, ("c", y, self.seq[y]))


SM = {}
_o = 0
for _n, _w in [("norm1_g", 16), ("norm2_g", 16), ("b_gate", 64), ("b_mod", 96), ("dqg", 1), ("dkg", 1),
               ("dsub", 1), ("rnorm", 1), ("nqg", 1), ("nkg", 1), ("mqn", 4), ("mkvn", 2),
               ("mq_nope", 1), ("mq_rope", 1), ("mk_nope", 1), ("mk_rope", 1),
               ("dlam", 256), ("rdecay", 8)]:
    SM[_n] = (_o, _w)
    _o += _w
NSM = _o


def _cols(v):
    v = np.asarray(v, np.float32).reshape(-1, 128)
    return np.ascontiguousarray(v.T)


def pack_smalls(inp, l):
    s = np.zeros((128, NSM), np.float32)

    def put(name, arr):
        o, w = SM[name]
        arr = np.asarray(arr, np.float32).reshape(128, w)
        s[:, o:o + w] = arr

    put("norm1_g", _cols(inp["norm1_g"][l]))
    put("norm2_g", _cols(inp["norm2_g"][l]))
    put("b_gate", np.stack([_cols(inp["b_gate"][l][n]) for n in range(4)], axis=1))
    put("b_mod", _cols(inp["b_mod"][l]))
    put("dqg", np.tile(inp["diff_qk_g"][l][0], 2))
    put("dkg", np.tile(inp["diff_qk_g"][l][1], 2))
    put("dsub", inp["diff_sub_g"][l])
    put("rnorm", inp["ret_norm_g"][l])
    put("nqg", np.tile(inp["na_qk_g"][l][0], 2))
    put("nkg", np.tile(inp["na_qk_g"][l][1], 2))
    put("mqn", _cols(inp["mla_q_norm_g"][l]))
    put("mkvn", _cols(inp["mla_kv_norm_g"][l]))
    put("mq_nope", inp["mla_qk_g"][l][0][:128])
    put("mq_rope", np.tile(inp["mla_qk_g"][l][0][128:], 2))
    put("mk_nope", inp["mla_qk_g"][l][1][:128])
    put("mk_rope", np.tile(inp["mla_qk_g"][l][1][128:], 2))
    put("dlam", np.tile(inp["diff_lambda"][l].reshape(1, 256), (128, 1)))
    put("rdecay", np.tile(inp["ret_decay"][l].reshape(1, 8), (128, 1)))
    return s


class Prog:
    def __init__(self, ext_in=(), ext_out=(), same_engine_sync=True):
        self.nc = bass.Bass("TRN2", target_bir_lowering=False)
        self.es = ExitStack()
        self.fw = FW(self.nc, self.es, same_engine_sync)
        self.ext_in = set(ext_in)
        self.ext_out = set(ext_out)
        self.dr = {}
        self.inputs = []
        self.outputs = []
        nc = self.nc
        self.ps = []
        self.psr = []
        for i in range(8):
            self.ps.append(self.es.enter_context(nc.psum_tensor("ps%d" % i, [128, 512], F32)))
            self.psr.append(Res("ps%d" % i))
        self.pnext = 0
        self.pes = None

    def inp(self, name, shape, dt=F32):
        t = self.nc.dram_tensor(name, list(shape), dt, kind="ExternalInput")
        self.inputs.append(name)
        self.dr[name] = (t, Res(name))
        return t

    def scratch(self, name, shape, dt):
        if name in self.ext_in:
            kind = "ExternalInput"
            self.inputs.append(name)
        elif name in self.ext_out:
            kind = "ExternalOutput"
            self.outputs.append(name)
        else:
            kind = "Internal"
        t = self.nc.dram_tensor(name, list(shape), dt, kind=kind)
        self.dr[name] = (t, Res(name))
        return t

    def out(self, name, shape, dt=F32):
        t = self.nc.dram_tensor(name, list(shape), dt, kind="ExternalOutput")
        self.outputs.append(name)
        self.dr[name] = (t, Res(name))
        return t

    def R(self, name):
        return self.dr[name][1]

    def phase_begin(self, name=None):
        self.fw.barrier()
        self.pes = ExitStack()
        self.pidx = getattr(self, "pidx", 0) + 1
        if name is None:
            import inspect
            name = inspect.stack()[1].function
        self.pes.enter_context(self.nc.named_scope("%02d_%s" % (self.pidx, name)))

    def phase_end(self):
        self.fw.barrier()
        self.pes.close()
        self.pes = None

    def sb(self, name, shape, dt=F32, persistent=False):
        es = self.es if persistent else self.pes
        self.uid = getattr(self, "uid", 0) + 1
        t = es.enter_context(self.nc.sbuf_tensor("sb%d_%s" % (self.uid, name), list(shape), dt))
        return t, Res(name)

    def bank(self, i=None):
        if i is None:
            i = self.pnext
            self.pnext = (self.pnext + 1) % 8
        return self.ps[i], self.psr[i]


def rope_tables(n_tok, dim):
    m = dim // 2
    inv = 10000.0 ** (-np.arange(0, m, 2, dtype=np.float32) / m)
    t = np.arange(n_tok)
    row = (t // 64).astype(np.float32)
    col = (t % 64).astype(np.float32)
    ar = row[:, None] * inv
    ac = col[:, None] * inv
    ang = np.concatenate([ar, ar, ac, ac], axis=-1).astype(np.float32)
    return np.cos(ang).astype(np.float32), np.sin(ang).astype(np.float32)


def rot_matrix(dim, reps):
    q = dim // 4
    Rm = np.zeros((dim * reps, dim * reps), np.float32)
    for r in range(reps):
        o = r * dim
        for i in range(q):
            Rm[o + q + i, o + i] = -1.0
            Rm[o + i, o + q + i] = 1.0
            Rm[o + 3 * q + i, o + 2 * q + i] = -1.0
            Rm[o + 2 * q + i, o + 3 * q + i] = 1.0
    return Rm


def build_consts():
    c = {}
    c["ident"] = np.eye(128, dtype=np.float32)
    c["ones"] = np.ones((128, 128), np.float32)
    bd = np.zeros((128, 128), np.float32)
    bd[:64, :64] = 1.0
    bd[64:, 64:] = 1.0
    c["bd64"] = bd
    cos64, sin64 = rope_tables(T, 64)
    c["cos64"] = np.ascontiguousarray(np.tile(cos64.T, (2, 1)))
    c["sin64"] = np.ascontiguousarray(np.tile(sin64.T, (2, 1)))
    cos128, sin128 = rope_tables(T, 128)
    c["cos128"] = np.ascontiguousarray(cos128.T)
    c["sin128"] = np.ascontiguousarray(sin128.T)
    n = np.arange(128, dtype=np.float32)
    dif = n[None, :] - n[:, None]
    c["rett"] = np.ascontiguousarray(np.concatenate([
        np.maximum(dif, 0), (dif >= 0).astype(np.float32), np.maximum(-dif, 0), (dif <= 0).astype(np.float32),
        np.tile(n[None, :] + 1.0, (128, 1)), np.tile(128.0 - n[None, :], (128, 1)),
        (127.0 - n)[:, None], n[:, None]], axis=1).astype(np.float32))
    c["rot64"] = rot_matrix(64, 2)
    c["rot128"] = rot_matrix(128, 1)
    return c


def ph_setup(P, do_mod=True):
    fw, nc = P.fw, P.nc
    P.phase_begin()
    C = {}
    for nm in ("ident", "ones", "bd64", "rot64", "rot128"):
        t, r = P.sb("c_" + nm, [128, 128], F32, persistent=True)
        fw.dma("sp", t[:], P.dr["k_" + nm][0].ap(), writes=[r])
        tb, rb = P.sb("cb_" + nm, [128, 128], BF16, persistent=True)
        fw.op("dve", lambda e, tb=tb, t=t: e.tensor_copy(tb[:], t[:]), reads=[r], writes=[rb])
        C[nm] = (t, r)
        C[nm + "_bf"] = (tb, rb)
    sm, smr = P.sb("smalls", [128, DEPTH, NSM], F32, persistent=True)
    fw.dma("sp", sm[:], P.dr["smalls"][0].ap().rearrange("l p n -> p l n"), writes=[smr])
    C["sm"] = (sm, smr)
    P.C = C

    modv, modr = P.sb("modv", [128, DEPTH, 6, 16, 2], F32, persistent=True)
    a12, a12r = P.sb("a12", [128, DEPTH, 2, 16, 2], F32, persistent=True)
    cv, cvr = P.sb("cvec", [128, 16, 2], F32)
    fw.dma("sp", cv[:], P.dr["cvec"][0].ap(), writes=[cvr])
    sv, svr = P.sb("svec", [128, 16, 2], BF16)
    fw.op("act", lambda e: e.activation(out=sv[:], in_=cv[:], func=AF.Silu), reads=[cvr], writes=[svr])
    NB = 2
    wb = [P.sb("wmod%d" % i, [128, 3072], BF16) for i in range(NB)]
    it = 0
    for l in range(DEPTH if do_mod else 0):
        wm = P.dr["w_mod"][0].ap()
        for g in range(4):
            pst, psr = P.bank()
            for kc in range(16):
                wt, wr = wb[it % NB]
                it += 1
                fw.dma("pool", wt[:], wm[l, kc * 128:(kc + 1) * 128, g * 3072:(g + 1) * 3072], writes=[wr])
                for n in range(24):
                    fw.op("pe", lambda e, n=n, wt=wt, kc=kc, pst=pst: e.matmul(
                        pst[:, n * 2:n * 2 + 2], wt[:, n * 128:(n + 1) * 128], sv[:, kc, :],
                        start=(kc == 0 and n == 0), stop=(kc == 15 and n == 23), skip_group_check=True),
                        reads=[wr, svr], writes=[psr])
            o, w = SM["b_mod"]
            mv = modv[:, l].rearrange("p m f j -> p (m f) j")
            fw.op("dve", lambda e, g=g, pst=pst, mv=mv, l=l, o=o: e.tensor_tensor(
                mv[:, g * 24:(g + 1) * 24, :], pst[:, 0:48].rearrange("p (n j) -> p n j", j=2),
                sm[:, l, o + g * 24:o + (g + 1) * 24].unsqueeze(2).to_broadcast([128, 24, 2]), ALU.add),
                reads=[psr, smr], writes=[modr])
    for l in range(DEPTH if do_mod else 0):
        for w_, (gn, mi) in enumerate((("norm1_g", 1), ("norm2_g", 4))):
            o, _ = SM[gn]
            fw.op("dve", lambda e, l=l, w_=w_, mi=mi, o=o: e.scalar_tensor_tensor(
                a12[:, l, w_], modv[:, l, mi], 1.0, sm[:, l, o:o + 16].unsqueeze(2).to_broadcast([128, 16, 2]),
                ALU.add, ALU.mult), reads=[modr, smr], writes=[a12r])
    P.modv, P.modr, P.a12, P.a12r = modv, modr, a12, a12r
    P.phase_end()


def ph_transpose_in(P):
    fw = P.fw
    P.phase_begin()
    ident, identr = P.C["ident"]
    xT, xTr = P.dr["xT"]
    xTv = xT.ap().rearrange("(fc p) t -> p fc t", p=128)
    srcs = [(P.dr["ctx_b"], 0, 2), (P.dr["x_b"], 0, 4), (P.dr["x_b"], 4, 4), (P.dr["x_b"], 8, 4), (P.dr["x_b"], 12, 4)]
    xin = [P.sb("xin%d" % i, [128, 2048], F32) for i in range(2)]
    stg = [P.sb("xstg%d" % i, [128, 16, 512], F32) for i in range(2)]
    it = 0
    tok0 = 0
    for gi, ((src, srcr), b0, nb) in enumerate(srcs):
        st, sr = stg[gi % 2]
        for j in range(nb):
            xt, xr = xin[it % 2]
            it += 1
            fw.dma("sp", xt[:], src.ap()[(b0 + j) * 128:(b0 + j + 1) * 128, :], reads=[srcr], writes=[xr])
            for f4 in range(4):
                pst, psr = P.bank()
                for q in range(4):
                    fc = f4 * 4 + q
                    fw.op("pe", lambda e, pst=pst, q=q, xt=xt, fc=fc: e.transpose(
                        pst[:, q * 128:(q + 1) * 128], xt[:, fc * 128:(fc + 1) * 128], ident[:]),
                        reads=[xr, identr], writes=[psr])
                eng = "dve" if f4 % 2 == 0 else "act"
                dst = st[:, f4 * 4:(f4 + 1) * 4, j * 128:(j + 1) * 128]
                srcp = pst[:].rearrange("p (q t) -> p q t", q=4)
                if eng == "dve":
                    fw.op("dve", lambda e, dst=dst, srcp=srcp: e.tensor_copy(dst, srcp), reads=[psr], writes=[sr])
                else:
                    fw.op("act", lambda e, dst=dst, srcp=srcp: e.copy(dst, srcp), reads=[psr], writes=[sr])
        w = nb * 128
        fw.dma("sp", xTv[:, :, tok0:tok0 + w], st[:, :, 0:w], reads=[sr], writes=[xTr])
        tok0 += w
    P.phase_end()


def ph_norm(P, l, which, dst):
    fw = P.fw
    P.phase_begin()
    ones, onesr = P.C["ones"]
    xT, xTr = P.dr["xT"]
    hT, hTr = P.dr[dst]
    xTv = xT.ap().rearrange("(fc p) t -> p fc t", p=128)
    hTv = hT.ap().rearrange("(fc p) t -> p fc t", p=128)
    shi = 0 if which == 0 else 3

    def lane(k):
        xt, xr = P.sb("nx%d" % k, [128, 16, 512], F32)
        sqs = [P.sb("nsq%d_%d" % (k, i), [128, 512], F32) for i in range(2)]
        rs, rsr = P.sb("nrs%d" % k, [128, 512], F32)
        tmp = [P.sb("ntmp%d_%d" % (k, i), [128, 512], F32) for i in range(2)]
        ht, hr = P.sb("nh%d" % k, [128, 16, 512], BF16)
        for gi in range(k, len(TG), 2):
            t0, w, j = TG[gi]
            for q4 in range(4):
                fw.dma("sp", xt[:, q4 * 4:(q4 + 1) * 4, 0:w], xTv[:, q4 * 4:(q4 + 1) * 4, t0:t0 + w], reads=[xTr], writes=[xr])
            yield
            pst, psr = P.bank()
            for fc in range(16):
                s_, s_r = sqs[fc % 2]
                fw.op("act", lambda e, s_=s_, fc=fc: e.activation(out=s_[:, 0:w], in_=xt[:, fc, 0:w], func=AF.Square),
                      reads=[xr], writes=[s_r])
                yield
                fw.op("pe", lambda e, s_=s_, fc=fc: e.matmul(pst[:, 0:w], ones[:], s_[:, 0:w], start=(fc == 0), stop=(fc == 15)),
                      reads=[onesr, s_r], writes=[psr])
                yield
            fw.op("act", lambda e: e.activation(out=rs[:, 0:w], in_=pst[:, 0:w], func=AF.Sqrt, bias=EPS, scale=1.0 / D),
                  reads=[psr], writes=[rsr])
            yield
            fw.op("dve", lambda e: e.reciprocal(rs[:, 0:w], rs[:, 0:w]), reads=[rsr], writes=[rsr])
            yield
            for fc in range(16):
                tt, tr = tmp[fc % 2]
                fw.op("dve", lambda e, tt=tt, fc=fc: e.tensor_tensor(tt[:, 0:w], xt[:, fc, 0:w], rs[:, 0:w], ALU.mult),
                      reads=[xr, rsr], writes=[tr])
                yield
                fw.op("act", lambda e, tt=tt, fc=fc: e.activation(
                    out=ht[:, fc, 0:w], in_=tt[:, 0:w], func=AF.Identity,
                    bias=P.modv[:, l, shi, fc, j:j + 1], scale=P.a12[:, l, which, fc, j:j + 1]),
                    reads=[tr, P.modr, P.a12r], writes=[hr])
                yield
            fw.dma("sp", hTv[:, :, t0:t0 + w], ht[:, :, 0:w], reads=[hr], writes=[hTr])
            yield

    interleave([lane(0), lane(1)])
    P.phase_end()


def ph_inproj(P, l):
    fw = P.fw
    P.phase_begin()
    hT, hTr = P.dr["hT"]
    pT, pTr = P.dr["pT"]
    pV, pVr = P.dr["pV"]
    h, hr = P.sb("ih", [128, 16, NT], BF16)
    hv = hT.ap().rearrange("(fc p) t -> p fc t", p=128)
    for q in range(4):
        fw.dma("sp", h[:, q * 4:(q + 1) * 4, :], hv[:, q * 4:(q + 1) * 4, :], reads=[hTr], writes=[hr])
    wbuf = [P.sb("iw%d" % i, [128, 16, 512], BF16) for i in range(2)]
    stg = [P.sb("istg%d" % i, [128, NT], F32) for i in range(2)]
    vst = [P.sb("ivst%d" % i, [128, 512], BF16) for i in range(2)]
    win = P.dr["w_in"][0].ap()
    vmap = {2: 0, 5: 1, 9: 2}
    ev = 0
    si = 0
    for cg in range(12):
        c0 = cg * 512
        cw = min(512, INW - c0)
        wt, wr = wbuf[cg % 2]
        fw.dma("pool", wt[:, :, 0:cw], win[l, :, c0:c0 + cw].rearrange("(kc p) n -> p kc n", p=128), writes=[wr])
        if cg in vmap:
            vi = vmap[cg]
            for tb in range(NTB):
                pst, psr = P.bank()
                for kc in range(16):
                    fw.op("pe", lambda e, pst=pst, kc=kc, tb=tb, wt=wt: e.matmul(
                        pst[:, :], h[:, kc, tb * 128:(tb + 1) * 128], wt[:, kc, :], start=(kc == 0), stop=(kc == 15)),
                        reads=[hr, wr], writes=[psr])
                vt, vr = vst[tb % 2]
                if ev % 2 == 0:
                    fw.op("dve", lambda e, vt=vt, pst=pst: e.tensor_copy(vt[:], pst[:]), reads=[psr], writes=[vr])
                else:
                    fw.op("act", lambda e, vt=vt, pst=pst: e.copy(vt[:], pst[:]), reads=[psr], writes=[vr])
                ev += 1
                fw.dma("sp", pV.ap()[vi, tb * 128:(tb + 1) * 128, :], vt[:], reads=[vr], writes=[pVr])
            continue
        for oc in range((cw + 127) // 128):
            m = min(128, cw - oc * 128)
            st, sr = stg[si % 2]
            si += 1
            for (t0, w, j) in TG:
                pst, psr = P.bank()
                for kc in range(16):
                    fw.op("pe", lambda e, pst=pst, kc=kc, wt=wt, oc=oc, m=m, t0=t0, w=w: e.matmul(
                        pst[0:m, 0:w], wt[:, kc, oc * 128:oc * 128 + m], h[:, kc, t0:t0 + w],
                        start=(kc == 0), stop=(kc == 15)), reads=[hr, wr], writes=[psr])
                if ev % 2 == 0:
                    fw.op("dve", lambda e, st=st, pst=pst, m=m, t0=t0, w=w: e.tensor_copy(st[0:m, t0:t0 + w], pst[0:m, 0:w]),
                          reads=[psr], writes=[sr])
                else:
                    fw.op("act", lambda e, st=st, pst=pst, m=m, t0=t0, w=w: e.copy(st[0:m, t0:t0 + w], pst[0:m, 0:w]),
                          reads=[psr], writes=[sr])
                ev += 1
            r0 = c0 + oc * 128
            fw.dma("sp", pT.ap()[r0:r0 + m, :], st[0:m, :], reads=[sr], writes=[pTr])
    P.phase_end()


WEIGHT_SHAPES = {
    "w_mod": (DEPTH, D, 6 * D), "w_in": (DEPTH, D, INW), "w_uq": (DEPTH, 512, 768), "w_ukv": (DEPTH, 256, 1024),
    "w_branch": (DEPTH, 4, 512, D), "w_gate": (DEPTH, 4, D, D), "w_o": (DEPTH, D, D),
    "peer_w_query": (DEPTH, D, D), "peer_sub_keys": (DEPTH, 8, 2, 128, 128),
    "peer_u": (DEPTH, 16384, D), "peer_v": (DEPTH, 16384, D),
}
CONST_SHAPES = {"k_ident": (128, 128), "k_ones": (128, 128), "k_bd64": (128, 128), "k_rot64": (128, 128),
                "k_rot128": (128, 128), "k_cos64": (128, T), "k_sin64": (128, T), "k_cos128": (128, T),
                "k_sin128": (128, T), "k_rett": (128, 6 * 128 + 2)}


def build_program(phases=None, ext_in=(), ext_out=(), same_engine_sync=True, weights=None, do_mod=True):
    P = Prog(ext_in, ext_out, same_engine_sync)
    P.inp("x_b", (T, D))
    P.inp("ctx_b", (L, D))
    P.inp("cvec", (128, 16, 2))
    P.inp("smalls", (DEPTH, 128, NSM))
    for k, shp in CONST_SHAPES.items():
        P.inp(k, shp)
    for k, shp in WEIGHT_SHAPES.items():
        if weights is None or k in weights:
            P.inp(k, shp)
    P.inp("nab", (DEPTH, 128, 8 * 16 * 64))
    P.scratch("xT", (D, NT), F32)
    P.scratch("hT", (D, NT), BF16)
    P.scratch("pT", (INW, NT), F32)
    P.scratch("pV", (3, NT, 512), BF16)
    P.scratch("yT", (4, 512, NT), BF16)
    P.scratch("qT", (D, NT), BF16)
    P.scratch("GdT", (16384, NT), BF16)
    P.scratch("GAT", (16384, NT), BF16)
    P.out("y", (T, D))
    run = (lambda n: True) if phases is None else (lambda n: n in phases)
    ph_setup(P, do_mod)
    if run("tin"):
        ph_transpose_in(P)
    for l in range(DEPTH):
        if run("norm1_%d" % l):
            ph_norm(P, l, 0, "hT")
        if run("inproj_%d" % l):
            ph_inproj(P, l)
        if run("diff_%d" % l):
            ph_mix_diff(P, l, l < DEPTH - 1)
        if run("mla_%d" % l):
            ph_mix_mla(P, l, l < DEPTH - 1)
        if run("ret_%d" % l):
            ph_mix_ret(P, l, l < DEPTH - 1)
        if run("na_%d" % l):
            ph_mix_na(P, l, l < DEPTH - 1)
        if run("merge_%d" % l):
            ph_merge(P, l, l < DEPTH - 1)
        if run("norm2_%d" % l):
            ph_norm(P, l, 1, "hT")
        if run("peerq_%d" % l):
            ph_peer_q(P, l)
        if run("peerg_%d" % l):
            ph_peer_gate(P, l, l < DEPTH - 1)
        if run("peeru_%d" % l):
            ph_peer_u(P, l, l < DEPTH - 1)
        if run("peerv_%d" % l):
            ph_peer_v(P, l, l < DEPTH - 1)
    if run("tout"):
        ph_transpose_out(P)
    P.fw.barrier()
    P.es.close()
    return P


def host_inputs(inp, b, consts, smalls):
    m = {"x_b": np.ascontiguousarray(inp["x"][b]), "ctx_b": np.ascontiguousarray(inp["ctx"][b])}
    cv = np.stack([_cols(inp["c"][b]), _cols(inp["c_ctx"])], axis=2)
    m["cvec"] = np.ascontiguousarray(cv.astype(np.float32))
    m["smalls"] = smalls
    for k, v in consts.items():
        m["k_" + k] = v
    for k in WEIGHT_SHAPES:
        m[k] = inp[k]
    return m


def load_tables(P, names):
    out = {}
    for nm in names:
        t, r = P.sb("tb_" + nm, [128, T], F32)
        P.fw.dma("sp", t[:], P.dr["k_" + nm][0].ap(), writes=[r])
        out[nm] = (t, r)
    return out


class Scr:
    def __init__(self, P, name, n, dt=F32, w=512):
        self.t = [P.sb("%s%d" % (name, i), [128, w], dt) for i in range(n)]
        self.i = 0

    def get(self):
        x = self.t[self.i % len(self.t)]
        self.i += 1
        return x


def interleave(gens):
    gens = list(gens)
    while gens:
        for g in list(gens):
            try:
                next(g)
            except StopIteration:
                gens.remove(g)


def run(gen):
    for _ in gen:
        pass


def rms_rows_g(P, S, srcs, w, bdmat, gsize, dst_r, out_rs):
    fw = P.fw
    pst, psr = P.bank()
    bd, bdr = bdmat
    for i, (ap, r, k) in enumerate(srcs):
        sq, sqr = S.get()
        fw.op("act", lambda e, sq=sq, ap=ap, k=k: e.activation(out=sq[0:k, 0:w], in_=ap, func=AF.Square), reads=[r], writes=[sqr])
        yield
        fw.op("pe", lambda e, sq=sq, k=k, i=i: e.matmul(pst[:, 0:w], bd[0:k, :], sq[0:k, 0:w], start=(i == 0), stop=(i == len(srcs) - 1)),
              reads=[sqr, bdr], writes=[psr])
        yield
    fw.op("act", lambda e: e.activation(out=out_rs[:, 0:w], in_=pst[:, 0:w], func=AF.Sqrt, bias=EPS, scale=1.0 / gsize),
          reads=[psr], writes=[dst_r])
    yield
    fw.op("dve", lambda e: e.reciprocal(out_rs[:, 0:w], out_rs[:, 0:w]), reads=[dst_r], writes=[dst_r])
    yield


def rms_rows(P, S, srcs, w, bdmat, gsize, dst_r, out_rs):
    run(rms_rows_g(P, S, srcs, w, bdmat, gsize, dst_r, out_rs))


def rope_apply_g(P, S, xn, xnr, k, t0, w, rotm, cos, sin, dst, dstr):
    fw = P.fw
    rm, rmr = rotm
    (ct, cr), (st, sr) = cos, sin
    p0 = t0 - L
    pst, psr = P.bank()
    fw.op("pe", lambda e: e.matmul(pst[0:k, 0:w], rm[0:k, 0:k], xn[0:k, 0:w], start=True, stop=True), reads=[xnr, rmr], writes=[psr])
    yield
    t1, t1r = S.get()
    fw.op("pool", lambda e: e.tensor_tensor(t1[0:k, 0:w], xn[0:k, 0:w], ct[0:k, p0:p0 + w], ALU.mult), reads=[xnr, cr], writes=[t1r])
    yield
    t2, t2r = S.get()
    fw.op("dve", lambda e: e.tensor_tensor(t2[0:k, 0:w], pst[0:k, 0:w], st[0:k, p0:p0 + w], ALU.mult), reads=[psr, sr], writes=[t2r])
    yield
    fw.op("dve", lambda e: e.tensor_tensor(dst[0:k, t0:t0 + w], t1[0:k, 0:w], t2[0:k, 0:w], ALU.add), reads=[t1r, t2r], writes=[dstr])
    yield


def rope_apply(P, S, xn, xnr, k, t0, w, rotm, cos, sin, dst, dstr):
    run(rope_apply_g(P, S, xn, xnr, k, t0, w, rotm, cos, sin, dst, dstr))


def prep_qk_g(P, S, src_ap, srcr, dst, dstr, gain, bdmat, gsize, rope=None, scale=None):
    fw = P.fw
    sm, smr = P.C["sm"]
    for (t0, w, j) in TG:
        x, xr = S.get()
        fw.dma("sp", x[:, 0:w], src_ap[:, t0:t0 + w], reads=[srcr], writes=[xr])
        yield
        do_rope = rope is not None and j == 0
        if bdmat is not None:
            rs, rsr = S.get()
            yield from rms_rows_g(P, S, [(x[:, 0:w], xr, 128)], w, bdmat, gsize, rsr, rs)
            if do_rope:
                xn, xnr = S.get()
                fw.op("dve", lambda e, xn=xn, x=x, rs=rs: e.scalar_tensor_tensor(xn[:, 0:w], x[:, 0:w], gain, rs[:, 0:w], ALU.mult, ALU.mult),
                      reads=[xr, rsr, smr], writes=[xnr])
            else:
                fw.op("dve", lambda e, x=x, rs=rs: e.scalar_tensor_tensor(dst[:, t0:t0 + w], x[:, 0:w], gain, rs[:, 0:w], ALU.mult, ALU.mult),
                      reads=[xr, rsr, smr], writes=[dstr])
            yield
        else:
            if do_rope:
                if scale is not None:
                    xn, xnr = S.get()
                    fw.op("act", lambda e, xn=xn, x=x: e.mul(xn[:, 0:w], x[:, 0:w], scale), reads=[xr], writes=[xnr])
                else:
                    xn, xnr = x, xr
            else:
                fw.op("act", lambda e, x=x: e.mul(dst[:, t0:t0 + w], x[:, 0:w], 1.0 if scale is None else scale), reads=[xr], writes=[dstr])
            yield
        if do_rope:
            yield from rope_apply_g(P, S, xn, xnr, 128, t0, w, rope[0], rope[1], rope[2], dst, dstr)


def prep_qk(P, S, *a, **kw):
    run(prep_qk_g(P, S, *a, **kw))


def attn_core(P, tag, heads, npass, parts_fn, V, Vr, scale, ctx_out, finish_fn, Ebufs):
    fw = P.fw
    onesb, onesbr = P.C["ones_bf"]
    groups = [g for g in TG if (g[2] == 0 or ctx_out)]
    heads = list(heads)

    def lane_gen(lane, lheads):
        osb = [[P.sb("%s_o%d_%d_%d" % (tag, lane, p, i), [128, 512], F32) for i in range(2)] for p in range(npass)]
        rz = [P.sb("%s_rz%d_%d" % (tag, lane, i), [128, 512], F32) for i in range(2)]
        Eb = [P.sb("%s_E%d_%d" % (tag, lane, i), [128, 512], BF16) for i in range(3)]
        ei = 0
        gi = 0
        for h in lheads:
            for (t0, w, j) in groups:
                nkc = 2 if j == 1 else NTB
                outs = []
                for p in range(npass):
                    Ob, Obr = P.bank(4 + 2 * lane)
                    Zb, Zbr = P.bank(5 + 2 * lane)
                    parts = parts_fn(h, p)

                    def _pv(kc, E, Er):
                        fw.op("pe", lambda e, E=E, kc=kc: e.matmul(Ob[:, 0:w], V[:, kc, h * 128:(h + 1) * 128], E[:, 0:w],
                                                                 start=(kc == 0), stop=(kc == nkc - 1)), reads=[Er, Vr], writes=[Obr])
                        yield
                        fw.op("pe", lambda e, E=E, kc=kc: e.matmul(Zb[:, 0:w], onesb[:], E[:, 0:w],
                                                                 start=(kc == 0), stop=(kc == nkc - 1)), reads=[Er, onesbr], writes=[Zbr])
                        yield

                    pend = []
                    for kc in range(nkc):
                        Sb, Sbr = P.bank(2 * lane + ei % 2)
                        for i, (kf, qf, kr_, qr_) in enumerate(parts):
                            fw.op("pe", lambda e, Sb=Sb, kf=kf, qf=qf, kc=kc, i=i: e.matmul(
                                Sb[:, 0:w], kf(kc * 128, (kc + 1) * 128), qf(t0, t0 + w), start=(i == 0), stop=(i == len(parts) - 1)),
                                reads=[kr_, qr_], writes=[Sbr])
                        yield
                        E, Er = Eb[ei % 3]
                        ei += 1
                        fw.op("act", lambda e, E=E, Sb=Sb: e.activation(out=E[:, 0:w], in_=Sb[:, 0:w], func=AF.Exp, scale=scale),
                              reads=[Sbr], writes=[Er], inc=True)
                        yield
                        pend.append((kc, E, Er))
                        if len(pend) > 2:
                            yield from _pv(*pend.pop(0))
                    while pend:
                        yield from _pv(*pend.pop(0))
                    rzt, rzr = rz[p % 2]
                    fw.op("dve", lambda e, rzt=rzt, Zb=Zb: e.reciprocal(rzt[:, 0:w], Zb[:, 0:w]), reads=[Zbr], writes=[rzr])
                    ot, otr = osb[p][gi % 2]
                    fw.op("dve", lambda e, ot=ot, Ob=Ob, rzt=rzt: e.tensor_tensor(ot[:, 0:w], Ob[:, 0:w], rzt[:, 0:w], ALU.mult),
                          reads=[Obr, rzr], writes=[otr])
                    yield
                    outs.append((ot, otr))
                gi += 1
                finish_fn(h, t0, w, outs)
                yield

    nh = len(heads)
    interleave([lane_gen(0, heads[:nh // 2]), lane_gen(1, heads[nh // 2:])])


def ph_mix_diff(P, l, ctx_out):
    fw = P.fw
    P.phase_begin()
    sm, smr = P.C["sm"]
    pT, pTr = P.dr["pT"]
    pV, pVr = P.dr["pV"]
    yT, yTr = P.dr["yT"]
    tabs = load_tables(P, ["cos64", "sin64"])
    S = Scr(P, "dsc", 16)
    q, qr = P.sb("dq", [128, 4, NT], BF16)
    k, kr = P.sb("dk", [128, 4, NT], BF16)
    V, Vr = P.sb("dv", [128, NTB, 512], BF16)
    fw.dma("sp", V[:], pV.ap()[0].rearrange("(tb p) n -> p tb n", p=128), reads=[pVr], writes=[Vr])
    rope = (P.C["rot64"], tabs["cos64"], tabs["sin64"])
    for h in range(4):
        interleave([
            prep_qk_g(P, S, pT.ap()[h * 128:(h + 1) * 128, :], pTr, q[:, h, :], qr, sm[:, l, SM["dqg"][0]:SM["dqg"][0] + 1], P.C["bd64"], 64, rope),
            prep_qk_g(P, S, pT.ap()[512 + h * 128:512 + (h + 1) * 128, :], pTr, k[:, h, :], kr, sm[:, l, SM["dkg"][0]:SM["dkg"][0] + 1], P.C["bd64"], 64, rope)])
    lam_init = 0.8 - 0.6 * math.exp(-0.3 * l)
    o = SM["dlam"][0]
    lt, ltr = P.sb("dlamt", [128, 128], F32)
    lc, lcr = P.sb("dlamc", [128, 4], F32)
    fw.op("dve", lambda e: e.tensor_tensor(lt[:, 0:64], sm[:, l, o:o + 64], sm[:, l, o + 64:o + 128], ALU.mult), reads=[smr], writes=[ltr])
    fw.op("dve", lambda e: e.tensor_tensor(lt[:, 64:128], sm[:, l, o + 128:o + 192], sm[:, l, o + 192:o + 256], ALU.mult), reads=[smr], writes=[ltr])
    fw.op("dve", lambda e: e.tensor_reduce(lc[:, 0:2], lt[:].rearrange("p (a b) -> p a b", a=2), AX.X, ALU.add), reads=[ltr], writes=[lcr])
    fw.op("act", lambda e: e.activation(out=lc[:, 0:2], in_=lc[:, 0:2], func=AF.Exp), reads=[lcr], writes=[lcr])
    fw.op("dve", lambda e: e.scalar_tensor_tensor(lc[:, 2:3], lc[:, 1:2], -lam_init, lc[:, 0:1], ALU.add, ALU.subtract), reads=[lcr], writes=[lcr])
    fw.op("dve", lambda e: e.tensor_scalar(lc[:, 3:4], sm[:, l, SM["dsub"][0]:SM["dsub"][0] + 1], 1.0 - lam_init, None, ALU.mult), reads=[smr], writes=[lcr])
    Ebufs = None
    ystg = [P.sb("dy%d" % i, [128, 512], BF16) for i in range(2)]
    cnt = [0]

    def parts_fn(h, p):
        b = 64 * p
        return [(lambda c0, c1: k[b:b + 64, h, c0:c1], lambda c0, c1: q[b:b + 64, h, c0:c1], kr, qr)]

    def finish(h, t0, w, outs):
        (o1, o1r), (o2, o2r) = outs
        y, yr_ = S.get()
        fw.op("dve", lambda e: e.scalar_tensor_tensor(y[:, 0:w], o2[:, 0:w], lc[:, 2:3], o1[:, 0:w], ALU.mult, ALU.add),
              reads=[o1r, o2r, lcr], writes=[yr_])
        rs, rsr = S.get()
        rms_rows(P, S, [(y[:, 0:w], yr_, 128)], w, P.C["ones"], 128, rsr, rs)
        yo, yor = ystg[cnt[0] % 2]
        cnt[0] += 1
        fw.op("dve", lambda e: e.scalar_tensor_tensor(yo[:, 0:w], y[:, 0:w], lc[:, 3:4], rs[:, 0:w], ALU.mult, ALU.mult),
              reads=[yr_, rsr, lcr], writes=[yor])
        fw.dma("sp", yT.ap()[0, h * 128:(h + 1) * 128, t0:t0 + w], yo[:, 0:w], reads=[yor], writes=[yTr])

    attn_core(P, "da", range(4), 2, parts_fn, V, Vr, 64 ** -0.5, ctx_out, finish, Ebufs)
    P.phase_end()


def ph_mix_mla(P, l, ctx_out):
    fw = P.fw
    P.phase_begin()
    sm, smr = P.C["sm"]
    pT, pTr = P.dr["pT"]
    yT, yTr = P.dr["yT"]
    ones, onesr = P.C["ones"]
    tabs = load_tables(P, ["cos64", "sin64"])
    rope = (P.C["rot64"], tabs["cos64"], tabs["sin64"])
    S = Scr(P, "msc", 10)
    wuq, wuqr = P.sb("m_wuq", [128, 4, 768], BF16)
    fw.dma("pool", wuq[:], P.dr["w_uq"][0].ap()[l].rearrange("(kc p) n -> p kc n", p=128), writes=[wuqr])
    wukv, wukvr = P.sb("m_wukv", [128, 2, 1024], BF16)
    fw.dma("pool", wukv[:], P.dr["w_ukv"][0].ap()[l].rearrange("(kc p) n -> p kc n", p=128), writes=[wukvr])
    cqn, cqnr = P.sb("m_cqn", [128, 4, NT], BF16)
    ckvn, ckvnr = P.sb("m_ckvn", [128, 2, NT], BF16)
    krt, krtr = P.sb("m_kr", [64, NT], F32)
    qn_, qnr = P.sb("m_qn", [128, 4, NT], BF16)
    qr_, qrr = P.sb("m_qr", [64, 4, NT], BF16)
    kn_, knr = P.sb("m_kn", [128, 4, NT], BF16)
    kro, kror = P.sb("m_kro", [64, 4, NT], BF16)
    V, Vr = P.sb("m_v", [128, NTB, 512], BF16)
    fw.dma("sp", krt[:], pT.ap()[5888:5952, :], reads=[pTr], writes=[krtr])
    for (nm, r0, nch, dst, dstr, gname) in (("cq", 5120, 4, cqn, cqnr, "mqn"), ("ckv", 5632, 2, ckvn, ckvnr, "mkvn")):
        go = SM[gname][0]
        for (t0, w, j) in TG:
            xs = []
            for c in range(nch):
                x, xr = S.get()
                fw.dma("sp", x[:, 0:w], pT.ap()[r0 + c * 128:r0 + (c + 1) * 128, t0:t0 + w], reads=[pTr], writes=[xr])
                xs.append((x, xr))
            rs, rsr = S.get()
            rms_rows(P, S, [(x[:, 0:w], xr, 128) for (x, xr) in xs], w, P.C["ones"], nch * 128, rsr, rs)
            for c, (x, xr) in enumerate(xs):
                fw.op("dve", lambda e, x=x, c=c, rs=rs: e.scalar_tensor_tensor(dst[:, c, t0:t0 + w], x[:, 0:w], sm[:, l, go + c:go + c + 1], rs[:, 0:w], ALU.mult, ALU.mult),
                      reads=[xr, rsr, smr], writes=[dstr])
    ev = 0
    for tb in range(NTB):
        pst, psr = P.bank()
        for kc in range(2):
            fw.op("pe", lambda e, pst=pst, kc=kc, tb=tb: e.matmul(
                pst[:].rearrange("p (h d) -> p h d", h=4), ckvn[:, kc, tb * 128:(tb + 1) * 128],
                wukv[:, kc, :].rearrange("p (h x) -> p h x", h=4)[:, :, 128:256], start=(kc == 0), stop=(kc == 1)),
                reads=[ckvnr, wukvr], writes=[psr])
        fw.op("dve" if tb % 2 == 0 else "act",
              (lambda e, pst=pst, tb=tb: e.tensor_copy(V[:, tb, :], pst[:])) if tb % 2 == 0 else (lambda e, pst=pst, tb=tb: e.copy(V[:, tb, :], pst[:])),
              reads=[psr], writes=[Vr])
    gq_n = sm[:, l, SM["mq_nope"][0]:SM["mq_nope"][0] + 1]
    gq_r = sm[:, l, SM["mq_rope"][0]:SM["mq_rope"][0] + 1]
    gk_n = sm[:, l, SM["mk_nope"][0]:SM["mk_nope"][0] + 1]
    gk_r = sm[:, l, SM["mk_rope"][0]:SM["mk_rope"][0] + 1]
    for h in range(4):
        for (t0, w, j) in TG:
            pn, pnr = P.bank()
            pr, prr = P.bank()
            for kc in range(4):
                fw.op("pe", lambda e, kc=kc: e.matmul(pn[:, 0:w], wuq[:, kc, h * 192:h * 192 + 128], cqn[:, kc, t0:t0 + w], start=(kc == 0), stop=(kc == 3)),
                      reads=[wuqr, cqnr], writes=[pnr])
            for kc in range(4):
                fw.op("pe", lambda e, kc=kc: e.matmul(pr[0:64, 0:w], wuq[:, kc, h * 192 + 128:h * 192 + 192], cqn[:, kc, t0:t0 + w], start=(kc == 0), stop=(kc == 3)),
                      reads=[wuqr, cqnr], writes=[prr])
            xn, xnr = S.get()
            xr_, xrr = S.get()
            fw.op("act", lambda e, xn=xn: e.copy(xn[:, 0:w], pn[:, 0:w]), reads=[pnr], writes=[xnr])
            fw.op("dve", lambda e, xr_=xr_: e.tensor_copy(xr_[0:64, 0:w], pr[0:64, 0:w]), reads=[prr], writes=[xrr])
            rs, rsr = S.get()
            rms_rows(P, S, [(xn[:, 0:w], xnr, 128), (xr_[0:64, 0:w], xrr, 64)], w, P.C["ones"], 192, rsr, rs)
            fw.op("dve", lambda e, xn=xn, rs=rs: e.scalar_tensor_tensor(qn_[:, h, t0:t0 + w], xn[:, 0:w], gq_n, rs[:, 0:w], ALU.mult, ALU.mult),
                  reads=[xnr, rsr, smr], writes=[qnr])
            if j == 0:
                xq, xqr = S.get()
                fw.op("dve", lambda e, xq=xq, xr_=xr_, rs=rs: e.scalar_tensor_tensor(xq[0:64, 0:w], xr_[0:64, 0:w], gq_r[0:64], rs[0:64, 0:w], ALU.mult, ALU.mult),
                      reads=[xrr, rsr, smr], writes=[xqr])
                rope_apply(P, S, xq, xqr, 64, t0, w, rope[0], rope[1], rope[2], qr_[:, h, :], qrr)
            else:
                fw.op("dve", lambda e, xr_=xr_, rs=rs: e.scalar_tensor_tensor(qr_[:, h, t0:t0 + w], xr_[0:64, 0:w], gq_r[0:64], rs[0:64, 0:w], ALU.mult, ALU.mult),
                      reads=[xrr, rsr, smr], writes=[qrr])
            pk, pkr = P.bank()
            for kc in range(2):
                fw.op("pe", lambda e, kc=kc: e.matmul(pk[:, 0:w], wukv[:, kc, h * 256:h * 256 + 128], ckvn[:, kc, t0:t0 + w], start=(kc == 0), stop=(kc == 1)),
                      reads=[wukvr, ckvnr], writes=[pkr])
            xk, xkr = S.get()
            fw.op("act", lambda e, xk=xk: e.copy(xk[:, 0:w], pk[:, 0:w]), reads=[pkr], writes=[xkr])
            rs2, rs2r = S.get()
            rms_rows(P, S, [(xk[:, 0:w], xkr, 128), (krt[0:64, t0:t0 + w], krtr, 64)], w, P.C["ones"], 192, rs2r, rs2)
            fw.op("dve", lambda e, xk=xk, rs2=rs2: e.scalar_tensor_tensor(kn_[:, h, t0:t0 + w], xk[:, 0:w], gk_n, rs2[:, 0:w], ALU.mult, ALU.mult),
                  reads=[xkr, rs2r, smr], writes=[knr])
            if j == 0:
                xq2, xq2r = S.get()
                fw.op("dve", lambda e, xq2=xq2, rs2=rs2: e.scalar_tensor_tensor(xq2[0:64, 0:w], krt[0:64, t0:t0 + w], gk_r[0:64], rs2[0:64, 0:w], ALU.mult, ALU.mult),
                      reads=[krtr, rs2r, smr], writes=[xq2r])
                rope_apply(P, S, xq2, xq2r, 64, t0, w, rope[0], rope[1], rope[2], kro[:, h, :], kror)
            else:
                fw.op("dve", lambda e, rs2=rs2: e.scalar_tensor_tensor(kro[:, h, t0:t0 + w], krt[0:64, t0:t0 + w], gk_r[0:64], rs2[0:64, 0:w], ALU.mult, ALU.mult),
                      reads=[krtr, rs2r, smr], writes=[kror])
    Ebufs = None
    ystg = [P.sb("my%d" % i, [128, 512], BF16) for i in range(2)]
    cnt = [0]

    def parts_fn(h, p):
        return [(lambda c0, c1: kn_[:, h, c0:c1], lambda c0, c1: qn_[:, h, c0:c1], knr, qnr),
                (lambda c0, c1: kro[:, h, c0:c1], lambda c0, c1: qr_[:, h, c0:c1], kror, qrr)]

    def finish(h, t0, w, outs):
        (o1, o1r), = outs
        yo, yor = ystg[cnt[0] % 2]
        cnt[0] += 1
        fw.op("act", lambda e: e.copy(yo[:, 0:w], o1[:, 0:w]), reads=[o1r], writes=[yor])
        fw.dma("sp", yT.ap()[3, h * 128:(h + 1) * 128, t0:t0 + w], yo[:, 0:w], reads=[yor], writes=[yTr])

    attn_core(P, "ma", range(4), 1, parts_fn, V, Vr, 192 ** -0.5, ctx_out, finish, Ebufs)
    P.phase_end()


def ph_mix_ret(P, l, ctx_out):
    fw = P.fw
    P.phase_begin()
    sm, smr = P.C["sm"]
    pT, pTr = P.dr["pT"]
    pV, pVr = P.dr["pV"]
    yT, yTr = P.dr["yT"]
    identb, identbr = P.C["ident_bf"]
    tabs = load_tables(P, ["cos128", "sin128"])
    rope = (P.C["rot128"], tabs["cos128"], tabs["sin128"])
    S = Scr(P, "rsc", 16)
    q, qr = P.sb("r_q", [128, 4, NT], BF16)
    k, kr = P.sb("r_k", [128, 4, NT], BF16)
    V, Vr = P.sb("r_v", [128, NTB, 512], BF16)
    fw.dma("sp", V[:], pV.ap()[1].rearrange("(tb p) n -> p tb n", p=128), reads=[pVr], writes=[Vr])
    for h in range(4):
        interleave([
            prep_qk_g(P, S, pT.ap()[1536 + h * 128:1536 + (h + 1) * 128, :], pTr, q[:, h, :], qr, None, None, None, rope, None),
            prep_qk_g(P, S, pT.ap()[2048 + h * 128:2048 + (h + 1) * 128, :], pTr, k[:, h, :], kr, None, None, None, rope, 128 ** -0.5)])
    rt, rtr = P.sb("r_rt", [128, 6 * 128 + 2], F32)
    fw.dma("sp", rt[:], P.dr["k_rett"][0].ap(), writes=[rtr])
    lg, lgr = P.sb("r_lg", [128, 8], F32)
    o = SM["rdecay"][0]
    fw.op("act", lambda e: e.activation(out=lg[:], in_=sm[:, l, o:o + 8], func=AF.Exp, scale=-1.0), reads=[smr], writes=[lgr])
    fw.op("act", lambda e: e.activation(out=lg[:], in_=lg[:], func=AF.Ln, bias=1.0), reads=[lgr], writes=[lgr])
    fw.op("act", lambda e: e.mul(lg[:], lg[:], -1.0), reads=[lgr], writes=[lgr])
    DmT, DmTr = P.sb("r_dm", [128, 8, 128], F32)
    XI, XIr = P.sb("r_xi", [128, 8, 128], F32)
    ZG, ZGr = P.sb("r_zg", [128, 8, 2], F32)
    c128, c128r = P.sb("r_c128", [128, 1], F32)
    fw.op("dve", lambda e: e.memset(c128[:], 128.0), writes=[c128r])
    for d in range(2):
        for h in range(4):
            i = d * 4 + h
            col = lg[:, i:i + 1]
            fw.op("act", lambda e, i=i, d=d, col=col: e.activation(out=DmT[:, i, :], in_=rt[:, d * 256:d * 256 + 128], func=AF.Exp, scale=col),
                  reads=[rtr, lgr], writes=[DmTr])
            fw.op("dve", lambda e, i=i, d=d: e.tensor_tensor(DmT[:, i, :], DmT[:, i, :], rt[:, d * 256 + 128:d * 256 + 256], ALU.mult),
                  reads=[rtr, DmTr], writes=[DmTr])
            fw.op("act", lambda e, i=i, d=d, col=col: e.activation(out=XI[:, i, :], in_=rt[:, 512 + d * 128:512 + (d + 1) * 128], func=AF.Exp, scale=col),
                  reads=[rtr, lgr], writes=[XIr])
            fw.op("act", lambda e, i=i, d=d, col=col: e.activation(out=ZG[:, i, 0:1], in_=rt[:, 768 + d:768 + d + 1], func=AF.Exp, scale=col),
                  reads=[rtr, lgr], writes=[ZGr])
            fw.op("act", lambda e, i=i, col=col: e.activation(out=ZG[:, i, 1:2], in_=c128[:], func=AF.Exp, scale=col),
                  reads=[c128r, lgr], writes=[ZGr])
    yacc, yaccr = P.sb("r_yacc", [128, 4, NT], F32)
    yres = [[Res("yacc%d_%d" % (h, c)) for c in range(NTB)] for h in range(4)]
    for h in range(4):
        fw.op("pool", lambda e, h=h: e.memset(yacc[:, h, :], 0.0), writes=yres[h])
    Rf = [P.sb("r_R%d" % i, [128, 128], F32) for i in range(8)]
    Rb = [P.sb("r_Rb%d" % i, [128, 128], BF16) for i in range(8)]
    stm = [P.sb("r_stm%d" % i, [128, 128], BF16) for i in range(8)]
    qx = [P.sb("r_qx%d" % i, [128, 128], BF16) for i in range(8)]
    kz = [P.sb("r_kz%d" % i, [128, 128], BF16) for i in range(8)]
    order = {0: list(range(NTB)), 1: [1, 0] + list(range(NTB - 1, 1, -1))}
    def _step(si, d, h):
        i = d * 4 + h
        c = order[d][si]
        sl = slice(c * 128, (c + 1) * 128)
        need_out = (c >= 2) or ctx_out
        R_, R_r = Rf[i]
        Rb_, Rb_r = Rb[i]
        if need_out:
            st_, st_r = P.bank()
            fw.op("pe", lambda e: e.matmul(st_[:, 0:128], k[:, h, sl], q[:, h, sl], start=True, stop=True), reads=[kr, qr], writes=[st_r])
            yield
            sm_, sm_r = stm[i]
            fw.op("dve", lambda e: e.tensor_tensor(sm_[:], st_[:, 0:128], DmT[:, i, :], ALU.mult), reads=[st_r, DmTr], writes=[sm_r])
            yield
            ob, obr = P.bank()
            fw.op("pe", lambda e: e.matmul(ob[:, 0:128], V[:, c, h * 128:(h + 1) * 128], sm_[:], start=True, stop=(si == 0)), reads=[Vr, sm_r], writes=[obr])
            yield
            if si > 0:
                qx_, qx_r = qx[i]
                fw.op("pool", lambda e: e.tensor_tensor(qx_[:], q[:, h, sl], XI[:, i, :], ALU.mult), reads=[qr, XIr], writes=[qx_r])
                yield
                fw.op("pe", lambda e: e.matmul(ob[:, 0:128], Rb_[:], qx_[:], start=False, stop=True), reads=[Rb_r, qx_r], writes=[obr])
                yield
            yr_ = yres[h][c]
            fw.op("dve", lambda e: e.tensor_tensor(yacc[:, h, sl], yacc[:, h, sl], ob[:, 0:128], ALU.add), reads=[obr, yr_], writes=[yr_])
            yield
        if si < NTB - 1:
            kt, ktr = P.bank()
            fw.op("pe", lambda e: e.matmul(kt[:, 0:128], k[:, h, sl], identb[:], start=True, stop=True), reads=[kr, identbr], writes=[ktr])
            yield
            kz_, kz_r = kz[i]
            fw.op("act", lambda e: e.activation(out=kz_[:], in_=kt[:, 0:128], func=AF.Copy, scale=ZG[:, i, 0:1]), reads=[ktr, ZGr], writes=[kz_r])
            yield
            rn, rnr = P.bank()
            fw.op("pe", lambda e: e.matmul(rn[:, 0:128], kz_[:], V[:, c, h * 128:(h + 1) * 128], start=True, stop=True), reads=[kz_r, Vr], writes=[rnr])
            yield
            if si == 0:
                fw.op("dve", lambda e: e.tensor_copy(R_[:], rn[:, 0:128]), reads=[rnr], writes=[R_r])
            else:
                fw.op("dve", lambda e: e.scalar_tensor_tensor(R_[:], R_[:], ZG[:, i, 1:2], rn[:, 0:128], ALU.mult, ALU.add), reads=[rnr, R_r, ZGr], writes=[R_r])
            yield
            fw.op("act", lambda e: e.copy(Rb_[:], R_[:]), reads=[R_r], writes=[Rb_r])
            yield

    for si in range(NTB):
        for d in range(2):
            interleave([_step(si, d, h) for h in range(4)])
    allres = [yres[h][c] for h in range(4) for c in range(NTB)]
    ystg = [P.sb("r_y%d" % i, [128, 512], BF16) for i in range(2)]
    cnt = 0
    go = SM["rnorm"][0]
    for h in range(4):
        for (t0, w, j) in TG:
            if j == 1 and not ctx_out:
                continue
            g_, g_r = S.get()
            fw.dma("sp", g_[:, 0:w], pT.ap()[3072 + h * 128:3072 + (h + 1) * 128, t0:t0 + w], reads=[pTr], writes=[g_r])
            fw.op("act", lambda e, g_=g_: e.activation(out=g_[:, 0:w], in_=g_[:, 0:w], func=AF.Silu), reads=[g_r], writes=[g_r])
            rs, rsr = S.get()
            sq, sqr = S.get()
            pst, psr = P.bank()
            ones, onesr = P.C["ones"]
            fw.op("act", lambda e, sq=sq, h=h: e.activation(out=sq[:, 0:w], in_=yacc[:, h, t0:t0 + w], func=AF.Square), reads=allres, writes=[sqr])
            fw.op("pe", lambda e, sq=sq, pst=pst: e.matmul(pst[:, 0:w], ones[:], sq[:, 0:w], start=True, stop=True), reads=[sqr, onesr], writes=[psr])
            fw.op("act", lambda e, rs=rs, pst=pst: e.activation(out=rs[:, 0:w], in_=pst[:, 0:w], func=AF.Sqrt, bias=EPS, scale=1.0 / 128), reads=[psr], writes=[rsr])
            fw.op("dve", lambda e, rs=rs: e.reciprocal(rs[:, 0:w], rs[:, 0:w]), reads=[rsr], writes=[rsr])
            t1, t1r = S.get()
            fw.op("dve", lambda e, t1=t1, h=h, rs=rs: e.scalar_tensor_tensor(t1[:, 0:w], yacc[:, h, t0:t0 + w], sm[:, l, go:go + 1], rs[:, 0:w], ALU.mult, ALU.mult),
                  reads=allres + [rsr, smr], writes=[t1r])
            yo, yor = ystg[cnt % 2]
            cnt += 1
            fw.op("dve", lambda e, yo=yo, t1=t1, g_=g_: e.tensor_tensor(yo[:, 0:w], t1[:, 0:w], g_[:, 0:w], ALU.mult), reads=[t1r, g_r], writes=[yor])
            fw.dma("sp", yT.ap()[1, h * 128:(h + 1) * 128, t0:t0 + w], yo[:, 0:w], reads=[yor], writes=[yTr])
    P.phase_end()


def build_nab(rpb):
    kc = np.arange(64)
    qc = np.arange(64)
    cs = np.clip(qc - 8, 0, 48)
    valid = (kc[:, None] >= cs[None, :]) & (kc[:, None] < cs[None, :] + 16)
    dc = np.clip(kc[:, None] - qc[None, :] + 15, 0, 30)
    out = np.full((2, 64, 8, 16, 64), NEG, np.float32)
    for i2 in range(2):
        for dr in range(16):
            d2 = dr + i2
            if d2 > 14:
                continue
            g = rpb[:, d2, :][:, dc]
            g = np.where(valid[None], g, np.float32(NEG))
            out[i2, :, :, dr, :] = np.transpose(g, (1, 0, 2))
    return np.ascontiguousarray(out.reshape(128, 8 * 16 * 64))


def extra_host_inputs(inp):
    return {"nab": np.stack([build_nab(np.asarray(inp["na_rpb"][l], np.float32)) for l in range(DEPTH)])}


def ph_mix_na(P, l, ctx_out):
    fw = P.fw
    P.phase_begin()
    sm, smr = P.C["sm"]
    pT, pTr = P.dr["pT"]
    pV, pVr = P.dr["pV"]
    yT, yTr = P.dr["yT"]
    identb, identbr = P.C["ident_bf"]
    S = Scr(P, "nsc", 14)
    q, qr = P.sb("n_q", [128, 4, NT], BF16)
    k, kr = P.sb("n_k", [128, 4, NT], BF16)
    for c in range(4):
        interleave([
            prep_qk_g(P, S, pT.ap()[3584 + c * 128:3584 + (c + 1) * 128, :], pTr, q[:, c, :], qr, sm[:, l, SM["nqg"][0]:SM["nqg"][0] + 1], P.C["bd64"], 64),
            prep_qk_g(P, S, pT.ap()[4096 + c * 128:4096 + (c + 1) * 128, :], pTr, k[:, c, :], kr, sm[:, l, SM["nkg"][0]:SM["nkg"][0] + 1], P.C["bd64"], 64)])
    NA_STOP = 99
    Vx, Vxr = P.sb("n_vx", [128, NTB, 8, 128], BF16)
    pv = pV.ap()[2].rearrange("(tb p) (h d) -> p tb h d", p=128, h=8)
    for hh in range(8):
        fw.dma("sp", Vx[:, :, hh, 0:64], pv[:, :, hh, :], reads=[pVr], writes=[Vxr])
    fw.op("pool", lambda e: e.memset(Vx[:, :, :, 64:128], 1.0), writes=[Vxr])
    nab, nabr = P.sb("n_nab", [128, 8, 16, 64], F32)
    fw.dma("sp", nab[:].rearrange("p h r c -> p (h r c)"), P.dr["nab"][0].ap()[l], writes=[nabr])
    es = [P.sb("n_es%d" % i, [128, 8, 128], F32) for i in range(2)]
    Eb = [P.sb("n_E%d" % i, [128, 8, 128], BF16) for i in range(3)]
    ytm = [P.sb("n_ytm%d" % i, [128, 8, 64], BF16) for i in range(2)]
    rzt = [P.sb("n_rz%d" % i, [128, 8], F32) for i in range(2)]
    ystg = [P.sb("n_ys%d" % i, [128, 4, 128], BF16) for i in range(2)]
    blocks = []
    if ctx_out:
        blocks += [("ctx", 0), ("ctx", 1)]
    blocks += [("lat", pr) for pr in range(16)]
    ci = 0
    if NA_STOP <= 2:
        blocks = []
    elif 30 <= NA_STOP < 40 or NA_STOP == 3:
        blocks = blocks[:2]
    elif NA_STOP == 4:
        blocks = blocks[:3]
    for bi, (kind, idx) in enumerate(blocks):
        if kind == "ctx":
            qt0 = idx * 128
            chunks = [(0, None), (1, None)]
        else:
            r = 2 * idx
            qt0 = 256 + r * 64
            rs0 = min(max(r - 4, 0), 24)
            rs1 = min(max(r + 1 - 4, 0), 24)
            nloc = 4 if rs1 == rs0 else 5
            chunks = [(0, None), (1, None)]
            for j in range(nloc):
                info = []
                for a in range(2):
                    rsa = rs0 if a == 0 else rs1
                    dr0 = (rs0 + 2 * j) - (r + a) + 7
                    inval = [i2 for i2 in range(2) if not (rsa <= rs0 + 2 * j + i2 < rsa + 8)]
                    info.append((min(max(dr0, 0), 15), inval))
                chunks.append((2 + rs0 // 2 + j, info))
        Ob = [P.bank(4 + 2 * (bi % 2)), P.bank(5 + 2 * (bi % 2))]

        def _stage1(cj, kc, info):
            nonlocal ci
            Sb = [P.bank(2 * (ci % 2)), P.bank(2 * (ci % 2) + 1)]
            E, Er = Eb[ci % 3]
            e_, e_r = es[ci % 2]
            ci += 1
            for h in range(8):
                hb = 64 * (h % 2)
                sb_, sb_r = Sb[h % 2]
                fw.op("pe", lambda e, sb_=sb_, h=h, hb=hb, kc=kc: e.matmul(
                    sb_[:, (h // 2) * 128:(h // 2 + 1) * 128], k[hb:hb + 64, h // 2, kc * 128:(kc + 1) * 128],
                    q[hb:hb + 64, h // 2, qt0:qt0 + 128], start=True, stop=True), reads=[kr, qr], writes=[sb_r])
            for par in range(2):
                sb_, sb_r = Sb[par]
                hs = slice(par * 4, par * 4 + 4)
                if info is None:
                    fw.op("act", lambda e, sb_=sb_, par=par, E=E: e.activation(
                        out=E[:].rearrange("p h t -> p (h t)")[:, par * 512:(par + 1) * 512], in_=sb_[:], func=AF.Exp, scale=0.125),
                        reads=[sb_r], writes=[Er])
                else:
                    for a_ in range(2):
                        dr0, inval = info[a_]
                        fw.op("dve", lambda e, sb_=sb_, hs=hs, a_=a_, dr0=dr0, e_=e_, par=par: e.scalar_tensor_tensor(
                            e_[:, hs, a_ * 64:(a_ + 1) * 64], sb_[:].rearrange("p (h t) -> p h t", h=4)[:, :, a_ * 64:(a_ + 1) * 64], 0.125,
                            nab[:, par:8:2, dr0, :], ALU.mult, ALU.add), reads=[sb_r, nabr], writes=[e_r])
                    fw.op("act", lambda e, hs=hs, E=E, e_=e_: e.activation(out=E[:, hs, :], in_=e_[:, hs, :], func=AF.Exp),
                          reads=[e_r], writes=[Er])
            if info is not None:
                for a_ in range(2):
                    for i2 in info[a_][1]:
                        fw.op("pool", lambda e, E=E, a_=a_, i2=i2: e.memset(E[i2 * 64:(i2 + 1) * 64, :, a_ * 64:(a_ + 1) * 64], 0.0), writes=[Er])
            return (cj, kc, E, Er)

        def _stage2(cj, kc, E, Er):
            for h in range(8):
                ob, obr = Ob[h // 4]
                first = (cj == 0 and h % 4 == 0)
                last = (cj == len(chunks) - 1 and h % 4 == 3)
                fw.op("pe", lambda e, ob=ob, h=h, E=E, kc=kc, first=first, last=last: e.matmul(
                    ob[:, (h % 4) * 128:(h % 4 + 1) * 128], E[:, (h % 2) * 4 + h // 2, :], Vx[:, kc, h, :], start=first, stop=last, skip_group_check=True),
                    reads=[Er, Vxr], writes=[obr])

        pend = None
        for cj, (kc, info) in enumerate(chunks):
            cur = _stage1(cj, kc, info)
            if pend is not None:
                _stage2(*pend)
            pend = cur
        _stage2(*pend)
        rz_, rz_r = rzt[bi % 2]
        yt_, yt_r = ytm[bi % 2]
        if NA_STOP in (31, 32):
            continue
        for half in range(2):
            ob, obr = Ob[half]
            ov = ob[:].rearrange("p (h d) -> p h d", h=4)
            fw.op("dve", lambda e, ov=ov, half=half, rz_=rz_: e.reciprocal(rz_[:, half * 4:half * 4 + 4], ov[:, :, 64]), reads=[obr], writes=[rz_r])
            fw.op("dve", lambda e, ov=ov, half=half, rz_=rz_, yt_=yt_: e.tensor_tensor(
                yt_[:, half * 4:half * 4 + 4, :], ov[:, :, 0:64], rz_[:, half * 4:half * 4 + 4].unsqueeze(2).to_broadcast([128, 4, 64]), ALU.mult),
                reads=[obr, rz_r], writes=[yt_r])
        if NA_STOP == 33:
            continue
        tp, tpr = P.bank(2 * (ci % 2))
        ytf = yt_[:].rearrange("p h d -> p (h d)")
        for c in range(4):
            fw.op("pe", lambda e, c=c: e.matmul(tp[:, c * 128:(c + 1) * 128], ytf[:, c * 128:(c + 1) * 128], identb[:], start=True, stop=True),
                  reads=[yt_r, identbr], writes=[tpr])
        ys_, ys_r = ystg[bi % 2]
        fw.op("act", lambda e: e.copy(ys_[:].rearrange("p c t -> p (c t)"), tp[:]), reads=[tpr], writes=[ys_r])
        fw.dma("sp", yT.ap()[2].rearrange("(c p) t -> p c t", p=128)[:, :, qt0:qt0 + 128], ys_[:], reads=[ys_r], writes=[yTr])
    P.phase_end()


MERGE_SB = [[(0, 256, 1), (256, 512, 0)], [(768, 512, 0), (1280, 256, 0)], [(1536, 512, 0), (2048, 256, 0)]]


def ph_merge(P, l, ctx_out):
    fw = P.fw
    P.phase_begin()
    sm, smr = P.C["sm"]
    hT, hTr = P.dr["hT"]
    yT, yTr = P.dr["yT"]
    xT, xTr = P.dr["xT"]
    hv = hT.ap().rearrange("(fc p) t -> p fc t", p=128)
    yv = yT.ap().rearrange("n (c p) t -> p (n c) t", p=128)
    wg = P.dr["w_gate"][0].ap()
    wbr = P.dr["w_branch"][0].ap()
    wo = P.dr["w_o"][0].ap()
    h, hr = P.sb("g_h", [128, 16, 768], BF16)
    y, yr = P.sb("g_y", [128, 16, 768], BF16)
    m, mr = P.sb("g_m", [128, 16, 768], BF16)
    Wg = [P.sb("g_wg%d" % i, [128, 4, 16, 128], BF16) for i in range(2)]
    Wb = [P.sb("g_wb%d" % i, [128, 4, 4, 128], BF16) for i in range(2)]
    Wo = [P.sb("g_wo%d" % i, [128, 16, 128], BF16) for i in range(2)]
    gs = [P.sb("g_gs%d" % i, [128, 512], F32) for i in range(3)]
    tm = [P.sb("g_tm%d" % i, [128, 512], F32) for i in range(3)]
    mac = [P.sb("g_mac%d" % i, [128, 512], F32) for i in range(2)]
    xs = [P.sb("g_xs%d" % i, [128, 512], F32) for i in range(6)]
    bo = SM["b_gate"][0]
    wi = 0
    gi = 0
    xi = 0
    for sbi, subs in enumerate(MERGE_SB):
        s0 = subs[0][0]
        subs = [s_ for s_ in subs if (s_[2] == 0 or ctx_out)]
        for q4 in range(4):
            fw.dma("sp", h[:, q4 * 4:(q4 + 1) * 4, :], hv[:, q4 * 4:(q4 + 1) * 4, s0:s0 + 768], reads=[hTr], writes=[hr])
            fw.dma("sp", y[:, q4 * 4:(q4 + 1) * 4, :], yv[:, q4 * 4:(q4 + 1) * 4, s0:s0 + 768], reads=[yTr], writes=[yr])
        def _loadw(fc, slot):
            wgt, wgr = Wg[slot % 2]
            wbt, wbr_ = Wb[slot % 2]
            for n in range(4):
                fw.dma("pool", wgt[:, n], wg[l, n, :, fc * 128:(fc + 1) * 128].rearrange("(kc p) c -> p kc c", p=128), writes=[wgr])
            fw.dma("pool", wbt[:], wbr[l, :, :, fc * 128:(fc + 1) * 128].rearrange("n (kc p) c -> p n kc c", p=128), writes=[wbr_])

        def _loadwo(fo):
            wot, wor = Wo[fo % 2]
            fw.dma("pool", wot[:], wo[l, :, fo * 128:(fo + 1) * 128].rearrange("(kc p) c -> p kc c", p=128), writes=[wor])

        _loadw(0, wi)
        for fc in range(16):
            wgt, wgr = Wg[wi % 2]
            wbt, wbr_ = Wb[wi % 2]
            wi += 1
            if fc + 1 < 16:
                _loadw(fc + 1, wi)
            else:
                _loadwo(0)
            for (t0, w, j) in subs:
                c0 = t0 - s0
                ma, mar = mac[gi % 2]
                for n in range(4):
                    pg, pgr = P.bank()
                    for kc in range(16):
                        fw.op("pe", lambda e, pg=pg, n=n, kc=kc, wgt=wgt: e.matmul(pg[:, 0:w], wgt[:, n, kc, :], h[:, kc, c0:c0 + w], start=(kc == 0), stop=(kc == 15)),
                              reads=[wgr, hr], writes=[pgr])
                    g_, g_r = gs[gi % 3]
                    fw.op("act", lambda e, g_=g_, pg=pg, n=n: e.activation(out=g_[:, 0:w], in_=pg[:, 0:w], func=AF.Sigmoid, bias=sm[:, l, bo + n * 16 + fc:bo + n * 16 + fc + 1]),
                          reads=[pgr, smr], writes=[g_r])
                    pb, pbr = P.bank()
                    for kc in range(4):
                        fw.op("pe", lambda e, pb=pb, n=n, kc=kc, wbt=wbt: e.matmul(pb[:, 0:w], wbt[:, n, kc, :], y[:, n * 4 + kc, c0:c0 + w], start=(kc == 0), stop=(kc == 3)),
                              reads=[wbr_, yr], writes=[pbr])
                    if n == 0:
                        fw.op("dve", lambda e, ma=ma, g_=g_, pb=pb: e.tensor_tensor(ma[:, 0:w], g_[:, 0:w], pb[:, 0:w], ALU.mult), reads=[g_r, pbr], writes=[mar])
                    else:
                        t_, t_r = tm[gi % 3]
                        fw.op("dve", lambda e, t_=t_, g_=g_, pb=pb: e.tensor_tensor(t_[:, 0:w], g_[:, 0:w], pb[:, 0:w], ALU.mult), reads=[g_r, pbr], writes=[t_r])
                        if n < 3:
                            fw.op("dve", lambda e, ma=ma, t_=t_: e.tensor_tensor(ma[:, 0:w], ma[:, 0:w], t_[:, 0:w], ALU.add), reads=[t_r, mar], writes=[mar])
                        else:
                            fw.op("dve", lambda e, ma=ma, t_=t_: e.tensor_tensor(m[:, fc, c0:c0 + w], ma[:, 0:w], t_[:, 0:w], ALU.add), reads=[t_r, mar], writes=[mr])
                    gi += 1
        for fo in range(16):
            wot, wor = Wo[fo % 2]
            if fo + 1 < 16:
                _loadwo(fo + 1)
            for (t0, w, j) in subs:
                c0 = t0 - s0
                po, por = P.bank()
                for kc in range(16):
                    fw.op("pe", lambda e, po=po, kc=kc, wot=wot: e.matmul(po[:, 0:w], wot[:, kc, :], m[:, kc, c0:c0 + w], start=(kc == 0), stop=(kc == 15)),
                          reads=[wor, mr], writes=[por])
                x_, x_r = xs[xi % 6]
                xi += 1
                xres = Res("xtile")
                fw.dma("sp", x_[:, 0:w], xT.ap()[fo * 128:(fo + 1) * 128, t0:t0 + w], reads=[xres], writes=[x_r])
                fw.op("dve", lambda e, x_=x_, po=po, fo=fo, j=j: e.scalar_tensor_tensor(x_[:, 0:w], po[:, 0:w], P.modv[:, l, 2, fo, j:j + 1], x_[:, 0:w], ALU.mult, ALU.add),
                      reads=[por, x_r, P.modr], writes=[x_r])
                fw.dma("pool", xT.ap()[fo * 128:(fo + 1) * 128, t0:t0 + w], x_[:, 0:w], reads=[x_r], writes=[xres])
    P.phase_end()


def ph_transpose_out(P):
    fw = P.fw
    P.phase_begin()
    ident, identr = P.C["ident"]
    xT, xTr = P.dr["xT"]
    yo, yor = P.dr["y"]
    xTv = xT.ap().rearrange("(fc p) t -> p fc t", p=128)
    xin = [P.sb("to_x%d" % i, [128, 16, 128], F32) for i in range(2)]
    stg = [P.sb("to_s%d" % i, [128, 2048], F32) for i in range(2)]
    for tb in range(16):
        xt, xr = xin[tb % 2]
        fw.dma("sp", xt[:], xTv[:, :, L + tb * 128:L + (tb + 1) * 128], reads=[xTr], writes=[xr])
        st, sr = stg[tb % 2]
        for f4 in range(4):
            pst, psr = P.bank()
            for q in range(4):
                fc = f4 * 4 + q
                fw.op("pe", lambda e, pst=pst, q=q, xt=xt, fc=fc: e.transpose(pst[:, q * 128:(q + 1) * 128], xt[:, fc, :], ident[:]),
                      reads=[xr, identr], writes=[psr])
            if f4 % 2 == 0:
                fw.op("dve", lambda e, st=st, pst=pst, f4=f4: e.tensor_copy(st[:, f4 * 512:(f4 + 1) * 512], pst[:]), reads=[psr], writes=[sr])
            else:
                fw.op("act", lambda e, st=st, pst=pst, f4=f4: e.copy(st[:, f4 * 512:(f4 + 1) * 512], pst[:]), reads=[psr], writes=[sr])
        fw.dma("sp", yo.ap()[tb * 128:(tb + 1) * 128, :], st[:], reads=[sr], writes=[yor])
    P.phase_end()


def ph_peer_q(P, l):
    fw = P.fw
    P.phase_begin()
    hT, hTr = P.dr["hT"]
    qT, qTr = P.dr["qT"]
    h, hr = P.sb("pq_h", [128, 16, NT], BF16)
    hv = hT.ap().rearrange("(fc p) t -> p fc t", p=128)
    for q4 in range(4):
        fw.dma("sp", h[:, q4 * 4:(q4 + 1) * 4, :], hv[:, q4 * 4:(q4 + 1) * 4, :], reads=[hTr], writes=[hr])
    wbuf = [P.sb("pq_w%d" % i, [128, 16, 512], BF16) for i in range(2)]
    stg = [P.sb("pq_s%d" % i, [128, NT], BF16) for i in range(2)]
    wq = P.dr["peer_w_query"][0].ap()
    ev = 0
    for cg in range(4):
        wt, wr = wbuf[cg % 2]
        fw.dma("pool", wt[:], wq[l, :, cg * 512:(cg + 1) * 512].rearrange("(kc p) n -> p kc n", p=128), writes=[wr])
        for oc in range(4):
            st, sr = stg[oc % 2]
            for (t0, w, j) in TG:
                pst, psr = P.bank()
                for kc in range(16):
                    fw.op("pe", lambda e, pst=pst, kc=kc, wt=wt, oc=oc: e.matmul(pst[:, 0:w], wt[:, kc, oc * 128:(oc + 1) * 128], h[:, kc, t0:t0 + w],
                                                                               start=(kc == 0), stop=(kc == 15)), reads=[hr, wr], writes=[psr])
                if ev % 2 == 0:
                    fw.op("dve", lambda e, st=st, pst=pst: e.tensor_copy(st[:, t0:t0 + w], pst[:, 0:w]), reads=[psr], writes=[sr])
                else:
                    fw.op("act", lambda e, st=st, pst=pst: e.copy(st[:, t0:t0 + w], pst[:, 0:w]), reads=[psr], writes=[sr])
                ev += 1
            r0 = cg * 512 + oc * 128
            fw.dma("sp", qT.ap()[r0:r0 + 128, :], st[:], reads=[sr], writes=[qTr])
    P.phase_end()


def ph_peer_gate(P, l, ctx_out):
    fw = P.fw
    P.phase_begin()
    identb, identbr = P.C["ident_bf"]
    qT, qTr = P.dr["qT"]
    GdT, GdTr = P.dr["GdT"]
    qv = qT.ap().rearrange("(c p) t -> p c t", p=128)
    gv = GdT.ap().rearrange("(i j) t -> j i t", j=128)
    skn, sknr = P.sb("pg_skn", [128, 16, 128], BF16)
    fw.dma("pool", skn[:], P.dr["peer_sub_keys"][0].ap()[l].rearrange("h s k d -> k (h s) d"), writes=[sknr])
    SK, SKr = P.sb("pg_sk", [128, 16, 128], BF16)
    for b4 in range(4):
        pst, psr = P.bank()
        for q_ in range(4):
            c = b4 * 4 + q_
            fw.op("pe", lambda e, pst=pst, q_=q_, c=c: e.matmul(pst[:, q_ * 128:(q_ + 1) * 128], skn[:, c, :], identb[:], start=True, stop=True),
                  reads=[sknr, identbr], writes=[psr])
        fw.op("act", lambda e, pst=pst, b4=b4: e.copy(SK[:, b4 * 4:(b4 + 1) * 4, :].rearrange("p c k -> p (c k)"), pst[:]), reads=[psr], writes=[SKr])
    qts = [P.sb("pg_qt%d" % i, [128, 16, 128], BF16) for i in range(2)]
    ssbs = [P.sb("pg_s%d" % i, [128, 16, 128], F32) for i in range(2)]
    thrs = [P.sb("pg_thr%d" % i, [128, 8, 128], F32) for i in range(2)]
    lncs = [P.sb("pg_lnc%d" % i, [128, 32], F32) for i in range(2)]
    s2, s2r = P.sb("pg_s2", [128, 16, 128], F32)
    top, topr = P.sb("pg_top", [128, 16, 16], F32)
    cand, candr = P.sb("pg_cand", [128, 8, 256], F32)
    cand2, cand2r = P.sb("pg_cand2", [128, 8, 256], F32)
    c8, c8r = P.sb("pg_c8", [128, 8, 16], F32)
    selm, selmr = P.sb("pg_selm", [128, 8, 16], F32)
    zz, zzr = P.sb("pg_z", [128, 16], F32)
    Zt = [P.sb("pg_Z%d" % i, [128, 8, 128], F32) for i in range(6)]
    Mt = [P.sb("pg_M%d" % i, [128, 8, 128], BF16) for i in range(4)]
    Tt = [P.sb("pg_T%d" % i, [128, 8, 128], BF16) for i in range(5)]
    gst = [P.sb("pg_gst%d" % i, [128, 128, 128], BF16) for i in range(1)]
    tbs = list(range(NTB)) if ctx_out else list(range(2, NTB))
    if GATE_TBS is not None:
        tbs = GATE_TBS

    def prologue(ti):
        tb = tbs[ti]
        qt, qtr = qts[ti % 2]
        ssb, ssbr = ssbs[ti % 2]
        thr, thrr = thrs[ti % 2]
        lnc, lncr = lncs[ti % 2]
        fw.dma("sp", qt[:], qv[:, :, tb * 128:(tb + 1) * 128], reads=[qTr], writes=[qtr])
        yield
        for b4 in range(4):
            pst, psr = P.bank()
            for q_ in range(4):
                c = b4 * 4 + q_
                fw.op("pe", lambda e, pst=pst, q_=q_, c=c, qt=qt: e.matmul(pst[:, q_ * 128:(q_ + 1) * 128], qt[:, c, :], SK[:, c, :], start=True, stop=True),
                      reads=[qtr, SKr], writes=[psr])
            dst = ssb[:, b4 * 4:(b4 + 1) * 4, :].rearrange("p c k -> p (c k)")
            fw.op("dve", lambda e, pst=pst, dst=dst: e.tensor_copy(dst, pst[:]), reads=[psr], writes=[ssbr])
            yield
        for c in range(16):
            fw.op("dve", lambda e, c=c: e.max(out=top[:, c, 0:8], in_=ssb[:, c, :]), reads=[ssbr], writes=[topr])
            fw.op("dve", lambda e, c=c: e.match_replace(out=s2[:, c, :], in_to_replace=top[:, c, 0:8], in_values=ssb[:, c, :], imm_value=-1e30),
                  reads=[ssbr, topr], writes=[s2r])
            fw.op("dve", lambda e, c=c: e.max(out=top[:, c, 8:16], in_=s2[:, c, :]), reads=[s2r], writes=[topr])
            yield
        tv = top[:].rearrange("p (h s) a -> p h s a", s=2)
        fw.op("dve", lambda e: e.tensor_tensor(cand[:].rearrange("p h (a b) -> p h a b", a=16),
                                             tv[:, :, 0, :].unsqueeze(3).to_broadcast([128, 8, 16, 16]),
                                             tv[:, :, 1, :].unsqueeze(2).to_broadcast([128, 8, 16, 16]), ALU.add), reads=[topr], writes=[candr])
        yield
        for hh in range(8):
            fw.op("dve", lambda e, hh=hh: e.max(out=c8[:, hh, 0:8], in_=cand[:, hh, :]), reads=[candr], writes=[c8r])
            fw.op("dve", lambda e, hh=hh: e.match_replace(out=cand2[:, hh, :], in_to_replace=c8[:, hh, 0:8], in_values=cand[:, hh, :], imm_value=-1e30),
                  reads=[candr, c8r], writes=[cand2r])
            fw.op("dve", lambda e, hh=hh: e.max(out=c8[:, hh, 8:16], in_=cand2[:, hh, :]), reads=[cand2r], writes=[c8r])
            yield
        fw.op("dve", lambda e: e.tensor_tensor(selm[:], c8[:], c8[:, :, 0:1].to_broadcast([128, 8, 16]), ALU.subtract), reads=[c8r], writes=[selmr])
        fw.op("act", lambda e: e.activation(out=selm[:], in_=selm[:], func=AF.Exp), reads=[selmr], writes=[selmr])
        yield
        fw.op("dve", lambda e: e.tensor_reduce(zz[:, 0:8], selm[:], AX.X, ALU.add), reads=[selmr], writes=[zzr])
        sv = ssb[:].rearrange("p (h s) k -> p h s k", s=2)
        fw.op("dve", lambda e: e.tensor_tensor(thr[:], sv[:, :, 0, :], c8[:, :, 15:16].to_broadcast([128, 8, 128]), ALU.subtract),
              reads=[ssbr, c8r], writes=[thrr])
        yield
        fw.op("act", lambda e: e.activation(out=lnc[:, 0:8], in_=zz[:, 0:8], func=AF.Ln), reads=[zzr], writes=[lncr])
        fw.op("dve", lambda e: e.tensor_tensor(lnc[:, 8:16], c8[:, :, 15], c8[:, :, 0], ALU.subtract), reads=[c8r], writes=[lncr])
        yield
        fw.op("dve", lambda e: e.tensor_tensor(lnc[:, 16:24], lnc[:, 8:16], lnc[:, 0:8], ALU.subtract), reads=[lncr], writes=[lncr])
        fw.op("dve", lambda e: e.tensor_scalar(lnc[:, 24:32], lnc[:, 16:24], -5e-6, None, ALU.add), reads=[lncr], writes=[lncr])
        yield

    steps = [(ig, hh) for ig in range(16) for hh in range(8)]
    nst = len(steps)
    for _ in prologue(0):
        pass
    for ti, tb in enumerate(tbs):
        ssb, ssbr = ssbs[ti % 2]
        thr, thrr = thrs[ti % 2]
        lnc, lncr = lncs[ti % 2]
        sv = ssb[:].rearrange("p (h s) k -> p h s k", s=2)
        nxt = prologue(ti + 1) if ti + 1 < len(tbs) else None
        gs_, gs_r = gst[0]
        zb = {}
        eb = {}
        tbuf = {}

        def _A(si):
            ig, hh = steps[si]
            z_, z_r = Zt[si % len(Zt)]
            fw.op("dve", lambda e: e.tensor_tensor(
                z_[:], sv[:, hh, 1, :].unsqueeze(1).to_broadcast([128, 8, 128]),
                thr[:, hh, ig * 8:(ig + 1) * 8].unsqueeze(2).to_broadcast([128, 8, 128]), ALU.add), reads=[ssbr, thrr], writes=[z_r], inc=True)
            zb[si] = (z_, z_r)

        def _B(si):
            ig, hh = steps[si]
            z_, z_r = zb[si]
            if si % 4 != 3:
                T_, T_r = Tt[si % 5]
                fw.op("act", lambda e: e.activation(out=z_[:], in_=z_[:], func=AF.Prelu, alpha=1e5, bias=5e-6), reads=[z_r], writes=[z_r], inc=True)
                fw.op("act", lambda e: e.activation(out=T_[:], in_=z_[:], func=AF.Exp, bias=lnc[:, 24 + hh:25 + hh]), reads=[z_r, lncr], writes=[T_r], inc=True)
                tbuf[si] = (T_, T_r)
                zb.pop(si)
                return
            e_, e_r = Mt[si % 4]
            fw.op("act", lambda e: e.activation(out=e_[:], in_=z_[:], func=AF.Exp, bias=lnc[:, 16 + hh:17 + hh]), reads=[z_r, lncr], writes=[e_r], inc=True)
            eb[si] = (e_, e_r)

        def _C(si):
            if si % 4 != 3:
                return
            z_, z_r = zb.pop(si)
            e_, e_r = eb.pop(si)
            T_, T_r = Tt[si % 5]
            fw.op("dve", lambda e: e.scalar_tensor_tensor(T_[:], z_[:], -5e-6, e_[:], ALU.is_ge, ALU.mult), reads=[z_r, e_r], writes=[T_r], inc=True)
            tbuf[si] = (T_, T_r)

        banks = None
        _A(0)
        _A(1)
        _A(2)
        _B(0)
        _B(1)
        for si in range(nst):
            ig, hh = steps[si]
            if si + 3 < nst:
                _A(si + 3)
            if si + 2 < nst:
                _B(si + 2)
            _C(si)
            if hh == 0:
                banks = [P.bank(), P.bank()]
            T_, T_r = tbuf.pop(si)
            for ii in range(8):
                bk, bkr = banks[ii // 4]
                fw.op("pe", lambda e, bk=bk, ii=ii, T_=T_, hh=hh: e.matmul(
                    bk[:, (ii % 4) * 128:(ii % 4 + 1) * 128], T_[:, ii, :], identb[:],
                    start=(hh == 0 and ii % 4 == 0), stop=(hh == 7 and ii % 4 == 3), skip_group_check=True), reads=[T_r, identbr], writes=[bkr])
            if hh == 7:
                for b in range(2):
                    bk, bkr = banks[b]
                    fw.op("act", lambda e, bk=bk, b=b, ig=ig: e.copy(gs_[:, ig * 8 + b * 4:ig * 8 + b * 4 + 4, :].rearrange("p i t -> p (i t)"), bk[:]),
                          reads=[bkr], writes=[gs_r])
            if nxt is not None and si >= 8 and hh in (2, 5):
                next(nxt, None)
        if nxt is not None:
            for _ in nxt:
                pass
        for q4 in range(4):
            fw.dma("sp", gv[:, q4 * 32:(q4 + 1) * 32, tb * 128:(tb + 1) * 128], gs_[:, q4 * 32:(q4 + 1) * 32, :], reads=[gs_r], writes=[GdTr])
    P.phase_end()


def ph_peer_u(P, l, ctx_out):
    fw = P.fw
    P.phase_begin()
    identb, identbr = P.C["ident_bf"]
    hT, hTr = P.dr["hT"]
    GdT, GdTr = P.dr["GdT"]
    GAT, GATr = P.dr["GAT"]
    pu = P.dr["peer_u"][0].ap()
    h, hr = P.sb("pu_h", [128, 16, NT], BF16)
    hv = hT.ap().rearrange("(fc p) t -> p fc t", p=128)
    for q4 in range(4):
        fw.dma("sp", h[:, q4 * 4:(q4 + 1) * 4, :], hv[:, q4 * 4:(q4 + 1) * 4, :], reads=[hTr], writes=[hr])
    Ur = [P.sb("pu_ur%d" % i, [128, 2048], BF16) for i in range(2)]
    UT = [P.sb("pu_ut%d" % i, [128, 16, 128], BF16) for i in range(2)]
    gt = [P.sb("pu_g%d" % i, [128, NT], BF16) for i in range(2)]
    at = [P.sb("pu_a%d" % i, [128, 512], BF16) for i in range(3)]
    st = [P.sb("pu_s%d" % i, [128, NT], BF16) for i in range(2)]
    groups = [g for g in TG if (g[2] == 0 or ctx_out)]
    c_lo = 0 if ctx_out else L
    ai = 0
    def _load(ec):
        ur, urr = Ur[ec % 2]
        fw.dma("pool", ur[:], pu[l, ec * 128:(ec + 1) * 128, :], writes=[urr])
        g_, g_r = gt[ec % 2]
        fw.dma("sp", g_[:, c_lo:NT], GdT.ap()[ec * 128:(ec + 1) * 128, c_lo:NT], reads=[GdTr], writes=[g_r])

    _load(0)
    for ec in range(128):
        if ec + 1 < 128:
            _load(ec + 1)
        ur, urr = Ur[ec % 2]
        g_, g_r = gt[ec % 2]
        ut, utr = UT[ec % 2]
        for b4 in range(4):
            pst, psr = P.bank()
            for q_ in range(4):
                kc = b4 * 4 + q_
                fw.op("pe", lambda e, pst=pst, q_=q_, kc=kc, ur=ur: e.matmul(pst[:, q_ * 128:(q_ + 1) * 128], ur[:, kc * 128:(kc + 1) * 128], identb[:], start=True, stop=True),
                      reads=[urr, identbr], writes=[psr])
            dst = ut[:, b4 * 4:(b4 + 1) * 4, :].rearrange("p c k -> p (c k)")
            if b4 % 2 == 0:
                fw.op("dve", lambda e, pst=pst, dst=dst: e.tensor_copy(dst, pst[:]), reads=[psr], writes=[utr])
            else:
                fw.op("pool", lambda e, pst=pst, dst=dst: e.tensor_copy(dst, pst[:]), reads=[psr], writes=[utr]) if False else \
                    fw.op("dve", lambda e, pst=pst, dst=dst: e.tensor_copy(dst, pst[:]), reads=[psr], writes=[utr])
        s_, s_r = st[ec % 2]
        for (t0, w, j) in groups:
            pst, psr = P.bank()
            for kc in range(16):
                fw.op("pe", lambda e, pst=pst, kc=kc, ut=ut: e.matmul(pst[:, 0:w], ut[:, kc, :], h[:, kc, t0:t0 + w], start=(kc == 0), stop=(kc == 15)),
                      reads=[utr, hr], writes=[psr])
            a_, a_r = at[ai % 3]
            ai += 1
            fw.op("act", lambda e, a_=a_, pst=pst: e.activation(out=a_[:, 0:w], in_=pst[:, 0:w], func=AF.Gelu), reads=[psr], writes=[a_r])
            fw.op("pool", lambda e, a_=a_, s_=s_, g_=g_: e.tensor_tensor(s_[:, t0:t0 + w], a_[:, 0:w], g_[:, t0:t0 + w], ALU.mult), reads=[a_r, g_r], writes=[s_r])
        fw.dma("sp", GAT.ap()[ec * 128:(ec + 1) * 128, c_lo:NT], s_[:, c_lo:NT], reads=[s_r], writes=[GATr])
    P.phase_end()


def ph_peer_v(P, l, ctx_out):
    fw = P.fw
    P.phase_begin()
    GAT, GATr = P.dr["GAT"]
    xT, xTr = P.dr["xT"]
    pv = P.dr["peer_v"][0].ap()
    acc, accr = P.sb("pv_acc", [128, 8, NT], F32)
    ares = [[Res("acc%d_%d" % (dc, gi)) for gi in range(len(TG))] for dc in range(8)]
    GA = [P.sb("pv_ga%d" % i, [128, 4, NT], BF16) for i in range(2)]
    Vw = [P.sb("pv_v%d" % i, [128, 4, 1024], BF16) for i in range(2)]
    xs = [P.sb("pv_x%d" % i, [128, 512], F32) for i in range(6)]
    groups = [(gi, g) for gi, g in enumerate(TG) if (g[2] == 0 or ctx_out)]
    c_lo = 0 if ctx_out else L
    gav = GAT.ap().rearrange("(g e p) t -> g p e t", p=128, e=4)
    pvv = pv[l].rearrange("(g e p) d -> g p e d", p=128, e=4)
    xi = 0
    ev = 0
    for dh in range(2):
        for g in range(32):
            ga, gar = GA[g % 2]
            vw, vwr = Vw[g % 2]
            fw.dma("sp", ga[:, :, c_lo:NT], gav[g][:, :, c_lo:NT], reads=[GATr], writes=[gar])
            fw.dma("pool", vw[:], pvv[g][:, :, dh * 1024:(dh + 1) * 1024], writes=[vwr])
            for dc in range(8):
                for gi, (t0, w, j) in groups:
                    pst, psr = P.bank()
                    for e4 in range(4):
                        fw.op("pe", lambda e, pst=pst, e4=e4, vw=vw, ga=ga, dc=dc: e.matmul(pst[:, 0:w], vw[:, e4, dc * 128:(dc + 1) * 128], ga[:, e4, t0:t0 + w],
                                                                                          start=(e4 == 0), stop=(e4 == 3)), reads=[vwr, gar], writes=[psr])
                    ar = ares[dc][gi]
                    eng = "dve" if ev % 2 == 0 else "pool"
                    ev += 1
                    if g == 0:
                        if eng == "dve":
                            fw.op("dve", lambda e, pst=pst, dc=dc: e.tensor_copy(acc[:, dc, t0:t0 + w], pst[:, 0:w]), reads=[psr], writes=[ar])
                        else:
                            fw.op("act", lambda e, pst=pst, dc=dc: e.copy(acc[:, dc, t0:t0 + w], pst[:, 0:w]), reads=[psr], writes=[ar])
                    else:
                        fw.op("dve", lambda e, pst=pst, dc=dc: e.tensor_tensor(acc[:, dc, t0:t0 + w], acc[:, dc, t0:t0 + w], pst[:, 0:w], ALU.add),
                              reads=[psr, ar], writes=[ar])
        for dc in range(8):
            fo = dh * 8 + dc
            for gi, (t0, w, j) in groups:
                x_, x_r = xs[xi % 6]
                xi += 1
                xres = Res("xtile")
                fw.dma("sp", x_[:, 0:w], xT.ap()[fo * 128:(fo + 1) * 128, t0:t0 + w], reads=[xres], writes=[x_r])
                fw.op("dve", lambda e, x_=x_, dc=dc, fo=fo, j=j: e.scalar_tensor_tensor(x_[:, 0:w], acc[:, dc, t0:t0 + w], P.modv[:, l, 5, fo, j:j + 1], x_[:, 0:w], ALU.mult, ALU.add),
                      reads=[ares[dc][gi], x_r, P.modr], writes=[x_r])
                fw.dma("pool", xT.ap()[fo * 128:(fo + 1) * 128, t0:t0 + w], x_[:, 0:w], reads=[x_r], writes=[xres])
    P.phase_end()


_PROG = {}


def kernel(**inputs):
    inp = {k: np.asarray(v) for k, v in inputs.items()}
    if "p" not in _PROG:
        _PROG["p"] = build_program()
    P = _PROG["p"]
    consts = build_consts()
    smalls = np.stack([pack_smalls(inp, l) for l in range(DEPTH)])
    extra = extra_host_inputs(inp)
    in_maps = []
    for b in range(8):
        m = host_inputs(inp, b, consts, smalls)
        m.update(extra)
        in_maps.append({k: np.ascontiguousarray(m[k]) for k in P.inputs})
    res = run_bass_kernel_spmd(P.nc, in_maps, core_ids=list(range(8)))
    return np.stack([np.asarray(res.results[b]["y"]) for b in range(8)]).astype(np.float32)
```
